# Optimizing a Trainium2 kernel written in Bass

```python
import numpy as np
import jax, jax.numpy as jnp
from jax import lax

D_MODEL = 1024
BATCH = 4
SEQ = 4096
DEPTH = 2

HEAD_DIM = 64
NSA_HEADS = 8
NSA_KV_GROUPS = 2
NSA_CMP_BLOCK = 32
NSA_CMP_STRIDE = 16
NSA_CMP_HIDDEN = 128
NSA_SEL_BLOCK = 64
NSA_TOP_N = 16
NSA_WINDOW = 512
MOBA_HEADS = 8
MOBA_BLOCK = 256
MOBA_TOP_K = 3
GLA_HEADS = 4
GLA_DK = D_MODEL // (2 * GLA_HEADS)
GLA_DV = D_MODEL // GLA_HEADS
GLA_GATE_RANK = 16
GLA_TAU = 16.0
GLA_CHUNK = 64
Q_BLOCK = 32
LN_EPS = 1e-5
DEEPNORM_ALPHA = (2.0 * DEPTH) ** 0.25
DEEPNORM_BETA = (8.0 * DEPTH) ** -0.25
NEG = -1e30
FORCE_BONUS = 1e4

NSA_W = NSA_HEADS * HEAD_DIM
NSA_KV_W = NSA_KV_GROUPS * HEAD_DIM
MOBA_W = MOBA_HEADS * HEAD_DIM
MIX_W = NSA_W + MOBA_W
EVEN_SPLITS = (NSA_W, 6 * NSA_KV_W, 3 * NSA_HEADS, NSA_W, MOBA_W, MOBA_W, MOBA_W, MOBA_W)
EVEN_IN = sum(EVEN_SPLITS)
ODD_SPLITS = (GLA_HEADS * GLA_DK, GLA_HEADS * GLA_DK, GLA_HEADS * GLA_DV, GLA_GATE_RANK, GLA_HEADS * GLA_DV)
ODD_IN = sum(ODD_SPLITS)

kernel_name = "nsa_moba_gla_deepnorm_hybrid"


def _split(h, sizes):
    return jnp.split(h, np.cumsum(sizes)[:-1].tolist(), axis=-1)


def alibi_slopes(n):
    return jnp.asarray(2.0 ** (-8.0 * np.arange(1, n + 1) / n), dtype=jnp.float32)


def masked_softmax(s, valid):
    s = jnp.where(valid, s.astype(jnp.float32), NEG)
    return jax.nn.softmax(s, axis=-1) * valid


def layer_norm(x, g, b):
    xf = x.astype(jnp.float32)
    mu = jnp.mean(xf, axis=-1, keepdims=True)
    var = jnp.mean(jnp.square(xf - mu), axis=-1, keepdims=True)
    return ((xf - mu) * lax.rsqrt(var + LN_EPS) * g + b).astype(x.dtype)


def nsa_mix(q, k_cmp, v_cmp, k_sel, v_sel, k_win, v_win, gates, cmp_pos, cmp_w1, cmp_b1, cmp_w2):
    B, S = q.shape[0], q.shape[1]
    G, Hg, hd = NSA_KV_GROUPS, NSA_HEADS // NSA_KV_GROUPS, HEAD_DIM
    L, d, SB, W, QB = NSA_CMP_BLOCK, NSA_CMP_STRIDE, NSA_SEL_BLOCK, NSA_WINDOW, Q_BLOCK
    scale = hd ** -0.5
    slopes = alibi_slopes(NSA_HEADS).reshape(G, Hg)
    n_cmp = (S - L) // d + 1
    cmp_idx = np.arange(n_cmp)[:, None] * d + np.arange(L)[None, :]
    cmp_end = jnp.asarray(cmp_idx[:, -1])

    def compress(kv, i):
        blocks = kv[:, cmp_idx] + cmp_pos[i][None, None, :, None, :]
        blocks = blocks.transpose(0, 1, 3, 2, 4).reshape(B, n_cmp, G, L * hd)
        hid = jax.nn.silu(blocks @ cmp_w1[i] + cmp_b1[i])
        return hid @ cmp_w2[i]

    kc = compress(k_cmp, 0)
    vc = compress(v_cmp, 1)
    NS = S // SB
    sel_start = np.arange(NS) * SB
    overlap = jnp.asarray(((cmp_idx[:, 0][:, None] <= sel_start[None, :] + SB - 1)
                           & (cmp_idx[:, -1][:, None] >= sel_start[None, :])).astype(np.float32))
    n_top = min(NSA_TOP_N, NS)
    ks_blk = k_sel.reshape(B, NS, SB, G, hd).transpose(0, 3, 1, 2, 4)
    vs_blk = v_sel.reshape(B, NS, SB, G, hd).transpose(0, 3, 1, 2, 4)
    blk_ids = jnp.arange(NS)
    bi = jnp.arange(B)[:, None, None, None]
    gi = jnp.arange(G)[None, :, None, None]
    kw_pad = jnp.pad(k_win, ((0, 0), (W, 0), (0, 0), (0, 0)))
    vw_pad = jnp.pad(v_win, ((0, 0), (W, 0), (0, 0), (0, 0)))

    def block(c):
        q0 = c * QB
        tq = q0 + jnp.arange(QB)
        qc = lax.dynamic_slice_in_dim(q, q0, QB, axis=1).reshape(B, QB, G, Hg, hd)
        gc = lax.dynamic_slice_in_dim(gates, q0, QB, axis=1)
        dist_c = (tq[:, None] - cmp_end[None, :]).astype(jnp.float32)
        s = jnp.einsum('bcgjd,bngd->bgjcn', qc, kc) * scale - slopes[:, :, None, None] * dist_c
        p_cmp = masked_softmax(s, cmp_end[None, :] <= tq[:, None])
        o_cmp = jnp.einsum('bgjcn,bngd->bcgjd', p_cmp.astype(vc.dtype), vc)
        imp = jnp.einsum('bgjcn,ns->bgcs', p_cmp, overlap)
        cur = tq // SB
        forced = ((blk_ids[None, :] == 0) | (blk_ids[None, :] == cur[:, None])
                  | (blk_ids[None, :] == cur[:, None] - 1)).astype(jnp.float32)
        blk_valid = blk_ids[None, :] <= cur[:, None]
        score = jnp.where(blk_valid, imp + FORCE_BONUS * forced, NEG)
        _, idx = lax.top_k(score, n_top)
        kg = ks_blk[bi, gi, idx].reshape(B, G, QB, n_top * SB, hd)
        vg = vs_blk[bi, gi, idx].reshape(B, G, QB, n_top * SB, hd)
        kpos = (idx[..., None] * SB + jnp.arange(SB)).reshape(B, G, QB, n_top * SB)
        dist_s = (tq[:, None] - kpos).astype(jnp.float32)[:, :, None]
        s = jnp.einsum('bcgjd,bgckd->bgjck', qc, kg) * scale - slopes[None, :, :, None, None] * dist_s
        p = masked_softmax(s, (kpos <= tq[:, None])[:, :, None])
        o_sel = jnp.einsum('bgjck,bgckd->bcgjd', p.astype(vg.dtype), vg)
        kw = lax.dynamic_slice_in_dim(kw_pad, q0, W + QB, axis=1)
        vw = lax.dynamic_slice_in_dim(vw_pad, q0, W + QB, axis=1)
        kpos_w = q0 - W + jnp.arange(W + QB)
        rel = tq[:, None] - kpos_w[None, :]
        valid_w = (rel >= 0) & (rel < W) & (kpos_w[None, :] >= 0)
        s = jnp.einsum('bcgjd,bkgd->bgjck', qc, kw) * scale - slopes[:, :, None, None] * rel.astype(jnp.float32)
        p = masked_softmax(s, valid_w)
        o_win = jnp.einsum('bgjck,bkgd->bcgjd', p.astype(vw.dtype), vw)
        g = jax.nn.sigmoid(gc.astype(jnp.float32)).reshape(B, QB, G, Hg, 3)
        o = g[..., 0:1] * o_cmp + g[..., 1:2] * o_sel + g[..., 2:3] * o_win
        return o.reshape(B, QB, NSA_W).astype(q.dtype)

    out = lax.map(block, jnp.arange(S // QB))
    return out.transpose(1, 0, 2, 3).reshape(B, S, NSA_W)


def moba_mix(q, k, v):
    B, S, H, hd = q.shape
    MB, QB = MOBA_BLOCK, Q_BLOCK
    NB = -(-S // MB)
    pad = NB * MB - S
    kp = jnp.pad(k, ((0, 0), (0, pad), (0, 0), (0, 0)))
    vp = jnp.pad(v, ((0, 0), (0, pad), (0, 0), (0, 0)))
    kblk = kp.reshape(B, NB, MB, H, hd)
    kmean = jnp.mean(kblk, axis=2)
    kbt = kblk.transpose(0, 3, 1, 2, 4)
    vbt = vp.reshape(B, NB, MB, H, hd).transpose(0, 3, 1, 2, 4)
    n_top = min(MOBA_TOP_K, NB)
    scale = hd ** -0.5
    slopes = alibi_slopes(H)[:, None, None]
    bi = jnp.arange(B)[:, None, None, None]
    hi = jnp.arange(H)[None, :, None, None]

    def block(c):
        q0 = c * QB
        tq = q0 + jnp.arange(QB)
        qc = lax.dynamic_slice_in_dim(q, q0, QB, axis=1)
        cur = tq // MB
        gate = jnp.einsum('bchd,bnhd->bhcn', qc, kmean).astype(jnp.float32)
        past = jnp.arange(NB)[None, :] < cur[:, None]
        _, idx = lax.top_k(jnp.where(past, gate, NEG), n_top)
        sel_valid = idx < cur[:, None]
        kg = kbt[bi, hi, idx].reshape(B, H, QB, n_top * MB, hd)
        vg = vbt[bi, hi, idx].reshape(B, H, QB, n_top * MB, hd)
        kpos_sel = (idx[..., None] * MB + jnp.arange(MB)).reshape(B, H, QB, n_top * MB)
        valid_sel = jnp.repeat(sel_valid, MB, axis=-1)
        own0 = (q0 // MB) * MB
        ko = lax.dynamic_slice_in_dim(kp, own0, MB, axis=1)
        vo = lax.dynamic_slice_in_dim(vp, own0, MB, axis=1)
        kpos_own = own0 + jnp.arange(MB)
        full = (B, H, QB, MB)
        s = jnp.concatenate([jnp.einsum('bchd,bhckd->bhck', qc, kg),
                             jnp.einsum('bchd,bkhd->bhck', qc, ko)], axis=-1) * scale
        dist = jnp.concatenate([tq[:, None] - kpos_sel,
                                jnp.broadcast_to(tq[:, None] - kpos_own[None, :], full)], axis=-1)
        valid = jnp.concatenate([valid_sel,
                                 jnp.broadcast_to(kpos_own[None, :] <= tq[:, None], full)], axis=-1)
        p = masked_softmax(s - slopes * dist.astype(jnp.float32), valid)
        ns = n_top * MB
        o = (jnp.einsum('bhck,bhckd->bchd', p[..., :ns].astype(vg.dtype), vg)
             + jnp.einsum('bhck,bkhd->bchd', p[..., ns:].astype(vo.dtype), vo))
        return o.reshape(B, QB, H * hd).astype(q.dtype)

    out = lax.map(block, jnp.arange(S // QB))
    return out.transpose(1, 0, 2, 3).reshape(B, S, H * hd)


def gla_mix(q, k, v, log_a):
    B, S, H, dk = q.shape
    dv = v.shape[-1]
    C = GLA_CHUNK
    n = S // C

    def to_chunks(t):
        return t.reshape(B, n, C, H, t.shape[-1]).transpose(1, 0, 3, 2, 4)

    causal = jnp.tril(jnp.ones((C, C), dtype=bool))

    def step(state, inp):
        qc, kc, vc, ac = inp
        b = jnp.cumsum(ac, axis=2)
        inter = jnp.einsum('bhcd,bhde->bhce', qc * jnp.exp(b), state)
        diff = b[:, :, :, None, :] - b[:, :, None, :, :]
        decay = jnp.exp(jnp.where(causal[:, :, None], diff, NEG))
        att = jnp.einsum('bhid,bhjd,bhijd->bhij', qc, kc, decay)
        intra = jnp.einsum('bhij,bhje->bhie', att, vc)
        b_last = b[:, :, -1:, :]
        new_state = (jnp.exp(b_last[:, :, 0, :])[..., None] * state
                     + jnp.einsum('bhcd,bhce->bhde', kc * jnp.exp(b_last - b), vc))
        return new_state, inter + intra

    s0 = jnp.zeros((B, H, dk, dv), jnp.float32)
    _, out = lax.scan(step, s0, (to_chunks(q), to_chunks(k), to_chunks(v), to_chunks(log_a)))
    return out.transpose(1, 0, 3, 2, 4).reshape(B, S, H, dv)


def even_layer(x, w_in, cmp_pos, cmp_w1, cmp_b1, cmp_w2, w_out, ln_g, ln_b):
    B, S, _ = x.shape
    h = x @ w_in
    nq, nkv, ngate, nz, mq, mk, mv, mz = _split(h, EVEN_SPLITS)
    kvs = [t.reshape(B, S, NSA_KV_GROUPS, HEAD_DIM) for t in _split(nkv, (NSA_KV_W,) * 6)]
    o_nsa = nsa_mix(nq.reshape(B, S, NSA_HEADS, HEAD_DIM), *kvs,
                    ngate.reshape(B, S, NSA_HEADS, 3), cmp_pos, cmp_w1, cmp_b1, cmp_w2)
    hs = (B, S, MOBA_HEADS, HEAD_DIM)
    o_moba = moba_mix(mq.reshape(hs), mk.reshape(hs), mv.reshape(hs))
    y = jnp.concatenate([o_nsa * jax.nn.silu(nz), o_moba * jax.nn.silu(mz)], axis=-1) @ w_out
    return layer_norm(DEEPNORM_ALPHA * x + y, ln_g, ln_b)


def odd_layer(x, w_in, gate_w2, gate_b, gn_g, w_out, ln_g, ln_b):
    B, S, _ = x.shape
    h = x @ w_in
    q, k, v, g_lr, z = _split(h, ODD_SPLITS)
    f32 = jnp.float32
    q = q.reshape(B, S, GLA_HEADS, GLA_DK).astype(f32) * (GLA_DK ** -0.5)
    k = k.reshape(B, S, GLA_HEADS, GLA_DK).astype(f32)
    v = v.reshape(B, S, GLA_HEADS, GLA_DV).astype(f32)
    log_a = jax.nn.log_sigmoid((g_lr @ gate_w2 + gate_b).astype(f32)) / GLA_TAU
    o = gla_mix(q, k, v, log_a.reshape(B, S, GLA_HEADS, GLA_DK))
    o = o * lax.rsqrt(jnp.mean(jnp.square(o), axis=-1, keepdims=True) + LN_EPS)
    o = o * gn_g.reshape(GLA_HEADS, GLA_DV)
    y = (o.reshape(B, S, GLA_HEADS * GLA_DV).astype(x.dtype) * jax.nn.silu(z)) @ w_out
    return layer_norm(DEEPNORM_ALPHA * x + y, ln_g, ln_b)


def setup_inputs(seed: int = 0) -> dict:
    key = jax.random.key(seed)
    ks = jax.random.split(key, 16)
    NE = (DEPTH + 1) // 2
    NO = DEPTH // 2
    f32 = jnp.float32

    def nrm(k, shape, scale):
        return jax.random.normal(k, shape, f32) * scale

    L, hd, HID = NSA_CMP_BLOCK, HEAD_DIM, NSA_CMP_HIDDEN
    return {
        "x": jax.random.normal(ks[0], (BATCH, SEQ, D_MODEL), f32),
        "ev_w_in": nrm(ks[1], (NE, D_MODEL, EVEN_IN), D_MODEL ** -0.5),
        "ev_cmp_pos": nrm(ks[2], (NE, 2, L, hd), 0.02),
        "ev_cmp_w1": nrm(ks[3], (NE, 2, L * hd, HID), (L * hd) ** -0.5),
        "ev_cmp_b1": nrm(ks[4], (NE, 2, HID), 0.01),
        "ev_cmp_w2": nrm(ks[5], (NE, 2, HID, hd), HID ** -0.5),
        "ev_w_out": nrm(ks[6], (NE, MIX_W, D_MODEL), DEEPNORM_BETA * MIX_W ** -0.5),
        "ev_ln_g": 1.0 + nrm(ks[7], (NE, D_MODEL), 0.01),
        "ev_ln_b": nrm(ks[8], (NE, D_MODEL), 0.01),
        "od_w_in": nrm(ks[9], (NO, D_MODEL, ODD_IN), D_MODEL ** -0.5),
        "od_gate_w2": nrm(ks[10], (NO, GLA_GATE_RANK, GLA_HEADS * GLA_DK), GLA_GATE_RANK ** -0.5),
        "od_gate_b": nrm(ks[11], (NO, GLA_HEADS * GLA_DK), 0.1),
        "od_gn_g": 1.0 + nrm(ks[12], (NO, GLA_HEADS * GLA_DV), 0.01),
        "od_w_out": nrm(ks[13], (NO, GLA_HEADS * GLA_DV, D_MODEL), DEEPNORM_BETA * (GLA_HEADS * GLA_DV) ** -0.5),
        "od_ln_g": 1.0 + nrm(ks[14], (NO, D_MODEL), 0.01),
        "od_ln_b": nrm(ks[15], (NO, D_MODEL), 0.01),
    }


def reference(x, ev_w_in, ev_cmp_pos, ev_cmp_w1, ev_cmp_b1, ev_cmp_w2, ev_w_out, ev_ln_g, ev_ln_b,
              od_w_in, od_gate_w2, od_gate_b, od_gn_g, od_w_out, od_ln_g, od_ln_b):
    for layer in range(DEPTH):
        i = layer // 2
        if layer % 2 == 0:
            x = even_layer(x, ev_w_in[i], ev_cmp_pos[i], ev_cmp_w1[i], ev_cmp_b1[i], ev_cmp_w2[i],
                           ev_w_out[i], ev_ln_g[i], ev_ln_b[i])
        else:
            x = odd_layer(x, od_w_in[i], od_gate_w2[i], od_gate_b[i], od_gn_g[i],
                          od_w_out[i], od_ln_g[i], od_ln_b[i])
    return x
```

```python
import numpy as np
from contextlib import ExitStack
import concourse.bass as bass
import concourse.mybir as mybir
from concourse.bass_utils import run_bass_kernel_spmd

F32 = mybir.dt.float32
BF16 = mybir.dt.bfloat16
AF = mybir.ActivationFunctionType
ALU = mybir.AluOpType
AX = mybir.AxisListType

S = 4096
D = 1024
NT = S // 128
NST = S // 512
SH = S // 2
ALPHA = float((2.0 * 2) ** 0.25)
LN_EPS = 1e-5
EV_IN = 3864
OD_IN = 3088
NEGM = -30000.0

EPOCH = 30000
STRICT_SAME_ENGINE = False
import os
N_DUMMY = int(os.environ.get('N_DUMMY', '1'))
N_FILL_SEL = int(os.environ.get('N_FILL_SEL', '0'))
L1_FILL = int(os.environ.get('L1_FILL', '0'))
FILL_N = int(os.environ.get('FILL_N', '512'))
NDMA = 24


class Buf:
    __slots__ = ("name", "w", "r")

    def __init__(self, name=""):
        self.name = name
        self.w = []
        self.r = []


class Ctx:
    def __init__(self, nc, stack):
        self.nc = nc
        self.stack = stack
        self.cur = stack
        self.eng = {"pe": nc.tensor, "act": nc.scalar, "dve": nc.vector,
                    "pool": nc.gpsimd, "sp": nc.sync}
        self.sem = {}
        self.cnt = {}
        self.nsem = 0
        for k in self.eng:
            self._new_sem(k)
        self.waited = {}
        self.dma_sems = [[self._alloc(f"dma{i}") for i in range(NDMA)], [self._alloc(f"swdma{i}") for i in range(12)]]
        self.dma_val = [[0] * NDMA, [0] * 12]
        self.dma_rr = [0, 0]
        self.sw_tickets = []
        self.n_ins = 0
        self.n_wait = 0
        self.nname = 0

    def _alloc(self, name):
        self.nsem += 1
        return self.stack.enter_context(self.nc.semaphore(f"{name}_{self.nsem}"))

    def _new_sem(self, k):
        self.sem[k] = self._alloc(f"e_{k}")
        self.cnt[k] = 0

    def sb(self, shape, dt, name=None):
        self.nname += 1
        return self.cur.enter_context(
            self.nc.sbuf_tensor(f"{name or 'sb'}_{self.nname}", list(shape), dt))

    def barrier(self):
        tickets = [(self.sem[k], self.cnt[k], k) for k in self.eng if self.cnt[k] > 0]
        for g_ in range(2):
            tickets += [(self.dma_sems[g_][i], v, "dma") for i, v in enumerate(self.dma_val[g_]) if v > 0]
        tickets += self.sw_tickets
        for e in self.eng:
            self._wait(e, [t for t in tickets if t[2] != e])

    def phase(self):
        ctx = self

        class _Ph:
            def __enter__(self_):
                self_.prev = ctx.cur
                self_.st = ExitStack()
                self_.st.__enter__()
                ctx.cur = self_.st
                return self_

            def __exit__(self_, *a):
                ctx.barrier()
                ctx.cur = self_.prev
                return self_.st.__exit__(*a)
        return _Ph()

    def ps(self, shape, dt, name=None):
        self.nname += 1
        return self.stack.enter_context(
            self.nc.psum_tensor(f"{name or 'ps'}_{self.nname}", list(shape), dt))

    def _wait(self, eng, tickets):
        best = {}
        for t in tickets:
            sem, val, src = t
            key = (eng, id(sem))
            if self.waited.get(key, 0) >= val:
                continue
            if key not in best or best[key][1] < val:
                best[key] = t
        for key, (sem, val, src) in best.items():
            self.eng[eng].wait_ge(sem, val)
            self.waited[key] = val
            self.n_wait += 1

    def _deps(self, eng, reads, writes):
        deps = []
        for b in reads:
            deps.extend(b.w)
        for b in writes:
            for t in b.w:
                if t[2] != eng or (STRICT_SAME_ENGINE and eng not in ("pe", "dma")):
                    deps.append(t)
            for t in b.r:
                if t[2] != eng or (STRICT_SAME_ENGINE and eng != "pe") or eng == "dma":
                    deps.append(t)
        return deps

    def _commit(self, ticket, reads, writes):
        for b in reads:
            b.r.append(ticket)
            if len(b.r) > 64:
                best = {}
                for t in b.r:
                    k = id(t[0])
                    if k not in best or best[k][1] < t[1]:
                        best[k] = t
                b.r = list(best.values())
        for b in writes:
            if ticket[2] == "dma" and b.w and all(t[2] == "dma" for t in b.w):
                b.w = b.w + [ticket]
                if len(b.w) > 32:
                    best = {}
                    for t in b.w:
                        k = id(t[0])
                        if k not in best or best[k][1] < t[1]:
                            best[k] = t
                    b.w = list(best.values())
            else:
                b.w = [ticket]
            b.r = []

    def op(self, eng, fn, reads=(), writes=()):
        self._wait(eng, self._deps(eng, reads, writes))
        ins = fn()
        if self.cnt[eng] >= EPOCH:
            self._new_sem(eng)
        self.cnt[eng] += 1
        ins.then_inc(self.sem[eng], 1)
        ticket = (self.sem[eng], self.cnt[eng], eng)
        self._commit(ticket, reads, writes)
        self.n_ins += 1
        return ticket

    def dma(self, q, out, in_, reads=(), writes=(), **kw):
        grp = 1 if q == "pool" else 0
        i = self.dma_rr[grp]
        self.dma_rr[grp] = (i + 1) % len(self.dma_sems[grp])
        sem = self.dma_sems[grp][i]
        deps = self._deps("dma", reads, writes)
        if self.dma_val[grp][i] > 0:
            deps.append((sem, self.dma_val[grp][i], "dma"))
        self._wait(q, deps)
        ins = self.eng[q].dma_start(out=out, in_=in_, **kw)
        self.dma_val[grp][i] += 16
        ins.then_inc(sem, 16)
        ticket = (sem, self.dma_val[grp][i], "dma")
        self._commit(ticket, reads, writes)
        self.n_ins += 1
        return ticket

    def wait_all(self, eng, bufs):
        deps = []
        for b in bufs:
            deps.extend(b.w)
            deps.extend(b.r)
        self._wait(eng, deps)


def _bf16(a):
    import ml_dtypes
    return np.asarray(a, dtype=np.float32).astype(ml_dtypes.bfloat16)


def make_consts(r=0):
    c = {}
    c["ident_f"] = np.eye(128, dtype=np.float32)
    c["ident_b"] = _bf16(np.eye(128))
    k = np.arange(128)[:, None]
    q = np.arange(128)[None, :]
    c["tri_b"] = _bf16((k <= q).astype(np.float32))
    c["ones_f"] = np.ones((128, 128), np.float32)
    slopes = 2.0 ** (-(np.arange(8) + 1.0))
    tq = np.arange(S, dtype=np.float64)
    qal = np.zeros((3, 8, S), np.float32)
    for h in range(8):
        qal[0, h] = 8.0 * slopes[h]
        qal[1, h] = 1024.0 * slopes[h]
        qal[2, h] = -8.0 * slopes[h] * tq
    c["qal"] = _bf16(qal[:, 4 * r:4 * r + 4, :])
    sel = np.zeros((128, 2), np.float32)
    sel[:, r] = 1.0
    c["sel"] = sel
    kp = np.arange(S)
    kal = np.stack([kp % 128, kp // 128, np.ones(S)]).astype(np.float32)
    c["kaug_n"] = _bf16(kal)
    cp = 16 * np.arange(255) + 31
    c["kaug_c"] = _bf16(np.stack([cp % 128, cp // 128, np.ones(255)]).astype(np.float32))
    e16 = (kp[None, :] // 256 == np.arange(16)[:, None]).astype(np.float32) * 30000.0
    c["kaug_m"] = _bf16(np.concatenate([e16, kal], axis=0))
    c["esel"] = _bf16((kp[None, :] // 64 == np.arange(64)[:, None]).astype(np.float32) * 30000.0)
    kk = np.arange(128)[:, None, None] + 128 * np.arange(4)[None, :, None]
    qq = np.arange(512)[None, None, :]
    c["cm"] = _bf16(np.where(kk <= qq, 0.0, NEGM))
    c["wm"] = _bf16(np.where(kk > qq, 0.0, NEGM))
    n = np.arange(128)[:, None, None] + 128 * np.arange(2)[None, :, None]
    qs = np.arange(S)[None, None, :]
    c["cmpm"] = _bf16(np.where((16 * n + 31 <= qs) & (n < 255), 0.0, NEGM))
    n2 = np.arange(128)[:, None, None] + 128 * np.arange(2)[None, :, None]
    sb = np.arange(64)[None, None, :]
    c["ovl"] = _bf16(((16 * n2 <= 64 * sb + 63) & (16 * n2 + 31 >= 64 * sb) & (n2 < 255)).astype(np.float32))
    tqm = (np.arange(128)[:, None, None] + 128 * np.arange(32)[None, :, None])
    cur = tqm // 64
    forced = (sb == 0) | (sb == cur) | (sb == cur - 1)
    c["selc"] = np.where(sb <= cur, np.where(forced, 1e4, 0.0), -1e30).astype(np.float32)
    nb = np.arange(16)[None, None, :]
    curm = tqm // 256
    mobc = np.where(nb < curm, 0.0, -1e30).astype(np.float32)
    ownc = np.where(nb == curm, 0.0, -1.0).astype(np.float32)
    c["mobc4"] = np.ascontiguousarray(np.broadcast_to(mobc.reshape(128, NST, 4, 1, 16), (128, NST, 4, 4, 16))).reshape(128, NST, 256)
    c["ownc4"] = np.ascontiguousarray(np.broadcast_to(ownc.reshape(128, NST, 4, 1, 16), (128, NST, 4, 4, 16))).reshape(128, NST, 256)
    return c


class _Stop(Exception):
    pass


class Prog:
    def __init__(self, mode="full"):
        self.mode = mode
        self.nc = bass.Bass("TRN2", target_bir_lowering=False)
        self.din = {}
        self.build()

    def dram_in(self, name, shape, dt=F32):
        t = self.nc.dram_tensor(name, list(shape), dt, kind="ExternalInput").ap()
        self.din[name] = t
        return t

    def build(self):
        nc = self.nc
        x = self.dram_in("x", [S, D])
        xh = self.dram_in("xh", [SH, D])
        W = {}
        for nm, shp in [("ev_wm", [D, 1024]), ("ev_wn", [D, 908]), ("ev_cmp_pos", [2, 64, 32]), ("ev_cmp_w1", [2, 2048, 128]),
                        ("ev_cmp_b1", [128, 2]), ("ev_cmp_w2", [2, 128, 64]), ("ev_w_out", [D, D]),
                        ("ev_ln_g", [1, D]), ("ev_ln_b", [1, D]), ("od_w1", [D, 1552]),
                        ("od_gate_w2", [16, 256]), ("od_gate_b", [128, 2]), ("od_gn_g", [1, 512]),
                        ("od_w_out", [D, D]), ("od_ln_g", [1, D]), ("od_ln_b", [1, D])]:
            W[nm] = self.dram_in(nm, shp)
        self.W = W
        C = {}
        for nm, arr in make_consts().items():
            C[nm] = self.dram_in("c_" + nm, list(arr.shape), F32 if arr.dtype == np.float32 else BF16)
        self.Cd = C
        out = nc.dram_tensor("out", [SH, D], F32, kind="ExternalOutput").ap()
        self.x, self.xh, self.out = x, xh, out
        self.x1d = nc.dram_tensor("x1d", [SH, D], F32).ap()
        self.ogl = [nc.dram_tensor(f"ogl{h}", [SH, 512], BF16).ap() for h in range(2)]
        self.ogg = [nc.dram_tensor(f"ogg{h}", [2 * SH, 512], BF16).ap() for h in range(2)]
        self.xTl = [nc.dram_tensor(f"xTl{q}", [D, 512], BF16).ap() for q in range(4)]
        self.xTg = [nc.dram_tensor(f"xTg{q}", [2 * D, 512], BF16).ap() for q in range(4)]
        self.og1l = [nc.dram_tensor(f"og1l{h}", [SH, 512], BF16).ap() for h in range(2)]
        self.og1g = [nc.dram_tensor(f"og1g{h}", [2 * SH, 512], BF16).ap() for h in range(2)]
        self.dbg = None
        self.bx1d = [Buf(f"x1d{t}") for t in range(NT // 2)]
        mk = lambda n: [Buf(n + "0"), Buf(n + "1")]
        self.bogl = mk("ogl"); self.bogg = mk("ogg"); self.bxTl = [Buf(f"xTl{q}") for q in range(4)]; self.bxTg = [Buf(f"xTg{q}") for q in range(4)]
        self.bog1l = mk("og1l"); self.bog1g = mk("og1g")

        with ExitStack() as st:
            c = Ctx(nc, st)
            self.c = c
            self.pf = [c.ps([128, 512], F32, "pf") for _ in range(7)]
            self.bpf = [Buf(f"pf{i}") for i in range(7)]
            pb0 = c.ps([128, 1024], BF16, "pb")
            self.pb = [pb0, pb0]
            b_pb0 = Buf("pb0")
            self.bpb = [b_pb0, b_pb0]
            self.ident_f = c.sb([128, 128], F32); self.ident_b = c.sb([128, 128], BF16)
            self.tri_b = c.sb([128, 128], BF16); self.ones_f = c.sb([128, 128], F32)
            self.sel = c.sb([128, 2], F32)
            self.bconst = Buf("const")
            for t, nm in [(self.ident_f, "ident_f"), (self.ident_b, "ident_b"), (self.tri_b, "tri_b"),
                          (self.ones_f, "ones_f"), (self.sel, "sel")]:
                c.dma("sp", t[:], C[nm][:, :], writes=[self.bconst])
            self.xT = c.sb([128, 8, S], BF16, "xT")
            self.bxT = [Buf(f"xT{t}") for t in range(NT)]
            self.wpass = [c.sb([128, 8, 1024], BF16, "wpA"), c.sb([128, 8, 1024], BF16, "wpB")]
            self.bwpass = [Buf("wpA"), Buf("wpB")]
            self.out_bufs = []
            self.layer0()
            with c.phase():
                l1w = self.l1_weights()
                self.final_pass(self.ogg, self.bogg, (self.wpass[0], self.bwpass[0]), self.xh, None, W["ev_ln_g"], W["ev_ln_b"],
                                self.x1d, self.bx1d, make_xT=True)
                with c.phase():
                    self.layer1(weights=l1w)
                bout = Buf("out"); self.out_bufs.append(bout)
                self.final_pass(self.og1g, self.bog1g, (l1w[2], l1w[3]), self.x1d, self.bx1d, W["od_ln_g"], W["od_ln_b"],
                                self.out, [bout] * (NT // 2), make_xT=False)
            c.wait_all("sp", self.out_bufs)
            print("instructions", c.n_ins, "waits", c.n_wait, "sems", c.nsem)

    def coll(self, kind, src, dst, bsrc, bdst):
        c, nc = self.c, self.nc
        c._wait("pool", c._deps("dma", [bsrc], [bdst]))
        sem = c._alloc("cc")
        ins = nc.gpsimd.collective_compute(kind, ALU.bypass, replica_groups=[[0, 1], [2, 3], [4, 5], [6, 7]],
                                           ins=[src[:, :]], outs=[dst[:, :]])
        ins.then_inc(sem, 1)
        tk = (sem, 1, "dma")
        c.sw_tickets.append(tk)
        c._commit(tk, [bsrc], [bdst])

    def load_weight_bf16(self, dst, dst_buf, src, col0, ncols):
        c = self.c
        for o in range(0, ncols, 1024):
            n = min(1024, ncols - o)
            c.dma("pool", dst[:, :, o:o + n],
                  src[:, col0 + o:col0 + o + n].rearrange("(c p) n -> p c n", p=128), writes=[dst_buf])

    def load_xT_alloc(self):
        c = self.c
        self.xin = [c.sb([128, D], F32, "xin") for _ in range(4)]
        self.bxin = [Buf() for _ in range(4)]

    def load_xT_tiles(self, src, t0, t1):
        c, nc = self.c, self.nc
        xin, bxin = self.xin, self.bxin
        for t in range(t0, t1):
            s = t % 4
            c.dma("sp", xin[s][:], src[t * 128:(t + 1) * 128, :], writes=[bxin[s]])
            for h in range(2):
                bank, bb = self.pf[2 + h], self.bpf[2 + h]
                for j in range(4):
                    ch = h * 4 + j
                    c.op("pe", lambda: nc.tensor.transpose(out=bank[:, j * 128:(j + 1) * 128],
                                                           in_=xin[s][:, ch * 128:(ch + 1) * 128],
                                                           identity=self.ident_f[:]),
                         reads=[bxin[s], self.bconst], writes=[bb])
                eng = "act" if h == 0 else "dve"
                dst = self.xT[:, h * 4:(h + 1) * 4, t * 128:(t + 1) * 128]
                src_ps = bank[:].rearrange("p (c n) -> p c n", c=4)
                if eng == "act":
                    c.op("act", lambda: nc.scalar.copy(out=dst, in_=src_ps), reads=[bb], writes=[self.bxT[t]])
                else:
                    c.op("dve", lambda: nc.vector.tensor_copy(out=dst, in_=src_ps), reads=[bb],
                         writes=[self.bxT[t]])

    def ln_store(self, r, br, g_t, b_t, bgb, dst_dram, dst_buf, t, also_T=None):
        c, nc = self.c, self.nc
        self.ln_half_bufs = [Buf("lnh0"), Buf("lnh1")]
        st = self.ln_stats; bst = self.bln
        for h in range(2):
            c.op("dve", lambda: nc.vector.bn_stats(out=st[:, h * 6:(h + 1) * 6], in_=r[:, h * 512:(h + 1) * 512]),
                 reads=[br], writes=[bst])
        mv = self.ln_mv
        c.op("dve", lambda: nc.vector.bn_aggr(out=mv[:, 0:2], in_=st[:, 0:12]), reads=[bst], writes=[bst])
        c.op("dve", lambda: nc.vector.tensor_scalar(out=mv[:, 2:3], in0=mv[:, 1:2], scalar1=LN_EPS, scalar2=None,
                                                    op0=ALU.add), reads=[bst], writes=[bst])
        c.op("act", lambda: nc.scalar.activation(out=mv[:, 3:4], in_=mv[:, 2:3], func=AF.Sqrt),
             reads=[bst], writes=[bst])
        c.op("dve", lambda: nc.vector.reciprocal(out=mv[:, 4:5], in_=mv[:, 3:4]), reads=[bst], writes=[bst])
        c.op("dve", lambda: nc.vector.tensor_scalar(out=r[:], in0=r[:], scalar1=mv[:, 0:1], scalar2=mv[:, 4:5],
                                                    op0=ALU.subtract, op1=ALU.mult), reads=[br, bst], writes=[br])
        c.op("pool", lambda: nc.gpsimd.tensor_tensor(out=r[:], in0=r[:], in1=g_t[:], op=ALU.mult),
             reads=[br, bgb], writes=[br])
        c.op("pool", lambda: nc.gpsimd.tensor_tensor(out=r[:], in0=r[:], in1=b_t[:], op=ALU.add),
             reads=[br, bgb], writes=[br])
        c.dma("pool", dst_dram[t * 128:(t + 1) * 128, :], r[:], reads=[br], writes=[dst_buf])
        if also_T:
            self.x1T_transposes(r, br, t)

    def x1T_transposes(self, r, br, t):
        c, nc = self.c, self.nc
        xt = self.xTt[t % 2]; bxt = self.bxTt[t % 2]
        for h in range(2):
            bank, bb = self.pf[h], self.bpf[h]
            for j in range(4):
                ch = h * 4 + j
                c.op("pe", lambda: nc.tensor.transpose(out=bank[:, j * 128:(j + 1) * 128],
                                                       in_=r[:, ch * 128:(ch + 1) * 128],
                                                       identity=self.ident_f[:]),
                     reads=[br, self.bconst], writes=[bb])
            c.op("act", lambda: nc.scalar.copy(out=xt[:, h * 4:(h + 1) * 4, :],
                                               in_=bank[:].rearrange("p (c n) -> p c n", c=4)),
                 reads=[bb], writes=[bxt])
        q_ = t // 4
        c.dma("pool", self.xTl[q_][:, (t % 4) * 128:(t % 4 + 1) * 128].rearrange("(c p) n -> p c n", p=128),
              xt[:], reads=[bxt], writes=[self.bxTl[q_]])
        if t % 4 == 3:
            self.coll("AllGather", self.xTl[q_], self.xTg[q_], self.bxTl[q_], self.bxTg[q_])
            for r_ in range(2):
                tok0 = r_ * SH + q_ * 512
                c.dma("sp", self.xT[:, :, tok0:tok0 + 512],
                      self.xTg[q_][r_ * D:(r_ + 1) * D, :].rearrange("(c p) n -> p c n", p=128),
                      reads=[self.bxTg[q_]], writes=self.bxT[tok0 // 128:tok0 // 128 + 4])

    def l1_weights(self):
        c, W = self.c, self.W
        wb = c.sb([128, 8, 1552], BF16, "w1"); bwb = Buf("w1")
        self.load_weight_bf16(wb, bwb, W["od_w1"], 0, 1552)
        wo = c.sb([128, 8, D], BF16, "wo1"); bwo = Buf("wo1")
        self.load_weight_bf16(wo, bwo, W["od_w_out"], 0, D)
        return wb, bwb, wo, bwo

    def layer1(self, weights=None):
        c, nc, W = self.c, self.nc, self.W
        pf, bpf, pb, bpb = self.pf, self.bpf, self.pb, self.bpb
        wb, bwb, wo, bwo = weights if weights is not None else self.l1_weights()
        gw2 = c.sb([16, 256], F32, "gw2"); negb = c.sb([128, 2], F32, "negb")
        gng = c.sb([128, 512], F32, "gng")
        bsm = Buf("small1")
        c.dma("sp", gw2[:], W["od_gate_w2"][:, :], writes=[bsm])
        c.dma("sp", negb[:], W["od_gate_b"][:, :], writes=[bsm])
        c.dma("sp", gng[:], W["od_gn_g"][0:1, :].partition_broadcast(128), writes=[bsm])
        c.op("dve", lambda: nc.vector.tensor_scalar(out=negb[:], in0=negb[:], scalar1=-1.0, scalar2=None,
                                                    op0=ALU.mult), reads=[bsm], writes=[bsm])
        Sf = c.sb([128, 2, 256], F32, "Sf"); Sb = c.sb([128, 2, 256], BF16, "Sb")
        bS = [Buf(f"S{h}") for h in range(2)]; bSb = [Buf(f"Sb{h}") for h in range(2)]
        for h in range(2):
            c.op("dve", lambda: nc.vector.memset(Sf[:, h, :], 0.0), writes=[bS[h]])
            c.op("pool", lambda: nc.gpsimd.memset(Sb[:, h, :], 0.0), writes=[bSb[h]])
        glT = c.sb([16, 512], F32, "glT"); bgl = Buf("glT")

        class Slot:
            pass
        slots = []
        e1_ = c.sb([128, 512], F32, "e1"); be1_ = Buf("e1")
        sp_ = e1_; bsp_ = be1_
        bs_ = c.sb([128, 512], F32, "bs"); bbs_ = Buf("bs")
        for si in range(2):
            o = Slot()
            o.e1, o.be1, o.sp, o.bsp, o.bs, o.bbs = e1_, be1_, sp_, bsp_, bs_, bbs_
            o.eb = c.sb([128, 512], F32, "eb"); o.beb = Buf("eb")
            o.enb = c.sb([128, 512], F32, "enb"); o.benb = Buf("enb")
            o.qt = c.sb([128, 512], BF16, "qt"); o.bqt = Buf("qt")
            o.kt = c.sb([128, 512], BF16, "kt"); o.bkt = Buf("kt")
            o.kh = c.sb([128, 512], BF16, "kh"); o.bkh = Buf("kh")
            o.khtok = c.sb([128, 4, 128], BF16, "khtok"); o.bkhtok = Buf("khtok")
            o.vtok = c.sb([128, 4, 256], BF16, "vtok"); o.bvtok = Buf("vtok")
            o.gz = c.sb([128, 4, 256], BF16, "gz"); o.bgz = Buf("gz")
            slots.append(o)
        zs = [c.sb([128, 256], F32, "zs") for _ in range(2)]; bzs = [Buf("zs0"), Buf("zs1")]
        attm = [c.sb([128, 128], BF16, "attm") for _ in range(2)]; battm = [Buf("attm0"), Buf("attm1")]
        _jk = c.sb([128, 256], BF16, "junk"); junk = [_jk, _jk]; _bj = Buf("junk"); bjunk = [_bj, _bj]
        bU = Buf("U")
        stat = c.sb([128, 2, 8], F32, "stat"); bstat = [Buf("stat0"), Buf("stat1")]
        ogt = c.sb([128, 2, 4, 512], BF16, "ogt"); bogt = [[Buf(f"ogt{p_}{j}") for j in range(4)] for p_ in range(2)]
        xT, bxT = self.xT, self.bxT
        dk_scale = 128.0 ** -0.5

        def prep_gen(T, h, o):
            tok = slice(T * 512, (T + 1) * 512)
            bx = bxT[T * 4:(T + 1) * 4]
            if h == 0:
                for ch in range(8):
                    c.op("pe", lambda: nc.tensor.matmul(pf[0][0:16, :], lhsT=wb[:, ch, 1024:1040], rhs=xT[:, ch, tok],
                                                        start=(ch == 0), stop=(ch == 7)),
                         reads=[bwb] + bx, writes=[bpf[0]])
                c.op("act", lambda: nc.scalar.copy(out=glT[:], in_=pf[0][0:16, :]), reads=[bpf[0]], writes=[bgl])
            yield
            c.op("pe", lambda: nc.tensor.matmul(pf[1][:], lhsT=gw2[:, h * 128:(h + 1) * 128], rhs=glT[:],
                                                start=True, stop=True), reads=[bsm, bgl], writes=[bpf[1]])
            c.op("act", lambda: nc.scalar.activation(out=o.e1[:], in_=pf[1][:], func=AF.Exp, scale=-1.0,
                                                     bias=negb[:, h:h + 1]), reads=[bpf[1], bsm], writes=[o.be1])
            c.op("act", lambda: nc.scalar.activation(out=o.sp[:], in_=o.e1[:], func=AF.Ln, bias=1.0, scale=1.0),
                 reads=[o.be1], writes=[o.bsp])
            for j in range(4):
                cs = slice(j * 128, (j + 1) * 128)
                c.op("dve", lambda: nc.vector.tensor_tensor_scan(out=o.bs[:, cs], data0=self.ones_f[:, :],
                                                                 data1=o.sp[:, cs], initial=0.0,
                                                                 op0=ALU.mult, op1=ALU.subtract),
                     reads=[o.bsp, self.bconst], writes=[o.bbs])
            c.op("act", lambda: nc.scalar.activation(out=o.eb[:], in_=o.bs[:], func=AF.Exp, scale=1.0 / 16),
                 reads=[o.bbs], writes=[o.beb])
            c.op("act", lambda: nc.scalar.activation(out=o.enb[:], in_=o.bs[:], func=AF.Exp, scale=-1.0 / 16),
                 reads=[o.bbs], writes=[o.benb])
            yield
            for ch in range(8):
                c.op("pe", lambda: nc.tensor.matmul(pf[0][:], lhsT=wb[:, ch, h * 128:(h + 1) * 128],
                                                    rhs=xT[:, ch, tok], start=(ch == 0), stop=(ch == 7)),
                     reads=[bwb] + bx, writes=[bpf[0]])
            c.op("dve", lambda: nc.vector.scalar_tensor_tensor(out=o.qt[:], in0=pf[0][:], scalar=dk_scale, in1=o.eb[:],
                                                               op0=ALU.mult, op1=ALU.mult),
                 reads=[bpf[0], o.beb], writes=[o.bqt])
            yield
            for ch in range(8):
                c.op("pe", lambda: nc.tensor.matmul(pf[1][:], lhsT=wb[:, ch, 256 + h * 128:256 + (h + 1) * 128],
                                                    rhs=xT[:, ch, tok], start=(ch == 0), stop=(ch == 7)),
                     reads=[bwb] + bx, writes=[bpf[1]])
            c.op("dve", lambda: nc.vector.tensor_tensor(out=o.kt[:], in0=pf[1][:], in1=o.enb[:], op=ALU.mult),
                 reads=[bpf[1], o.benb], writes=[o.bkt])
            for j in range(4):
                cs = slice(j * 128, (j + 1) * 128)
                c.op("dve", lambda: nc.vector.scalar_tensor_tensor(
                    out=o.kh[:, cs], in0=pf[1][:, cs], scalar=o.eb[:, j * 128 + 127:j * 128 + 128], in1=o.enb[:, cs],
                    op0=ALU.mult, op1=ALU.mult), reads=[bpf[1], o.beb, o.benb], writes=[o.bkh])
            yield
            for j in range(4):
                cs = slice(j * 128, (j + 1) * 128)
                c.op("pe", lambda: nc.tensor.transpose(out=pb[0][:, cs], in_=o.kh[:, cs], identity=self.ident_b[:]),
                     reads=[o.bkh, self.bconst], writes=[bpb[0]])
            c.op("act", lambda: nc.scalar.copy(out=o.khtok[:].rearrange("p j d -> p (j d)"), in_=pb[0][:, 0:512]),
                 reads=[bpb[0]], writes=[o.bkhtok])
            for j in range(4):
                yield
                bank = 2 + (j % 2)
                for ch in range(8):
                    c.op("pe", lambda: nc.tensor.matmul(
                        pf[bank][:, 0:256], lhsT=xT[:, ch, T * 512 + j * 128:T * 512 + (j + 1) * 128],
                        rhs=wb[:, ch, 512 + h * 256:512 + (h + 1) * 256], start=(ch == 0), stop=(ch == 7)),
                        reads=[bwb, bx[j]], writes=[bpf[bank]])
                yield
                for ch in range(8):
                    c.op("pe", lambda: nc.tensor.matmul(
                        pf[bank][:, 256:512], lhsT=xT[:, ch, T * 512 + j * 128:T * 512 + (j + 1) * 128],
                        rhs=wb[:, ch, 1040 + h * 256:1040 + (h + 1) * 256], start=(ch == 0), stop=(ch == 7)),
                        reads=[bwb, bx[j]], writes=[bpf[bank]])
                c.op("act", lambda: nc.scalar.copy(out=o.vtok[:, j, :], in_=pf[bank][:, 0:256]), reads=[bpf[bank]],
                     writes=[o.bvtok])
                zi = j % 2
                c.op("act", lambda: nc.scalar.activation(out=zs[zi][:], in_=pf[bank][:, 256:512], func=AF.Silu),
                     reads=[bpf[bank]], writes=[bzs[zi]])
                c.op("pool", lambda: nc.gpsimd.tensor_tensor(out=o.gz[:, j, :], in0=zs[zi][:],
                                                             in1=gng[:, h * 256:(h + 1) * 256], op=ALU.mult),
                     reads=[bzs[zi], bsm], writes=[o.bgz])

        def adv(gen, n):
            if gen is None:
                return
            for _ in range(n):
                try:
                    next(gen)
                except StopIteration:
                    return

        def recur(T, h, o, gen=None):
            par = T % 2
            for j in range(4):
                cs = slice(j * 128, (j + 1) * 128)
                ai = j % 2
                c.op("pe", lambda: nc.tensor.matmul(pf[4][:, 0:128], lhsT=o.kt[:, cs], rhs=o.qt[:, cs],
                                                    start=True, stop=True), reads=[o.bkt, o.bqt], writes=[bpf[4]])
                c.op("dve", lambda: nc.vector.tensor_tensor(out=attm[ai][:], in0=pf[4][:, 0:128], in1=self.tri_b[:],
                                                            op=ALU.mult), reads=[bpf[4], self.bconst], writes=[battm[ai]])
                c.op("pe", lambda: nc.tensor.matmul(pf[4][:, 256:512], lhsT=o.khtok[:, j, :], rhs=o.vtok[:, j, :],
                                                    start=True, stop=True), reads=[o.bkhtok, o.bvtok], writes=[bU])
                adv(gen, 2)
                for _ in range(L1_FILL):
                    c.op("pe", lambda: nc.tensor.matmul(pf[6][:, :], lhsT=self.ident_b[:, :], rhs=wo[:, 0, 0:512],
                                                        start=True, stop=True), reads=[bwo], writes=[])
                c.op("pe", lambda: nc.tensor.matmul(pf[5][:, 0:256], lhsT=attm[ai][:], rhs=o.vtok[:, j, :],
                                                    start=True, stop=False), reads=[battm[ai], o.bvtok], writes=[bpf[5]])
                c.op("pe", lambda: nc.tensor.matmul(pf[5][:, 0:256], lhsT=o.qt[:, cs], rhs=Sb[:, h, :],
                                                    start=False, stop=True), reads=[o.bqt, bSb[h]], writes=[bpf[5]])
                c.op("dve", lambda: nc.vector.scalar_tensor_tensor(
                    out=Sf[:, h, :], in0=Sf[:, h, :], scalar=o.eb[:, j * 128 + 127:j * 128 + 128],
                    in1=pf[4][:, 256:512], op0=ALU.mult, op1=ALU.add), reads=[bS[h], o.beb, bU], writes=[bS[h]])
                c.op("pool", lambda: nc.gpsimd.tensor_copy(out=Sb[:, h, :], in_=Sf[:, h, :]),
                     reads=[bS[h]], writes=[bSb[h]])
                c.op("act", lambda: nc.scalar.activation(out=junk[ai][:], in_=pf[5][:, 0:256], func=AF.Square,
                                                         accum_out=stat[:, ai, 0:1]), reads=[bpf[5]], writes=[bjunk[ai], bstat[ai]])
                c.op("dve", lambda: nc.vector.tensor_scalar(out=stat[:, ai, 1:2], in0=stat[:, ai, 0:1], scalar1=1.0 / 256,
                                                            scalar2=LN_EPS, op0=ALU.mult, op1=ALU.add),
                     reads=[bstat[ai]], writes=[bstat[ai]])
                c.op("act", lambda: nc.scalar.activation(out=stat[:, ai, 2:3], in_=stat[:, ai, 1:2], func=AF.Sqrt),
                     reads=[bstat[ai]], writes=[bstat[ai]])
                c.op("dve", lambda: nc.vector.reciprocal(out=stat[:, ai, 3:4], in_=stat[:, ai, 2:3]),
                     reads=[bstat[ai]], writes=[bstat[ai]])
                c.op("dve", lambda: nc.vector.scalar_tensor_tensor(
                    out=ogt[:, par, j, h * 256:(h + 1) * 256], in0=pf[5][:, 0:256], scalar=stat[:, ai, 3:4],
                    in1=o.gz[:, j, :], op0=ALU.mult, op1=ALU.mult), reads=[bpf[5], bstat[ai], o.bgz], writes=[bogt[par][j]])
                adv(gen, 2)
            adv(gen, 1000)

        def final(T):
            par = T % 2
            c.dma("pool", self.og1l[T // 4][(T % 4) * 512:(T % 4 + 1) * 512, :].rearrange("(s p) n -> p s n", p=128),
                  ogt[:, par, :, :], reads=bogt[par], writes=[self.bog1l[T // 4]])
            if T % 4 == 3:
                self.coll("AllGather", self.og1l[T // 4], self.og1g[T // 4], self.bog1l[T // 4], self.bog1g[T // 4])

        items = [(T, h) for T in range(NST) for h in range(2)]
        adv(prep_gen(items[0][0], items[0][1], slots[0]), 1000)
        for i, (T, h) in enumerate(items):
            if i + 1 < len(items):
                adv(prep_gen(items[i + 1][0], items[i + 1][1], slots[(i + 1) % 2]), 1000)
            recur(T, h, slots[i % 2], None)
            if h == 1:
                final(T)


    def proj_feat(self, bank, bbank, wt, bw, col0, m, T):
        c, nc = self.c, self.nc
        tok = slice(T * 512, (T + 1) * 512)
        for ch in range(8):
            c.op("pe", lambda: nc.tensor.matmul(bank[0:m, :], lhsT=wt[:, ch, col0:col0 + m], rhs=self.xT[:, ch, tok],
                                                start=(ch == 0), stop=(ch == 7)),
                 reads=[bw] + self.bxT[T * 4:(T + 1) * 4], writes=[bbank])

    def proj_tok(self, dst_ap, bbank, wt, bw, col0, n, t):
        c, nc = self.c, self.nc
        for ch in range(8):
            c.op("pe", lambda: nc.tensor.matmul(dst_ap, lhsT=self.xT[:, ch, t * 128:(t + 1) * 128],
                                                rhs=wt[:, ch, col0:col0 + n], start=(ch == 0), stop=(ch == 7)),
                 reads=[bw, self.bxT[t]], writes=[bbank])

    def attn_branch(self, T, ktiles, KAfn, nk_fn, QA_ap, bQA, extra_fn, Vfn, vcols, bK, sub_range_fn, n_fill=None):
        c, nc = self.c, self.nc
        first = {}
        last = {}
        for a in ktiles:
            for s_ in sub_range_fn(a):
                first.setdefault(s_, a)
                last[s_] = a
        pend = None

        def emit_pv(a, PT, bPT, nk):
            for s_ in sub_range_fn(a):
                c.op("pe", lambda: nc.tensor.matmul(self.pf[2 + s_][:, 0:vcols], lhsT=PT[0:nk, s_ * 128:(s_ + 1) * 128],
                                                    rhs=Vfn(a), start=(first[s_] == a), stop=(last[s_] == a)),
                     reads=[bPT, bK], writes=[self.bpf[2 + s_]])

        for a in ktiles:
            i = self.sc_rr
            self.sc_rr ^= 1
            bank, bb = self.pf[i], self.bpf[i]
            nk = nk_fn(a)
            ex = extra_fn(a)
            subs = sub_range_fn(a)
            c0, c1 = min(subs) * 128, (max(subs) + 1) * 128
            c.op("pe", lambda: nc.tensor.matmul(bank[0:nk, c0:c1], lhsT=KAfn(a), rhs=QA_ap[:, c0:c1], start=True,
                                                stop=(len(ex) == 0)), reads=[bK, bQA], writes=[bb])
            for ei, (l_ap, r_ap, bufs) in enumerate(ex):
                c.op("pe", lambda: nc.tensor.matmul(bank[0:nk, c0:c1], lhsT=l_ap, rhs=r_ap[:, c0:c1], start=False,
                                                    stop=(ei == len(ex) - 1)), reads=bufs, writes=[bb])
            PT, bPT = self.PT[i], self.bPT[i]
            c.op("act", lambda: nc.scalar.activation(out=PT[0:nk, c0:c1], in_=bank[0:nk, c0:c1], func=AF.Exp, scale=0.125),
                 reads=[bb], writes=[bPT])
            for _ in range(N_DUMMY if n_fill is None else n_fill):
                c.op("pe", lambda: nc.tensor.matmul(self.pf[6][:, 0:FILL_N], lhsT=self.ident_b[:, :], rhs=self.warm_rhs[:, 0:FILL_N],
                                                    start=True, stop=True), reads=[], writes=[])
            if pend is not None:
                emit_pv(*pend)
            pend = (a, PT, bPT, nk)
        emit_pv(*pend)

    def dbg_dump(self, blk, ap, buf, np_, ncols):
        c, nc = self.c, self.nc
        t = c.sb([128, 1024], F32, "dbgd"); b = Buf("dbgd")
        c.op("pool", lambda: nc.gpsimd.tensor_copy(out=t[0:np_, 0:ncols], in_=ap), reads=[buf], writes=[b])
        bo = Buf("dbgo"); self.out_bufs.append(bo)
        c.dma("sp", self.dbg[blk * 128:blk * 128 + np_, 0:ncols], t[0:np_, 0:ncols], reads=[b], writes=[bo])

    def layer0(self):
        c, nc, W, Cd = self.c, self.nc, self.W, self.Cd
        pf, bpf, pb, bpb = self.pf, self.bpf, self.pb, self.bpb
        xT, bxT = self.xT, self.bxT
        self.sc_rr = 0
        with c.phase():
            cm = c.sb([128, 4, 512], BF16, "cm"); wm = c.sb([128, 4, 512], BF16, "wm")
            bc0 = Buf("c0")
            c.dma("sp", cm[:], Cd["cm"][:, :, :], writes=[bc0])
            c.dma("sp", wm[:], Cd["wm"][:, :, :], writes=[bc0])
            self.warm_rhs = cm[:, 0, :]
            self.PT = [c.sb([128, 512], BF16, "PT") for _ in range(2)]
            self.bPT = [Buf("PT0"), Buf("PT1")]
            QA = c.sb([96, 4, 512], BF16, "QA"); bQA = Buf("QA")
            c.op("dve", lambda: nc.vector.memset(QA[:], 0.0), writes=[bQA])
            zs = c.sb([128, 4, 256], BF16, "zs0"); bzs = Buf("zs0")
            oh = c.sb([128, 4, 256], F32, "oh"); boh = [Buf(f"oh{s_}") for s_ in range(4)]
            og = c.sb([128, 4, 256], BF16, "og0"); bog = Buf("og0")
            st = c.sb([128, 16], F32, "st0"); bsts = [Buf(f"st0_{i}") for i in range(4)]

            def diag_extra(T):
                def f(a):
                    if a >= 4 * T:
                        return [(self.ident_b[:], cm[:, a - 4 * T, :], [self.bconst, bc0])]
                    return []
                return f

            def causal_subs(T):
                return lambda a: [s_ for s_ in range(4) if 4 * T + s_ >= a]

            wpass, bwpass = self.wpass, self.bwpass

            def load_pass_weights(p):
                wt, bw = wpass[p % 2], bwpass[p % 2]
                if p == 0:
                    self.load_weight_bf16(wt, bw, W["ev_wm"], 0, 1024)
                elif p == 1:
                    self.load_weight_bf16(wt, bw, W["ev_wn"], 0, 908)

            load_pass_weights(0)
            for hq in range(1):
                with c.phase():
                    wM, bwM = wpass[hq % 2], bwpass[hq % 2]
                    self.load_xT_alloc()
                    self.load_xT_tiles(self.x, 0, 4)
                    load_pass_weights(hq + 1)
                    KA = c.sb([96, 4, S], BF16, "KAm"); bKA = Buf("KAm")
                    c.op("pool", lambda: nc.gpsimd.memset(KA[64:96, :, :], 0.0), writes=[bKA])
                    Vm = c.sb([128, NT, 4, 65], BF16, "Vm")
                    mobc = c.sb([128, NST, 256], F32, "mobc"); ownc = c.sb([128, NST, 256], F32, "ownc")
                    c.dma("sp", mobc[:], Cd["mobc4"][:, :, :], writes=[bc0])
                    c.dma("sp", ownc[:], Cd["ownc4"][:, :, :], writes=[bc0])
                    for j in range(4):
                        c.dma("sp", KA[64:83, j, :], Cd["kaug_m"][:, :], writes=[bKA])
                    c.op("pool", lambda: nc.gpsimd.memset(Vm[:, :, :, 64:65], 1.0), writes=[bKA])
                    kms = c.sb([64, 4, 16], F32, "kms"); kmT = c.sb([64, 4, 16], BF16, "kmT"); bkm = Buf("km")
                    gm = c.sb([128, 16, 16], F32, "gm"); m8 = c.sb([128, 16, 8], F32, "m8"); b1 = c.sb([128, 16, 16], F32, "b1")
                    bgm = Buf("gm"); bm = [Buf(f"bm{i}") for i in range(16)]
                    btok = c.sb([128, 16, 80], BF16, "btok"); bbtok = Buf("btok")
                    c.op("dve", lambda: nc.vector.memset(btok[:], 0.0), writes=[bbtok])
                    for T in range(NST):
                        tok = slice(T * 512, (T + 1) * 512)
                        if T + 1 < NST:
                            self.load_xT_tiles(self.x, (T + 1) * 4, (T + 2) * 4)
                        for jp in range(2):
                            i = jp % 2
                            self.proj_feat(pf[i], bpf[i], wM, bwM, 256 + jp * 128, 128, T)
                            c.op("act", lambda: nc.scalar.copy(out=KA[0:64, 2 * jp, tok], in_=pf[i][0:64, :]),
                                 reads=[bpf[i]], writes=[bKA])
                            c.op("dve", lambda: nc.vector.tensor_copy(out=KA[0:64, 2 * jp + 1, tok], in_=pf[i][64:128, :]),
                                 reads=[bpf[i]], writes=[bKA])
                        for s_ in range(4):
                            t = T * 4 + s_
                            bank = 2 + (s_ % 2)
                            self.proj_tok(pf[bank][:, 0:256], bpf[bank], wM, bwM, 512, 256, t)
                            c.op("act" if s_ % 2 == 0 else "dve",
                                 (lambda: nc.scalar.copy(out=Vm[:, t, :, 0:64],
                                                         in_=pf[bank][:, 0:256].rearrange("p (h d) -> p h d", h=4)))
                                 if s_ % 2 == 0 else
                                 (lambda: nc.vector.tensor_copy(out=Vm[:, t, :, 0:64],
                                                                in_=pf[bank][:, 0:256].rearrange("p (h d) -> p h d", h=4))),
                                 reads=[bpf[bank]], writes=[bKA])
                    for j in range(4):
                        c.op("dve", lambda: nc.vector.tensor_reduce(
                            out=kms[:, j, :], in_=KA[0:64, j, :].rearrange("p (n m) -> p n m", m=256), axis=AX.X,
                            op=ALU.add), reads=[bKA], writes=[bkm])
                    c.op("dve", lambda: nc.vector.tensor_scalar(out=kmT[:], in0=kms[:], scalar1=1.0 / 256, scalar2=None,
                                                                op0=ALU.mult), reads=[bkm], writes=[bkm])
                    for T in range(NST):
                        tok = slice(T * 512, (T + 1) * 512)
                        for jp in range(2):
                            i = jp % 2
                            self.proj_feat(pf[i], bpf[i], wM, bwM, jp * 128, 128, T)
                            c.op("act", lambda: nc.scalar.copy(out=QA[0:64, 2 * jp, :], in_=pf[i][0:64, :]),
                                 reads=[bpf[i]], writes=[bQA])
                            c.op("dve", lambda: nc.vector.tensor_copy(out=QA[0:64, 2 * jp + 1, :], in_=pf[i][64:128, :]),
                                 reads=[bpf[i]], writes=[bQA])
                        c.dma("sp", QA[80:83, :, :], Cd["qal"][:, 0:4, tok], writes=[bQA])
                        for s_ in range(4):
                            for j in range(4):
                                idx = s_ * 4 + j
                                c.op("pe", lambda: nc.tensor.matmul(pf[2][:, idx * 16:(idx + 1) * 16],
                                                                    lhsT=QA[0:64, j, s_ * 128:(s_ + 1) * 128],
                                                                    rhs=kmT[:, j, :], start=True, stop=True),
                                     reads=[bQA, bkm], writes=[bpf[2]])
                        c.op("dve", lambda: nc.vector.tensor_tensor(out=gm[:].rearrange("p i n -> p (i n)"), in0=pf[2][:, 0:256],
                                                                    in1=mobc[:, T, :], op=ALU.add),
                             reads=[bpf[2], bc0], writes=[bgm])
                        for idx in range(16):
                            c.op("dve", lambda: nc.vector.max(out=m8[:, idx, :], in_=gm[:, idx, :]), reads=[bgm], writes=[bm[idx]])
                            c.op("dve", lambda: nc.vector.tensor_scalar(out=b1[:, idx, :], in0=gm[:, idx, :], scalar1=m8[:, idx, 2:3],
                                                                        scalar2=1.0, op0=ALU.is_ge, op1=ALU.subtract),
                                 reads=[bgm, bm[idx]], writes=[bm[idx]])
                        c.op("dve", lambda: nc.vector.tensor_tensor(out=btok[:, :, 64:80], in0=b1[:],
                                                                    in1=ownc[:, T, :].rearrange("p (i n) -> p i n", n=16),
                                                                    op=ALU.max), reads=bm + [bc0], writes=[bbtok])
                        for s_ in range(4):
                            for j in range(4):
                                idx = s_ * 4 + j
                                slot = (idx % 8) * 128
                                c.op("pe", lambda: nc.tensor.transpose(out=pb[0][0:80, slot:slot + 128], in_=btok[:, idx, :],
                                                                       identity=self.ident_b[:]),
                                     reads=[bbtok, self.bconst], writes=[bpb[0]])
                                c.op("act", lambda: nc.scalar.copy(out=QA[64:80, j, s_ * 128:(s_ + 1) * 128],
                                                                   in_=pb[0][64:80, slot:slot + 128]), reads=[bpb[0]], writes=[bQA])
                        for s_ in range(4):
                            t = T * 4 + s_
                            bank = s_ % 2
                            self.proj_tok(pf[bank][:, 0:256], bpf[bank], wM, bwM, 768, 256, t)
                            c.op("act", lambda: nc.scalar.activation(out=zs[:, s_, :], in_=pf[bank][:, 0:256], func=AF.Silu),
                                 reads=[bpf[bank]], writes=[bzs])
                        for j in range(4):
                            self.attn_branch(T, list(range(4 * T + 4)),
                                             lambda a: KA[0:96, j, a * 128:(a + 1) * 128], lambda a: 128,
                                             QA[0:96, j, :], bQA, diag_extra(T),
                                             lambda a: Vm[:, a, j, :], 65, bKA, causal_subs(T))
                            for s_ in range(4):
                                bst = bsts[s_]; k0 = s_ * 4
                                c.op("dve", lambda: nc.vector.reciprocal(out=st[:, k0:k0 + 1], in_=pf[2 + s_][:, 64:65]),
                                     reads=[bpf[2 + s_]], writes=[bst])
                                c.op("dve", lambda: nc.vector.tensor_scalar(out=oh[:, s_, j * 64:(j + 1) * 64],
                                                                            in0=pf[2 + s_][:, 0:64], scalar1=st[:, k0:k0 + 1],
                                                                            scalar2=None, op0=ALU.mult),
                                     reads=[bpf[2 + s_], bst], writes=[boh[s_]])
                        c.op("pool", lambda: nc.gpsimd.tensor_tensor(out=og[:], in0=oh[:], in1=zs[:], op=ALU.mult),
                             reads=boh + [bzs], writes=[bog])
                        if self.mode == "dbg":
                            self.dbg_dump(0, QA[0:83, 0, :], bQA, 83, 512)
                            self.dbg_dump(1, KA[0:83, 0, 0:512], bKA, 83, 512)
                            self.dbg_dump(2, zs[:, 0, :], bzs, 128, 256)
                            self.dbg_dump(3, oh[:, 0, :], boh[0], 128, 256)
                            self.dbg_dump(4, Vm[:, 0, 0, :], bKA, 128, 65)
                            self.dbg_dump(5, kmT[:, 0, :], bkm, 64, 16)
                            self.dbg_dump(6, self.PT[0][:], self.bPT[0], 128, 512)
                            self.dbg_dump(7, og[:, 0, :], bog, 128, 256)
                            self.dbg_dump(8, pf[2][:, 0:65], bpf[2], 128, 65)
                            raise _Stop()
                        c.dma("pool", self.ogl[T // 4][(T % 4) * 512:(T % 4 + 1) * 512, 0:256].rearrange("(s p) n -> p s n", p=128),
                              og[:], reads=[bog], writes=[self.bogl[T // 4]])

            for g in range(1):
                with c.phase():
                    wN, bwN = wpass[1], bwpass[1]
                    self.load_weight_bf16(wpass[0], bwpass[0], W["ev_w_out"], 0, D)
                    KS = c.sb([96, S], BF16, "KS"); KWn = c.sb([96, S], BF16, "KW"); bKN = Buf("KN")
                    c.op("pool", lambda: nc.gpsimd.memset(KS[64:96, :], 0.0), writes=[bKN])
                    c.op("pool", lambda: nc.gpsimd.memset(KWn[64:96, :], 0.0), writes=[bKN])
                    c.op("dve", lambda: nc.vector.memset(QA[64:96, :, :], 0.0), writes=[bQA])
                    Vs = c.sb([128, NT, 65], BF16, "Vs"); Vw = c.sb([128, NT, 65], BF16, "Vw")
                    c.dma("sp", KS[64:67, :], Cd["kaug_n"][:, :], writes=[bKN])
                    c.dma("sp", KWn[64:67, :], Cd["kaug_n"][:, :], writes=[bKN])
                    c.op("pool", lambda: nc.gpsimd.memset(Vs[:, :, 64:65], 1.0), writes=[bKN])
                    c.op("pool", lambda: nc.gpsimd.memset(Vw[:, :, 64:65], 1.0), writes=[bKN])
                    esel = c.sb([96, S], BF16, "esel"); selc = c.sb([128, NT, 64], F32, "selc")
                    besel = Buf("esel")
                    c.op("pool", lambda: nc.gpsimd.memset(esel[64:96, :], 0.0), writes=[besel])
                    c.dma("sp", esel[0:64, :], Cd["esel"][:, :], writes=[besel])
                    c.dma("sp", selc[:], Cd["selc"][:, :, :], writes=[bc0])
                    KCA = c.sb([96, 256], BF16, "KCA"); VCA = c.sb([128, 2, 129], BF16, "VCA"); bKC = Buf("KC")
                    c.op("dve", lambda: nc.vector.memset(VCA[:], 0.0), writes=[bKC])
                    c.op("dve", lambda: nc.vector.memset(KCA[:], 0.0), writes=[bKC])
                    c.dma("sp", KCA[64:67, 0:255], Cd["kaug_c"][:, :], writes=[bKC])
                    c.dma("sp", VCA[:, :, 65:129], Cd["ovl"][:, :, :], writes=[bKC])
                    c.op("pool", lambda: nc.gpsimd.memset(VCA[:, :, 64:65], 1.0), writes=[bKC])
                    w2b = c.sb([128, 2, 64], BF16, "w2b"); w2f = c.sb([128, 2, 64], F32, "w2f"); bw2 = Buf("w2")
                    c.dma("sp", w2f[:], W["ev_cmp_w2"].rearrange("k h d -> h k d"), writes=[bw2])
                    c.op("dve", lambda: nc.vector.tensor_copy(out=w2b[:], in_=w2f[:]), reads=[bw2], writes=[bw2])
                    b1t = c.sb([128, 2], F32, "b1t")
                    c.dma("sp", b1t[:], W["ev_cmp_b1"][:, :], writes=[bw2])
                    with c.phase():
                        cmpT = c.sb([128, S], BF16, "cmpT"); bcmpT = Buf("cmpT")
                        w1f = c.sb([128, 32, 128], F32, "w1f"); w1b = c.sb([128, 32, 128], BF16, "w1b"); bw1 = Buf("w1")
                        posf = c.sb([128, 32], F32, "posf"); posb = c.sb([128, 32], BF16, "posb")
                        for kv in range(2):
                            c.dma("sp", w1f[kv * 64:(kv + 1) * 64, :, :],
                                  W["ev_cmp_w1"][kv].rearrange("(l d) h -> d l h", d=64), writes=[bw1])
                            c.dma("sp", posf[kv * 64:(kv + 1) * 64, :], W["ev_cmp_pos"][kv], writes=[bw1])
                        c.op("pool", lambda: nc.gpsimd.tensor_copy(out=w1b[:], in_=w1f[:]), reads=[bw1], writes=[bw1])
                        c.op("dve", lambda: nc.vector.tensor_copy(out=posb[:], in_=posf[:]), reads=[bw1], writes=[bw1])
                        for T in range(NST):
                            tok = slice(T * 512, (T + 1) * 512)
                            self.proj_feat(pf[0], bpf[0], wN, bwN, 256, 128, T)
                            c.op("act", lambda: nc.scalar.copy(out=cmpT[:, tok], in_=pf[0][:, :]), reads=[bpf[0]],
                                 writes=[bcmpT])
                            self.proj_feat(pf[1], bpf[1], wN, bwN, 384, 128, T)
                            c.op("dve", lambda: nc.vector.tensor_copy(out=KS[0:64, tok], in_=pf[1][0:64, :]),
                                 reads=[bpf[1]], writes=[bKN])
                            c.op("act", lambda: nc.scalar.copy(out=KWn[0:64, tok], in_=pf[1][64:128, :]), reads=[bpf[1]],
                                 writes=[bKN])
                            for s_ in range(4):
                                t = T * 4 + s_
                                bank = 2 + (s_ % 2)
                                self.proj_tok(pf[bank][:, 0:128], bpf[bank], wN, bwN, 512, 128, t)
                                c.op("dve", lambda: nc.vector.tensor_copy(out=Vs[:, t, 0:64], in_=pf[bank][:, 0:64]),
                                     reads=[bpf[bank]], writes=[bKN])
                                c.op("act", lambda: nc.scalar.copy(out=Vw[:, t, 0:64], in_=pf[bank][:, 64:128]),
                                     reads=[bpf[bank]], writes=[bKN])
                        hidT = c.sb([128, 256], BF16, "hidT"); bhid = Buf("hid")
                        bh = c.sb([128, 2], F32, "bh")
                        for kv in range(2):
                            rows = slice(kv * 64, (kv + 1) * 64)
                            for l in range(32):
                                c.op("pe", lambda: nc.tensor.matmul(pf[0][:, 0:255], lhsT=w1b[rows, l, :],
                                                                    rhs=cmpT[rows, l:l + 16 * 254 + 1:16],
                                                                    start=(l == 0), stop=(l == 31)),
                                     reads=[bw1, bcmpT], writes=[bpf[0]])
                            for l in range(32):
                                c.op("pe", lambda: nc.tensor.matmul(pf[1][:, 0:1], lhsT=w1b[rows, l, :],
                                                                    rhs=posb[rows, l:l + 1], start=(l == 0), stop=(l == 31)),
                                     reads=[bw1], writes=[bpf[1]])
                            c.op("dve", lambda: nc.vector.tensor_tensor(out=bh[:, kv:kv + 1], in0=pf[1][:, 0:1],
                                                                        in1=b1t[:, kv:kv + 1], op=ALU.add),
                                 reads=[bpf[1], bw2], writes=[bhid])
                            c.op("act", lambda: nc.scalar.activation(out=hidT[:, 0:255], in_=pf[0][:, 0:255], func=AF.Silu,
                                                                     bias=bh[:, kv:kv + 1], scale=1.0),
                                 reads=[bpf[0], bhid], writes=[bhid])
                            if kv == 0:
                                c.op("pe", lambda: nc.tensor.matmul(pf[2][0:64, 0:255], lhsT=w2b[:, 0, :], rhs=hidT[:, 0:255],
                                                                    start=True, stop=True), reads=[bw2, bhid], writes=[bpf[2]])
                                c.op("dve", lambda: nc.vector.tensor_copy(out=KCA[0:64, 0:255], in_=pf[2][0:64, 0:255]),
                                     reads=[bpf[2]], writes=[bKC])
                            else:
                                for nt_, nn in enumerate([128, 127]):
                                    c.op("pe", lambda: nc.tensor.matmul(pf[2][0:nn, 0:64], lhsT=hidT[:, nt_ * 128:nt_ * 128 + nn],
                                                                        rhs=w2b[:, 1, :], start=True, stop=True),
                                         reads=[bw2, bhid], writes=[bpf[2]])
                                    c.op("dve", lambda: nc.vector.tensor_copy(out=VCA[0:nn, nt_, 0:64], in_=pf[2][0:nn, 0:64]),
                                         reads=[bpf[2]], writes=[bKC])
                    cmpm = c.sb([128, 2, 512], BF16, "cmpm"); bcmpm = Buf("cmpm")
                    BST = c.sb([96, 512], BF16, "BST"); bBST = Buf("BST")
                    c.op("pool", lambda: nc.gpsimd.memset(BST[64:96, :], 0.0), writes=[bBST])
                    sg = c.sb([128, 4, 12], F32, "sg"); bsg = Buf("sg")
                    imp = c.sb([128, 4, 64], F32, "imp"); bimp = [Buf(f"imp{s_}") for s_ in range(4)]
                    sc = c.sb([128, 64], F32, "sc"); sc2 = c.sb([128, 64], F32, "sc2")
                    m8a = c.sb([128, 8], F32, "m8a"); m8b = c.sb([128, 8], F32, "m8b"); bsc = Buf("sc")
                    bsel = c.sb([128, 64], BF16, "bsel")
                    nkc = [128, 127]
                    for T in range(NST):
                        tok = slice(T * 512, (T + 1) * 512)
                        c.dma("sp", cmpm[:], Cd["cmpm"][:, :, tok], writes=[bcmpm])
                        for jp in range(2):
                            i = jp % 2
                            self.proj_feat(pf[i], bpf[i], wN, bwN, jp * 128, 128, T)
                            c.op("act", lambda: nc.scalar.copy(out=QA[0:64, 2 * jp, :], in_=pf[i][0:64, :]),
                                 reads=[bpf[i]], writes=[bQA])
                            c.op("dve", lambda: nc.vector.tensor_copy(out=QA[0:64, 2 * jp + 1, :], in_=pf[i][64:128, :]),
                                 reads=[bpf[i]], writes=[bQA])
                        c.dma("sp", QA[64:67, :, :], Cd["qal"][:, 0:4, tok], writes=[bQA])
                        for s_ in range(4):
                            t = T * 4 + s_
                            bank = s_ % 2
                            self.proj_tok(pf[bank][:, 0:256], bpf[bank], wN, bwN, 652, 256, t)
                            c.op("act", lambda: nc.scalar.activation(out=zs[:, s_, :], in_=pf[bank][:, 0:256], func=AF.Silu),
                                 reads=[bpf[bank]], writes=[bzs])
                        for s_ in range(4):
                            t = T * 4 + s_
                            self.proj_tok(pf[2][:, s_ * 12:(s_ + 1) * 12], bpf[2], wN, bwN, 640, 12, t)
                        c.op("act", lambda: nc.scalar.activation(out=sg[:].rearrange("p s n -> p (s n)"), in_=pf[2][:, 0:48],
                                                                 func=AF.Sigmoid), reads=[bpf[2]], writes=[bsg])
                        for j in range(4):
                            self.attn_branch(
                                T, [0, 1], lambda a: KCA[0:96, a * 128:a * 128 + nkc[a]], lambda a: nkc[a],
                                QA[0:96, j, :], bQA,
                                lambda a: [(self.ident_b[0:nkc[a], 0:nkc[a]], cmpm[0:nkc[a], a, :], [self.bconst, bcmpm])],
                                lambda a: VCA[0:nkc[a], a, :], 129, bKC, lambda a: [0, 1, 2, 3])
                            for s_ in range(4):
                                acc, bacc = pf[2 + s_], bpf[2 + s_]
                                bst = bsts[s_]; k0 = s_ * 4
                                c.op("dve", lambda: nc.vector.tensor_scalar(out=st[:, k0 + 0:k0 + 1], in0=acc[:, 64:65], scalar1=1e-30,
                                                                            scalar2=None, op0=ALU.max), reads=[bacc], writes=[bst])
                                c.op("dve", lambda: nc.vector.reciprocal(out=st[:, k0 + 1:k0 + 2], in_=st[:, k0 + 0:k0 + 1]), reads=[bst], writes=[bst])
                                c.op("dve", lambda: nc.vector.tensor_tensor(out=st[:, k0 + 2:k0 + 3], in0=st[:, k0 + 1:k0 + 2],
                                                                            in1=sg[:, s_, j * 3:j * 3 + 1], op=ALU.mult),
                                     reads=[bst, bsg], writes=[bst])
                                c.op("dve", lambda: nc.vector.tensor_scalar(out=oh[:, s_, j * 64:(j + 1) * 64], in0=acc[:, 0:64],
                                                                            scalar1=st[:, k0 + 2:k0 + 3], scalar2=None, op0=ALU.mult),
                                     reads=[bacc, bst], writes=[boh[s_]])
                                if j == 0:
                                    c.op("dve", lambda: nc.vector.tensor_scalar(out=imp[:, s_, :], in0=acc[:, 65:129],
                                                                                scalar1=st[:, k0 + 1:k0 + 2], scalar2=None, op0=ALU.mult),
                                         reads=[bacc, bst], writes=[bimp[s_]])
                                else:
                                    c.op("dve", lambda: nc.vector.scalar_tensor_tensor(
                                        out=imp[:, s_, :], in0=acc[:, 65:129], scalar=st[:, k0 + 1:k0 + 2], in1=imp[:, s_, :],
                                        op0=ALU.mult, op1=ALU.add), reads=[bacc, bst, bimp[s_]], writes=[bimp[s_]])
                        for s_ in range(4):
                            t = T * 4 + s_
                            c.op("dve", lambda: nc.vector.tensor_tensor(out=sc[:], in0=imp[:, s_, :], in1=selc[:, t, :], op=ALU.add),
                                 reads=[bimp[s_], bc0], writes=[bsc])
                            c.op("dve", lambda: nc.vector.max(out=m8a[:], in_=sc[:]), reads=[bsc], writes=[bsc])
                            c.op("dve", lambda: nc.vector.match_replace(out=sc2[:], in_to_replace=m8a[:], in_values=sc[:],
                                                                        imm_value=-3e38), reads=[bsc], writes=[bsc])
                            c.op("dve", lambda: nc.vector.max(out=m8b[:], in_=sc2[:]), reads=[bsc], writes=[bsc])
                            c.op("dve", lambda: nc.vector.tensor_scalar(out=bsel[:], in0=sc[:], scalar1=m8b[:, 7:8], scalar2=1.0,
                                                                        op0=ALU.is_ge, op1=ALU.subtract), reads=[bsc], writes=[bsc])
                            c.op("pe", lambda: nc.tensor.transpose(out=pb[0][0:64, s_ * 128:(s_ + 1) * 128], in_=bsel[:],
                                                                   identity=self.ident_b[:]), reads=[bsc, self.bconst],
                                 writes=[bpb[0]])
                        c.op("act", lambda: nc.scalar.copy(out=BST[0:64, :], in_=pb[0][0:64, 0:512]), reads=[bpb[0]], writes=[bBST])
                        for j in range(4):
                            def sel_extra(a):
                                ex = [(esel[:, a * 128:(a + 1) * 128], BST[:], [besel, bBST])]
                                if a >= 4 * T:
                                    ex.append((self.ident_b[:], cm[:, a - 4 * T, :], [self.bconst, bc0]))
                                return ex
                            self.attn_branch(T, list(range(4 * T + 4)), lambda a: KS[0:96, a * 128:(a + 1) * 128],
                                             lambda a: 128, QA[0:96, j, :], bQA, sel_extra,
                                             lambda a: Vs[:, a, :], 65, bKN, causal_subs(T), n_fill=N_FILL_SEL)
                            for br, gi in ((0, 1),):
                                for s_ in range(4):
                                    acc, bacc = pf[2 + s_], bpf[2 + s_]
                                    bst = bsts[s_]; k0 = s_ * 4
                                    c.op("dve", lambda: nc.vector.reciprocal(out=st[:, k0 + 1:k0 + 2], in_=acc[:, 64:65]),
                                         reads=[bacc], writes=[bst])
                                    c.op("dve", lambda: nc.vector.tensor_tensor(out=st[:, k0 + 2:k0 + 3], in0=st[:, k0 + 1:k0 + 2],
                                                                                in1=sg[:, s_, j * 3 + gi:j * 3 + gi + 1],
                                                                                op=ALU.mult), reads=[bst, bsg], writes=[bst])
                                    c.op("dve", lambda: nc.vector.scalar_tensor_tensor(
                                        out=oh[:, s_, j * 64:(j + 1) * 64], in0=acc[:, 0:64], scalar=st[:, k0 + 2:k0 + 3],
                                        in1=oh[:, s_, j * 64:(j + 1) * 64], op0=ALU.mult, op1=ALU.add),
                                        reads=[bacc, bst, boh[s_]], writes=[boh[s_]])

                            def win_extra(a):
                                if a >= 4 * T:
                                    return [(self.ident_b[:], cm[:, a - 4 * T, :], [self.bconst, bc0])]
                                return [(self.ident_b[:], wm[:, a - (4 * T - 4), :], [self.bconst, bc0])]

                            def win_subs(a):
                                return [s_ for s_ in range(4) if 4 * T + s_ - 4 <= a <= 4 * T + s_]
                            self.attn_branch(T, list(range(max(0, 4 * T - 4), 4 * T + 4)),
                                             lambda a: KWn[0:96, a * 128:(a + 1) * 128], lambda a: 128, QA[0:96, j, :], bQA,
                                             win_extra, lambda a: Vw[:, a, :], 65, bKN, win_subs)
                            for s_ in range(4):
                                acc, bacc = pf[2 + s_], bpf[2 + s_]
                                bst = bsts[s_]; k0 = s_ * 4
                                c.op("dve", lambda: nc.vector.reciprocal(out=st[:, k0 + 1:k0 + 2], in_=acc[:, 64:65]),
                                     reads=[bacc], writes=[bst])
                                c.op("dve", lambda: nc.vector.tensor_tensor(out=st[:, k0 + 2:k0 + 3], in0=st[:, k0 + 1:k0 + 2],
                                                                            in1=sg[:, s_, j * 3 + 2:j * 3 + 3],
                                                                            op=ALU.mult), reads=[bst, bsg], writes=[bst])
                                c.op("dve", lambda: nc.vector.scalar_tensor_tensor(
                                    out=oh[:, s_, j * 64:(j + 1) * 64], in0=acc[:, 0:64], scalar=st[:, k0 + 2:k0 + 3],
                                    in1=oh[:, s_, j * 64:(j + 1) * 64], op0=ALU.mult, op1=ALU.add),
                                    reads=[bacc, bst, boh[s_]], writes=[boh[s_]])
                        c.op("pool", lambda: nc.gpsimd.tensor_tensor(out=og[:], in0=oh[:], in1=zs[:], op=ALU.mult),
                             reads=boh + [bzs], writes=[bog])
                        c.dma("pool", self.ogl[T // 4][(T % 4) * 512:(T % 4 + 1) * 512, 256:512].rearrange("(s p) n -> p s n", p=128),
                              og[:], reads=[bog], writes=[self.bogl[T // 4]])
                        if T % 4 == 3:
                            self.coll("AllGather", self.ogl[T // 4], self.ogg[T // 4], self.bogl[T // 4], self.bogg[T // 4])

    def final_pass(self, ogg, bogg, wo_src, res_src, res_bufs, lng_src, lnb_src, dst, dst_bufs, make_xT):
        c, nc = self.c, self.nc
        pf, bpf, pb, bpb = self.pf, self.bpf, self.pb, self.bpb
        NTH = NT // 2
        with c.phase():
            if isinstance(wo_src, tuple):
                wo, bwo = wo_src
            else:
                wo = c.sb([128, 8, D], BF16, "wo0"); bwo = Buf("wo0")
                self.load_weight_bf16(wo, bwo, wo_src, 0, D)
            lng = c.sb([128, D], F32, "lng0"); lnb = c.sb([128, D], F32, "lnb0"); bsm = Buf("small0")
            c.dma("sp", lng[:], lng_src[0:1, :].partition_broadcast(128), writes=[bsm])
            c.dma("sp", lnb[:], lnb_src[0:1, :].partition_broadcast(128), writes=[bsm])
            cand_ = [c.sb([128, 2, 2, 512], BF16, "cand") for _ in range(2)]; bcand_ = [Buf("cand0"), Buf("cand1")]
            ogt_ = [c.sb([128, D], BF16, "ogt0") for _ in range(2)]; bogt_ = [Buf("ogt0a"), Buf("ogt0b")]
            ogT_ = [c.sb([128, 8, 128], BF16, "ogT0") for _ in range(2)]; bogT_ = [Buf("ogT0a"), Buf("ogT0b")]
            res_ = [c.sb([128, D], F32, "res0") for _ in range(2)]; bres_ = [Buf("res0a"), Buf("res0b")]
            rr_ = [c.sb([128, D], F32, "rr0") for _ in range(2)]; brr_ = [Buf("rr0a"), Buf("rr0b")]
            lnst_ = [c.sb([128, 12], F32, "lnst0") for _ in range(2)]; lnmv_ = [c.sb([128, 8], F32, "lnmv0") for _ in range(2)]
            bln_ = [Buf("ln0a"), Buf("ln0b")]
            if make_xT:
                self.xTt = [c.sb([128, 8, 128], BF16, "xTt") for _ in range(2)]; self.bxTt = [Buf("xTt0"), Buf("xTt1")]

            def stage_a_pe(t):
                p_ = t % 2
                cand, bcand = cand_[p_], bcand_[p_]
                ogt, bogt, ogT, bogT = ogt_[p_], bogt_[p_], ogT_[p_], bogT_[p_]
                res, bres = res_[p_], bres_[p_]
                for r_ in range(2):
                    for h_ in range(2):
                        row0 = r_ * SH + t * 128
                        c.dma("sp", cand[:, r_, h_, :], ogg[h_][row0:row0 + 128, :], reads=[bogg[h_]], writes=[bcand])
                c.dma("sp", res[:], res_src[t * 128:(t + 1) * 128, :],
                      reads=([res_bufs[t]] if res_bufs else []), writes=[bres])
                for r_ in range(2):
                    o_ = ogt[:, r_ * 512:(r_ + 1) * 512]
                    c.op("dve", lambda: nc.vector.tensor_scalar(out=o_, in0=cand[:, r_, 0, :], scalar1=self.sel[:, 0:1],
                                                                scalar2=None, op0=ALU.mult),
                         reads=[bcand, self.bconst], writes=[bogt])
                    c.op("dve", lambda: nc.vector.scalar_tensor_tensor(out=o_, in0=cand[:, r_, 1, :], scalar=self.sel[:, 1:2],
                                                                       in1=o_, op0=ALU.mult, op1=ALU.add),
                         reads=[bcand, self.bconst, bogt], writes=[bogt])
                for ch in range(8):
                    c.op("pe", lambda: nc.tensor.transpose(out=pb[1][:, ch * 128:(ch + 1) * 128],
                                                           in_=ogt[:, ch * 128:(ch + 1) * 128], identity=self.ident_b[:]),
                         reads=[bogt, self.bconst], writes=[bpb[1]])
                c.op("act", lambda: nc.scalar.copy(out=ogT[:].rearrange("p c n -> p (c n)"), in_=pb[1][:]),
                     reads=[bpb[1]], writes=[bogT])
                for half in range(2):
                    bk = 4 + half
                    for ch in range(8):
                        c.op("pe", lambda: nc.tensor.matmul(pf[bk][:], lhsT=ogT[:, ch, :],
                                                            rhs=wo[:, ch, half * 512:(half + 1) * 512],
                                                            start=(ch == 0), stop=(ch == 7)), reads=[bogT, bwo], writes=[bpf[bk]])

            def stage_a_dve(t):
                p_ = t % 2
                res, bres, rr, brr = res_[p_], bres_[p_], rr_[p_], brr_[p_]
                for half in range(2):
                    bk = 4 + half
                    c.op("dve", lambda: nc.vector.scalar_tensor_tensor(
                        out=rr[:, half * 512:(half + 1) * 512], in0=res[:, half * 512:(half + 1) * 512], scalar=ALPHA,
                        in1=pf[bk][:], op0=ALU.mult, op1=ALU.add), reads=[bres, bpf[bk]], writes=[brr])

            def stage_bc(t):
                p_ = t % 2
                rr, brr = rr_[p_], brr_[p_]
                self.ln_stats, self.ln_mv, self.bln = lnst_[p_], lnmv_[p_], bln_[p_]
                self.ln_store(rr, brr, lng, lnb, bsm, dst, dst_bufs[t], t, also_T=make_xT)

            stage_a_pe(0)
            stage_a_dve(0)
            for t in range(NTH):
                if t + 1 < NTH:
                    stage_a_pe(t + 1)
                stage_bc(t)
                if t + 1 < NTH:
                    stage_a_dve(t + 1)


_CACHE = {}


def _get_prog(mode):
    if mode not in _CACHE:
        _CACHE[mode] = Prog(mode)
    return _CACHE[mode]


def _in_map(p, inputs, core):
    b, r = core // 2, core % 2
    asc = np.ascontiguousarray
    ar = np.arange

    def f(k):
        return np.asarray(inputs[k], dtype=np.float32)[0]
    x = np.asarray(inputs["x"], dtype=np.float32)[b]
    m = {"x": asc(x), "xh": asc(x[r * SH:(r + 1) * SH])}
    w_in = f("ev_w_in")
    cols_m = np.concatenate([ar(base + r * 256, base + (r + 1) * 256) for base in (1816, 2328, 2840, 3352)])
    m["ev_wm"] = asc(w_in[:, cols_m])
    cols_n = np.concatenate([ar(r * 256, (r + 1) * 256)]
                            + [ar(512 + si * 128 + r * 64, 512 + si * 128 + (r + 1) * 64) for si in (0, 1, 2, 4, 3, 5)]
                            + [ar(1280 + r * 12, 1280 + (r + 1) * 12), ar(1304 + r * 256, 1304 + (r + 1) * 256)])
    m["ev_wn"] = asc(w_in[:, cols_n])
    m["ev_cmp_pos"] = asc(f("ev_cmp_pos").transpose(0, 2, 1))
    m["ev_cmp_w1"] = asc(f("ev_cmp_w1"))
    m["ev_cmp_b1"] = asc(f("ev_cmp_b1").T)
    m["ev_cmp_w2"] = asc(f("ev_cmp_w2"))
    perm = np.concatenate([ar(512, 768), ar(0, 256), ar(768, 1024), ar(256, 512)])
    m["ev_w_out"] = asc(f("ev_w_out")[perm, :])
    m["ev_ln_g"] = asc(f("ev_ln_g").reshape(1, D))
    m["ev_ln_b"] = asc(f("ev_ln_b").reshape(1, D))
    w1 = f("od_w_in")
    cols1 = np.concatenate([ar(r * 256, (r + 1) * 256), ar(512 + r * 256, 512 + (r + 1) * 256),
                            ar(1024 + r * 512, 1024 + (r + 1) * 512), ar(2048, 2064),
                            ar(2064 + r * 512, 2064 + (r + 1) * 512)])
    m["od_w1"] = asc(w1[:, cols1])
    m["od_gate_w2"] = asc(f("od_gate_w2")[:, r * 256:(r + 1) * 256])
    m["od_gate_b"] = asc(f("od_gate_b")[r * 256:(r + 1) * 256].reshape(2, 128).T)
    m["od_gn_g"] = asc(f("od_gn_g")[r * 512:(r + 1) * 512].reshape(1, 512))
    m["od_w_out"] = asc(f("od_w_out"))
    m["od_ln_g"] = asc(f("od_ln_g").reshape(1, D))
    m["od_ln_b"] = asc(f("od_ln_b").reshape(1, D))
    for nm, arr in make_consts(r).items():
        m["c_" + nm] = arr
    return m


def kernel(**inputs):
    inputs = {k: np.asarray(v) for k, v in inputs.items()}
    p = _get_prog("full")
    in_maps = [_in_map(p, inputs, core) for core in range(8)]
    res = run_bass_kernel_spmd(p.nc, in_maps, core_ids=list(range(8)))
    out = np.empty((4, S, D), np.float32)
    for core in range(8):
        b, r = core // 2, core % 2
        out[b, r * SH:(r + 1) * SH] = res.results[core]["out"]
    return out
```

```python
import numpy as np
from contextlib import ExitStack
import concourse.bass as bass
import concourse.mybir as mybir
from concourse.bass_utils import run_bass_kernel_spmd

F32 = mybir.dt.float32
BF16 = mybir.dt.bfloat16
AF = mybir.ActivationFunctionType
ALU = mybir.AluOpType
AX = mybir.AxisListType

S = 4096
D = 1024
NT = S // 128
NST = S // 512
SH = S // 2
ALPHA = float((2.0 * 2) ** 0.25)
LN_EPS = 1e-5
EV_IN = 3864
OD_IN = 3088
NEGM = -30000.0

EPOCH = 30000
STRICT_SAME_ENGINE = False
import os
N_DUMMY = int(os.environ.get('N_DUMMY', '1'))
N_FILL_SEL = int(os.environ.get('N_FILL_SEL', '0'))
L1_FILL = int(os.environ.get('L1_FILL', '0'))
FILL_N = int(os.environ.get('FILL_N', '512'))
NDMA = 24


class Buf:
    __slots__ = ("name", "w", "r")

    def __init__(self, name=""):
        self.name = name
        self.w = []
        self.r = []


class Ctx:
    def __init__(self, nc, stack):
        self.nc = nc
        self.stack = stack
        self.cur = stack
        self.eng = {"pe": nc.tensor, "act": nc.scalar, "dve": nc.vector,
                    "pool": nc.gpsimd, "sp": nc.sync}
        self.sem = {}
        self.cnt = {}
        self.nsem = 0
        for k in self.eng:
            self._new_sem(k)
        self.waited = {}
        self.dma_sems = [[self._alloc(f"dma{i}") for i in range(NDMA)], [self._alloc(f"swdma{i}") for i in range(12)]]
        self.dma_val = [[0] * NDMA, [0] * 12]
        self.dma_rr = [0, 0]
        self.sw_tickets = []
        self.n_ins = 0
        self.n_wait = 0
        self.nname = 0

    def _alloc(self, name):
        self.nsem += 1
        return self.stack.enter_context(self.nc.semaphore(f"{name}_{self.nsem}"))

    def _new_sem(self, k):
        self.sem[k] = self._alloc(f"e_{k}")
        self.cnt[k] = 0

    def sb(self, shape, dt, name=None):
        self.nname += 1
        return self.cur.enter_context(
            self.nc.sbuf_tensor(f"{name or 'sb'}_{self.nname}", list(shape), dt))

    def barrier(self):
        tickets = [(self.sem[k], self.cnt[k], k) for k in self.eng if self.cnt[k] > 0]
        for g_ in range(2):
            tickets += [(self.dma_sems[g_][i], v, "dma") for i, v in enumerate(self.dma_val[g_]) if v > 0]
        tickets += self.sw_tickets
        for e in self.eng:
            self._wait(e, [t for t in tickets if t[2] != e])

    def phase(self):
        ctx = self

        class _Ph:
            def __enter__(self_):
                self_.prev = ctx.cur
                self_.st = ExitStack()
                self_.st.__enter__()
                ctx.cur = self_.st
                return self_

            def __exit__(self_, *a):
                ctx.barrier()
                ctx.cur = self_.prev
                return self_.st.__exit__(*a)
        return _Ph()

    def ps(self, shape, dt, name=None):
        self.nname += 1
        return self.stack.enter_context(
            self.nc.psum_tensor(f"{name or 'ps'}_{self.nname}", list(shape), dt))

    def _wait(self, eng, tickets):
        best = {}
        for t in tickets:
            sem, val, src = t
            key = (eng, id(sem))
            if self.waited.get(key, 0) >= val:
                continue
            if key not in best or best[key][1] < val:
                best[key] = t
        for key, (sem, val, src) in best.items():
            self.eng[eng].wait_ge(sem, val)
            self.waited[key] = val
            self.n_wait += 1

    def _deps(self, eng, reads, writes):
        deps = []
        for b in reads:
            deps.extend(b.w)
        for b in writes:
            for t in b.w:
                if t[2] != eng or (STRICT_SAME_ENGINE and eng not in ("pe", "dma")):
                    deps.append(t)
            for t in b.r:
                if t[2] != eng or (STRICT_SAME_ENGINE and eng != "pe") or eng == "dma":
                    deps.append(t)
        return deps

    def _commit(self, ticket, reads, writes):
        for b in reads:
            b.r.append(ticket)
            if len(b.r) > 64:
                best = {}
                for t in b.r:
                    k = id(t[0])
                    if k not in best or best[k][1] < t[1]:
                        best[k] = t
                b.r = list(best.values())
        for b in writes:
            if ticket[2] == "dma" and b.w and all(t[2] == "dma" for t in b.w):
                b.w = b.w + [ticket]
                if len(b.w) > 32:
                    best = {}
                    for t in b.w:
                        k = id(t[0])
                        if k not in best or best[k][1] < t[1]:
                            best[k] = t
                    b.w = list(best.values())
            else:
                b.w = [ticket]
            b.r = []

    def op(self, eng, fn, reads=(), writes=()):
        self._wait(eng, self._deps(eng, reads, writes))
        ins = fn()
        if self.cnt[eng] >= EPOCH:
            self._new_sem(eng)
        self.cnt[eng] += 1
        ins.then_inc(self.sem[eng], 1)
        ticket = (self.sem[eng], self.cnt[eng], eng)
        self._commit(ticket, reads, writes)
        self.n_ins += 1
        return ticket

    def dma(self, q, out, in_, reads=(), writes=(), **kw):
        grp = 1 if q == "pool" else 0
        i = self.dma_rr[grp]
        self.dma_rr[grp] = (i + 1) % len(self.dma_sems[grp])
        sem = self.dma_sems[grp][i]
        deps = self._deps("dma", reads, writes)
        if self.dma_val[grp][i] > 0:
            deps.append((sem, self.dma_val[grp][i], "dma"))
        self._wait(q, deps)
        ins = self.eng[q].dma_start(out=out, in_=in_, **kw)
        self.dma_val[grp][i] += 16
        ins.then_inc(sem, 16)
        ticket = (sem, self.dma_val[grp][i], "dma")
        self._commit(ticket, reads, writes)
        self.n_ins += 1
        return ticket

    def wait_all(self, eng, bufs):
        deps = []
        for b in bufs:
            deps.extend(b.w)
            deps.extend(b.r)
        self._wait(eng, deps)


def _bf16(a):
    import ml_dtypes
    return np.asarray(a, dtype=np.float32).astype(ml_dtypes.bfloat16)


def make_consts(r=0):
    c = {}
    c["ident_f"] = np.eye(128, dtype=np.float32)
    c["ident_b"] = _bf16(np.eye(128))
    k = np.arange(128)[:, None]
    q = np.arange(128)[None, :]
    c["tri_b"] = _bf16((k <= q).astype(np.float32))
    c["ones_f"] = np.ones((128, 128), np.float32)
    slopes = 2.0 ** (-(np.arange(8) + 1.0))
    tq = np.arange(S, dtype=np.float64)
    qal = np.zeros((3, 8, S), np.float32)
    for h in range(8):
        qal[0, h] = 8.0 * slopes[h]
        qal[1, h] = 1024.0 * slopes[h]
        qal[2, h] = -8.0 * slopes[h] * tq
    c["qal"] = _bf16(qal[:, 4 * r:4 * r + 4, :])
    sel = np.zeros((128, 2), np.float32)
    sel[:, r] = 1.0
    c["sel"] = sel
    kp = np.arange(S)
    kal = np.stack([kp % 128, kp // 128, np.ones(S)]).astype(np.float32)
    c["kaug_n"] = _bf16(kal)
    cp = 16 * np.arange(255) + 31
    c["kaug_c"] = _bf16(np.stack([cp % 128, cp // 128, np.ones(255)]).astype(np.float32))
    e16 = (kp[None, :] // 256 == np.arange(16)[:, None]).astype(np.float32) * 30000.0
    c["kaug_m"] = _bf16(np.concatenate([e16, kal], axis=0))
    c["esel"] = _bf16((kp[None, :] // 64 == np.arange(64)[:, None]).astype(np.float32) * 30000.0)
    kk = np.arange(128)[:, None, None] + 128 * np.arange(4)[None, :, None]
    qq = np.arange(512)[None, None, :]
    c["cm"] = _bf16(np.where(kk <= qq, 0.0, NEGM))
    c["wm"] = _bf16(np.where(kk > qq, 0.0, NEGM))
    n = np.arange(128)[:, None, None] + 128 * np.arange(2)[None, :, None]
    qs = np.arange(S)[None, None, :]
    c["cmpm"] = _bf16(np.where((16 * n + 31 <= qs) & (n < 255), 0.0, NEGM))
    n2 = np.arange(128)[:, None, None] + 128 * np.arange(2)[None, :, None]
    sb = np.arange(64)[None, None, :]
    c["ovl"] = _bf16(((16 * n2 <= 64 * sb + 63) & (16 * n2 + 31 >= 64 * sb) & (n2 < 255)).astype(np.float32))
    tqm = (np.arange(128)[:, None, None] + 128 * np.arange(32)[None, :, None])
    cur = tqm // 64
    forced = (sb == 0) | (sb == cur) | (sb == cur - 1)
    c["selc"] = np.where(sb <= cur, np.where(forced, 1e4, 0.0), -1e30).astype(np.float32)
    nb = np.arange(16)[None, None, :]
    curm = tqm // 256
    mobc = np.where(nb < curm, 0.0, -1e30).astype(np.float32)
    ownc = np.where(nb == curm, 0.0, -1.0).astype(np.float32)
    c["mobc4"] = np.ascontiguousarray(np.broadcast_to(mobc.reshape(128, NST, 4, 1, 16), (128, NST, 4, 4, 16))).reshape(128, NST, 256)
    c["ownc4"] = np.ascontiguousarray(np.broadcast_to(ownc.reshape(128, NST, 4, 1, 16), (128, NST, 4, 4, 16))).reshape(128, NST, 256)
    return c


class _Stop(Exception):
    pass


class Prog:
    def __init__(self, mode="full"):
        self.mode = mode
        self.nc = bass.Bass("TRN2", target_bir_lowering=False)
        self.din = {}
        self.build()

    def dram_in(self, name, shape, dt=F32):
        t = self.nc.dram_tensor(name, list(shape), dt, kind="ExternalInput").ap()
        self.din[name] = t
        return t

    def build(self):
        nc = self.nc
        x = self.dram_in("x", [S, D])
        xh = self.dram_in("xh", [SH, D])
        W = {}
        for nm, shp in [("ev_wm", [D, 1024]), ("ev_wn", [D, 908]), ("ev_cmp_pos", [2, 64, 32]), ("ev_cmp_w1", [2, 2048, 128]),
                        ("ev_cmp_b1", [128, 2]), ("ev_cmp_w2", [2, 128, 64]), ("ev_w_out", [D, D]),
                        ("ev_ln_g", [1, D]), ("ev_ln_b", [1, D]), ("od_w1", [D, 1552]),
                        ("od_gate_w2", [16, 256]), ("od_gate_b", [128, 2]), ("od_gn_g", [1, 512]),
                        ("od_w_out", [D, D]), ("od_ln_g", [1, D]), ("od_ln_b", [1, D])]:
            W[nm] = self.dram_in(nm, shp)
        self.W = W
        C = {}
        for nm, arr in make_consts().items():
            C[nm] = self.dram_in("c_" + nm, list(arr.shape), F32 if arr.dtype == np.float32 else BF16)
        self.Cd = C
        out = nc.dram_tensor("out", [SH, D], F32, kind="ExternalOutput").ap()
        self.x, self.xh, self.out = x, xh, out
        self.x1d = nc.dram_tensor("x1d", [SH, D], F32).ap()
        self.ogl = [nc.dram_tensor(f"ogl{h}", [SH, 512], BF16).ap() for h in range(2)]
        self.ogg = [nc.dram_tensor(f"ogg{h}", [2 * SH, 512], BF16).ap() for h in range(2)]
        self.xTl = [nc.dram_tensor(f"xTl{q}", [D, 512], BF16).ap() for q in range(4)]
        self.xTg = [nc.dram_tensor(f"xTg{q}", [2 * D, 512], BF16).ap() for q in range(4)]
        self.og1l = [nc.dram_tensor(f"og1l{h}", [SH, 512], BF16).ap() for h in range(2)]
        self.og1g = [nc.dram_tensor(f"og1g{h}", [2 * SH, 512], BF16).ap() for h in range(2)]
        self.dbg = None
        self.bx1d = [Buf(f"x1d{t}") for t in range(NT // 2)]
        mk = lambda n: [Buf(n + "0"), Buf(n + "1")]
        self.bogl = mk("ogl"); self.bogg = mk("ogg"); self.bxTl = [Buf(f"xTl{q}") for q in range(4)]; self.bxTg = [Buf(f"xTg{q}") for q in range(4)]
        self.bog1l = mk("og1l"); self.bog1g = mk("og1g")

        with ExitStack() as st:
            c = Ctx(nc, st)
            self.c = c
            self.pf = [c.ps([128, 512], F32, "pf") for _ in range(7)]
            self.bpf = [Buf(f"pf{i}") for i in range(7)]
            pb0 = c.ps([128, 1024], BF16, "pb")
            self.pb = [pb0, pb0]
            b_pb0 = Buf("pb0")
            self.bpb = [b_pb0, b_pb0]
            self.ident_f = c.sb([128, 128], F32); self.ident_b = c.sb([128, 128], BF16)
            self.tri_b = c.sb([128, 128], BF16); self.ones_f = c.sb([128, 128], F32)
            self.sel = c.sb([128, 2], F32)
            self.bconst = Buf("const")
            for t, nm in [(self.ident_f, "ident_f"), (self.ident_b, "ident_b"), (self.tri_b, "tri_b"),
                          (self.ones_f, "ones_f"), (self.sel, "sel")]:
                c.dma("sp", t[:], C[nm][:, :], writes=[self.bconst])
            self.xT = c.sb([128, 8, S], BF16, "xT")
            self.bxT = [Buf(f"xT{t}") for t in range(NT)]
            self.wpass = [c.sb([128, 8, 1024], BF16, "wpA"), c.sb([128, 8, 1024], BF16, "wpB")]
            self.bwpass = [Buf("wpA"), Buf("wpB")]
            self.out_bufs = []
            self.layer0()
            with c.phase():
                l1w = self.l1_weights()
                self.final_pass(self.ogg, self.bogg, (self.wpass[0], self.bwpass[0]), self.xh, None, W["ev_ln_g"], W["ev_ln_b"],
                                self.x1d, self.bx1d, make_xT=True)
                self.reload_xT_quarter(3)
                with c.phase():
                    self.layer1(weights=l1w)
                bout = Buf("out"); self.out_bufs.append(bout)
                self.final_pass(self.og1g, self.bog1g, (l1w[2], l1w[3]), self.x1d, self.bx1d, W["od_ln_g"], W["od_ln_b"],
                                self.out, [bout] * (NT // 2), make_xT=False)
            c.wait_all("sp", self.out_bufs)
            print("instructions", c.n_ins, "waits", c.n_wait, "sems", c.nsem)

    def coll(self, kind, src, dst, bsrc, bdst):
        c, nc = self.c, self.nc
        c._wait("pool", c._deps("dma", [bsrc], [bdst]))
        sem = c._alloc("cc")
        ins = nc.gpsimd.collective_compute(kind, ALU.bypass, replica_groups=[[0, 1], [2, 3], [4, 5], [6, 7]],
                                           ins=[src[:, :]], outs=[dst[:, :]])
        ins.then_inc(sem, 1)
        tk = (sem, 1, "dma")
        c.sw_tickets.append(tk)
        c._commit(tk, [bsrc], [bdst])

    def load_weight_bf16(self, dst, dst_buf, src, col0, ncols):
        c = self.c
        for o in range(0, ncols, 1024):
            n = min(1024, ncols - o)
            c.dma("pool", dst[:, :, o:o + n],
                  src[:, col0 + o:col0 + o + n].rearrange("(c p) n -> p c n", p=128), writes=[dst_buf])

    def load_xT_alloc(self):
        c = self.c
        self.xin = [c.sb([128, D], F32, "xin") for _ in range(4)]
        self.bxin = [Buf() for _ in range(4)]

    def load_xT_tiles(self, src, t0, t1):
        c, nc = self.c, self.nc
        xin, bxin = self.xin, self.bxin
        for t in range(t0, t1):
            s = t % 4
            c.dma("sp", xin[s][:], src[t * 128:(t + 1) * 128, :], writes=[bxin[s]])
            for h in range(2):
                bank, bb = self.pf[2 + h], self.bpf[2 + h]
                for j in range(4):
                    ch = h * 4 + j
                    c.op("pe", lambda: nc.tensor.transpose(out=bank[:, j * 128:(j + 1) * 128],
                                                           in_=xin[s][:, ch * 128:(ch + 1) * 128],
                                                           identity=self.ident_f[:]),
                         reads=[bxin[s], self.bconst], writes=[bb])
                eng = "act" if h == 0 else "dve"
                dst = self.xT[:, h * 4:(h + 1) * 4, t * 128:(t + 1) * 128]
                src_ps = bank[:].rearrange("p (c n) -> p c n", c=4)
                if eng == "act":
                    c.op("act", lambda: nc.scalar.copy(out=dst, in_=src_ps), reads=[bb], writes=[self.bxT[t]])
                else:
                    c.op("dve", lambda: nc.vector.tensor_copy(out=dst, in_=src_ps), reads=[bb],
                         writes=[self.bxT[t]])

    def ln_store(self, r, br, g_t, b_t, bgb, dst_dram, dst_buf, t, also_T=None):
        c, nc = self.c, self.nc
        self.ln_half_bufs = [Buf("lnh0"), Buf("lnh1")]
        st = self.ln_stats; bst = self.bln
        for h in range(2):
            c.op("dve", lambda: nc.vector.bn_stats(out=st[:, h * 6:(h + 1) * 6], in_=r[:, h * 512:(h + 1) * 512]),
                 reads=[br], writes=[bst])
        mv = self.ln_mv
        c.op("dve", lambda: nc.vector.bn_aggr(out=mv[:, 0:2], in_=st[:, 0:12]), reads=[bst], writes=[bst])
        c.op("dve", lambda: nc.vector.tensor_scalar(out=mv[:, 2:3], in0=mv[:, 1:2], scalar1=LN_EPS, scalar2=None,
                                                    op0=ALU.add), reads=[bst], writes=[bst])
        c.op("act", lambda: nc.scalar.activation(out=mv[:, 3:4], in_=mv[:, 2:3], func=AF.Sqrt),
             reads=[bst], writes=[bst])
        c.op("dve", lambda: nc.vector.reciprocal(out=mv[:, 4:5], in_=mv[:, 3:4]), reads=[bst], writes=[bst])
        c.op("dve", lambda: nc.vector.tensor_scalar(out=r[:], in0=r[:], scalar1=mv[:, 0:1], scalar2=mv[:, 4:5],
                                                    op0=ALU.subtract, op1=ALU.mult), reads=[br, bst], writes=[br])
        c.op("pool", lambda: nc.gpsimd.tensor_tensor(out=r[:], in0=r[:], in1=g_t[:], op=ALU.mult),
             reads=[br, bgb], writes=[br])
        c.op("pool", lambda: nc.gpsimd.tensor_tensor(out=r[:], in0=r[:], in1=b_t[:], op=ALU.add),
             reads=[br, bgb], writes=[br])
        c.dma("pool", dst_dram[t * 128:(t + 1) * 128, :], r[:], reads=[br], writes=[dst_buf])
        if also_T:
            self.x1T_transposes(r, br, t)

    def x1T_transposes(self, r, br, t):
        c, nc = self.c, self.nc
        xt = self.xTt[t % 2]; bxt = self.bxTt[t % 2]
        for h in range(2):
            bank, bb = self.pf[h], self.bpf[h]
            for j in range(4):
                ch = h * 4 + j
                c.op("pe", lambda: nc.tensor.transpose(out=bank[:, j * 128:(j + 1) * 128],
                                                       in_=r[:, ch * 128:(ch + 1) * 128],
                                                       identity=self.ident_f[:]),
                     reads=[br, self.bconst], writes=[bb])
            c.op("act", lambda: nc.scalar.copy(out=xt[:, h * 4:(h + 1) * 4, :],
                                               in_=bank[:].rearrange("p (c n) -> p c n", c=4)),
                 reads=[bb], writes=[bxt])
        q_ = t // 4
        c.dma("pool", self.xTl[q_][:, (t % 4) * 128:(t % 4 + 1) * 128].rearrange("(c p) n -> p c n", p=128),
              xt[:], reads=[bxt], writes=[self.bxTl[q_]])
        if t % 4 == 3:
            self.coll("AllGather", self.xTl[q_], self.xTg[q_], self.bxTl[q_], self.bxTg[q_])
            if q_ >= 1:
                self.reload_xT_quarter(q_ - 1)

    def reload_xT_quarter(self, q_):
        c = self.c
        for r_ in range(2):
            tok0 = r_ * SH + q_ * 512
            c.dma("sp", self.xT[:, :, tok0:tok0 + 512],
                  self.xTg[q_][r_ * D:(r_ + 1) * D, :].rearrange("(c p) n -> p c n", p=128),
                  reads=[self.bxTg[q_]], writes=self.bxT[tok0 // 128:tok0 // 128 + 4])

    def l1_weights(self):
        c, W = self.c, self.W
        wb = c.sb([128, 8, 1552], BF16, "w1"); bwb = Buf("w1")
        self.load_weight_bf16(wb, bwb, W["od_w1"], 0, 1552)
        wo = c.sb([128, 8, D], BF16, "wo1"); bwo = Buf("wo1")
        self.load_weight_bf16(wo, bwo, W["od_w_out"], 0, D)
        return wb, bwb, wo, bwo

    def layer1(self, weights=None):
        c, nc, W = self.c, self.nc, self.W
        pf, bpf, pb, bpb = self.pf, self.bpf, self.pb, self.bpb
        wb, bwb, wo, bwo = weights if weights is not None else self.l1_weights()
        gw2 = c.sb([16, 256], F32, "gw2"); negb = c.sb([128, 2], F32, "negb")
        gng = c.sb([128, 512], F32, "gng")
        bsm = Buf("small1")
        c.dma("sp", gw2[:], W["od_gate_w2"][:, :], writes=[bsm])
        c.dma("sp", negb[:], W["od_gate_b"][:, :], writes=[bsm])
        c.dma("sp", gng[:], W["od_gn_g"][0:1, :].partition_broadcast(128), writes=[bsm])
        c.op("dve", lambda: nc.vector.tensor_scalar(out=negb[:], in0=negb[:], scalar1=-1.0, scalar2=None,
                                                    op0=ALU.mult), reads=[bsm], writes=[bsm])
        Sf = c.sb([128, 2, 256], F32, "Sf"); Sb = c.sb([128, 2, 256], BF16, "Sb")
        bS = [Buf(f"S{h}") for h in range(2)]; bSb = [Buf(f"Sb{h}") for h in range(2)]
        for h in range(2):
            c.op("dve", lambda: nc.vector.memset(Sf[:, h, :], 0.0), writes=[bS[h]])
            c.op("pool", lambda: nc.gpsimd.memset(Sb[:, h, :], 0.0), writes=[bSb[h]])
        glT = c.sb([16, 512], F32, "glT"); bgl = Buf("glT")

        class Slot:
            pass
        slots = []
        e1_ = c.sb([128, 512], F32, "e1"); be1_ = Buf("e1")
        sp_ = e1_; bsp_ = be1_
        bs_ = c.sb([128, 512], F32, "bs"); bbs_ = Buf("bs")
        for si in range(2):
            o = Slot()
            o.e1, o.be1, o.sp, o.bsp, o.bs, o.bbs = e1_, be1_, sp_, bsp_, bs_, bbs_
            o.eb = c.sb([128, 512], F32, "eb"); o.beb = Buf("eb")
            o.enb = c.sb([128, 512], F32, "enb"); o.benb = Buf("enb")
            o.qt = c.sb([128, 512], BF16, "qt"); o.bqt = Buf("qt")
            o.kt = c.sb([128, 512], BF16, "kt"); o.bkt = Buf("kt")
            o.kh = c.sb([128, 512], BF16, "kh"); o.bkh = Buf("kh")
            o.khtok = c.sb([128, 4, 128], BF16, "khtok"); o.bkhtok = Buf("khtok")
            o.vtok = c.sb([128, 4, 256], BF16, "vtok"); o.bvtok = Buf("vtok")
            o.gz = c.sb([128, 4, 256], BF16, "gz"); o.bgz = Buf("gz")
            slots.append(o)
        zs = [c.sb([128, 256], F32, "zs") for _ in range(2)]; bzs = [Buf("zs0"), Buf("zs1")]
        attm = [c.sb([128, 128], BF16, "attm") for _ in range(2)]; battm = [Buf("attm0"), Buf("attm1")]
        _jk = c.sb([128, 256], BF16, "junk"); junk = [_jk, _jk]; _bj = Buf("junk"); bjunk = [_bj, _bj]
        bU = Buf("U")
        stat = c.sb([128, 2, 8], F32, "stat"); bstat = [Buf("stat0"), Buf("stat1")]
        ogt = c.sb([128, 2, 4, 512], BF16, "ogt"); bogt = [[Buf(f"ogt{p_}{j}") for j in range(4)] for p_ in range(2)]
        xT, bxT = self.xT, self.bxT
        dk_scale = 128.0 ** -0.5

        def prep_gen(T, h, o):
            tok = slice(T * 512, (T + 1) * 512)
            bx = bxT[T * 4:(T + 1) * 4]
            if h == 0:
                for ch in range(8):
                    c.op("pe", lambda: nc.tensor.matmul(pf[0][0:16, :], lhsT=wb[:, ch, 1024:1040], rhs=xT[:, ch, tok],
                                                        start=(ch == 0), stop=(ch == 7)),
                         reads=[bwb] + bx, writes=[bpf[0]])
                c.op("act", lambda: nc.scalar.copy(out=glT[:], in_=pf[0][0:16, :]), reads=[bpf[0]], writes=[bgl])
            yield
            c.op("pe", lambda: nc.tensor.matmul(pf[1][:], lhsT=gw2[:, h * 128:(h + 1) * 128], rhs=glT[:],
                                                start=True, stop=True), reads=[bsm, bgl], writes=[bpf[1]])
            c.op("act", lambda: nc.scalar.activation(out=o.e1[:], in_=pf[1][:], func=AF.Exp, scale=-1.0,
                                                     bias=negb[:, h:h + 1]), reads=[bpf[1], bsm], writes=[o.be1])
            c.op("act", lambda: nc.scalar.activation(out=o.sp[:], in_=o.e1[:], func=AF.Ln, bias=1.0, scale=1.0),
                 reads=[o.be1], writes=[o.bsp])
            for j in range(4):
                cs = slice(j * 128, (j + 1) * 128)
                c.op("dve", lambda: nc.vector.tensor_tensor_scan(out=o.bs[:, cs], data0=self.ones_f[:, :],
                                                                 data1=o.sp[:, cs], initial=0.0,
                                                                 op0=ALU.mult, op1=ALU.subtract),
                     reads=[o.bsp, self.bconst], writes=[o.bbs])
            c.op("act", lambda: nc.scalar.activation(out=o.eb[:], in_=o.bs[:], func=AF.Exp, scale=1.0 / 16),
                 reads=[o.bbs], writes=[o.beb])
            c.op("act", lambda: nc.scalar.activation(out=o.enb[:], in_=o.bs[:], func=AF.Exp, scale=-1.0 / 16),
                 reads=[o.bbs], writes=[o.benb])
            yield
            for ch in range(8):
                c.op("pe", lambda: nc.tensor.matmul(pf[0][:], lhsT=wb[:, ch, h * 128:(h + 1) * 128],
                                                    rhs=xT[:, ch, tok], start=(ch == 0), stop=(ch == 7)),
                     reads=[bwb] + bx, writes=[bpf[0]])
            c.op("dve", lambda: nc.vector.scalar_tensor_tensor(out=o.qt[:], in0=pf[0][:], scalar=dk_scale, in1=o.eb[:],
                                                               op0=ALU.mult, op1=ALU.mult),
                 reads=[bpf[0], o.beb], writes=[o.bqt])
            yield
            for ch in range(8):
                c.op("pe", lambda: nc.tensor.matmul(pf[1][:], lhsT=wb[:, ch, 256 + h * 128:256 + (h + 1) * 128],
                                                    rhs=xT[:, ch, tok], start=(ch == 0), stop=(ch == 7)),
                     reads=[bwb] + bx, writes=[bpf[1]])
            c.op("dve", lambda: nc.vector.tensor_tensor(out=o.kt[:], in0=pf[1][:], in1=o.enb[:], op=ALU.mult),
                 reads=[bpf[1], o.benb], writes=[o.bkt])
            for j in range(4):
                cs = slice(j * 128, (j + 1) * 128)
                c.op("dve", lambda: nc.vector.scalar_tensor_tensor(
                    out=o.kh[:, cs], in0=pf[1][:, cs], scalar=o.eb[:, j * 128 + 127:j * 128 + 128], in1=o.enb[:, cs],
                    op0=ALU.mult, op1=ALU.mult), reads=[bpf[1], o.beb, o.benb], writes=[o.bkh])
            yield
            for j in range(4):
                cs = slice(j * 128, (j + 1) * 128)
                c.op("pe", lambda: nc.tensor.transpose(out=pb[0][:, cs], in_=o.kh[:, cs], identity=self.ident_b[:]),
                     reads=[o.bkh, self.bconst], writes=[bpb[0]])
            c.op("act", lambda: nc.scalar.copy(out=o.khtok[:].rearrange("p j d -> p (j d)"), in_=pb[0][:, 0:512]),
                 reads=[bpb[0]], writes=[o.bkhtok])
            for j in range(4):
                yield
                bank = 2 + (j % 2)
                for ch in range(8):
                    c.op("pe", lambda: nc.tensor.matmul(
                        pf[bank][:, 0:256], lhsT=xT[:, ch, T * 512 + j * 128:T * 512 + (j + 1) * 128],
                        rhs=wb[:, ch, 512 + h * 256:512 + (h + 1) * 256], start=(ch == 0), stop=(ch == 7)),
                        reads=[bwb, bx[j]], writes=[bpf[bank]])
                yield
                for ch in range(8):
                    c.op("pe", lambda: nc.tensor.matmul(
                        pf[bank][:, 256:512], lhsT=xT[:, ch, T * 512 + j * 128:T * 512 + (j + 1) * 128],
                        rhs=wb[:, ch, 1040 + h * 256:1040 + (h + 1) * 256], start=(ch == 0), stop=(ch == 7)),
                        reads=[bwb, bx[j]], writes=[bpf[bank]])
                c.op("act", lambda: nc.scalar.copy(out=o.vtok[:, j, :], in_=pf[bank][:, 0:256]), reads=[bpf[bank]],
                     writes=[o.bvtok])
                zi = j % 2
                c.op("act", lambda: nc.scalar.activation(out=zs[zi][:], in_=pf[bank][:, 256:512], func=AF.Silu),
                     reads=[bpf[bank]], writes=[bzs[zi]])
                c.op("pool", lambda: nc.gpsimd.tensor_tensor(out=o.gz[:, j, :], in0=zs[zi][:],
                                                             in1=gng[:, h * 256:(h + 1) * 256], op=ALU.mult),
                     reads=[bzs[zi], bsm], writes=[o.bgz])

        def adv(gen, n):
            if gen is None:
                return
            for _ in range(n):
                try:
                    next(gen)
                except StopIteration:
                    return

        def recur(T, h, o, gen=None):
            par = T % 2
            for j in range(4):
                cs = slice(j * 128, (j + 1) * 128)
                ai = j % 2
                c.op("pe", lambda: nc.tensor.matmul(pf[4][:, 0:128], lhsT=o.kt[:, cs], rhs=o.qt[:, cs],
                                                    start=True, stop=True), reads=[o.bkt, o.bqt], writes=[bpf[4]])
                c.op("dve", lambda: nc.vector.tensor_tensor(out=attm[ai][:], in0=pf[4][:, 0:128], in1=self.tri_b[:],
                                                            op=ALU.mult), reads=[bpf[4], self.bconst], writes=[battm[ai]])
                c.op("pe", lambda: nc.tensor.matmul(pf[4][:, 256:512], lhsT=o.khtok[:, j, :], rhs=o.vtok[:, j, :],
                                                    start=True, stop=True), reads=[o.bkhtok, o.bvtok], writes=[bU])
                adv(gen, 2)
                for _ in range(L1_FILL):
                    c.op("pe", lambda: nc.tensor.matmul(pf[6][:, :], lhsT=self.ident_b[:, :], rhs=wo[:, 0, 0:512],
                                                        start=True, stop=True), reads=[bwo], writes=[])
                c.op("pe", lambda: nc.tensor.matmul(pf[5][:, 0:256], lhsT=attm[ai][:], rhs=o.vtok[:, j, :],
                                                    start=True, stop=False), reads=[battm[ai], o.bvtok], writes=[bpf[5]])
                c.op("pe", lambda: nc.tensor.matmul(pf[5][:, 0:256], lhsT=o.qt[:, cs], rhs=Sb[:, h, :],
                                                    start=False, stop=True), reads=[o.bqt, bSb[h]], writes=[bpf[5]])
                c.op("dve", lambda: nc.vector.scalar_tensor_tensor(
                    out=Sf[:, h, :], in0=Sf[:, h, :], scalar=o.eb[:, j * 128 + 127:j * 128 + 128],
                    in1=pf[4][:, 256:512], op0=ALU.mult, op1=ALU.add), reads=[bS[h], o.beb, bU], writes=[bS[h]])
                c.op("pool", lambda: nc.gpsimd.tensor_copy(out=Sb[:, h, :], in_=Sf[:, h, :]),
                     reads=[bS[h]], writes=[bSb[h]])
                c.op("act", lambda: nc.scalar.activation(out=junk[ai][:], in_=pf[5][:, 0:256], func=AF.Square,
                                                         accum_out=stat[:, ai, 0:1]), reads=[bpf[5]], writes=[bjunk[ai], bstat[ai]])
                c.op("dve", lambda: nc.vector.tensor_scalar(out=stat[:, ai, 1:2], in0=stat[:, ai, 0:1], scalar1=1.0 / 256,
                                                            scalar2=LN_EPS, op0=ALU.mult, op1=ALU.add),
                     reads=[bstat[ai]], writes=[bstat[ai]])
                c.op("act", lambda: nc.scalar.activation(out=stat[:, ai, 2:3], in_=stat[:, ai, 1:2], func=AF.Sqrt),
                     reads=[bstat[ai]], writes=[bstat[ai]])
                c.op("dve", lambda: nc.vector.reciprocal(out=stat[:, ai, 3:4], in_=stat[:, ai, 2:3]),
                     reads=[bstat[ai]], writes=[bstat[ai]])
                c.op("dve", lambda: nc.vector.scalar_tensor_tensor(
                    out=ogt[:, par, j, h * 256:(h + 1) * 256], in0=pf[5][:, 0:256], scalar=stat[:, ai, 3:4],
                    in1=o.gz[:, j, :], op0=ALU.mult, op1=ALU.mult), reads=[bpf[5], bstat[ai], o.bgz], writes=[bogt[par][j]])
                adv(gen, 2)
            adv(gen, 1000)

        def final(T):
            par = T % 2
            c.dma("pool", self.og1l[T // 4][(T % 4) * 512:(T % 4 + 1) * 512, :].rearrange("(s p) n -> p s n", p=128),
                  ogt[:, par, :, :], reads=bogt[par], writes=[self.bog1l[T // 4]])
            if T % 4 == 3:
                self.coll("AllGather", self.og1l[T // 4], self.og1g[T // 4], self.bog1l[T // 4], self.bog1g[T // 4])

        items = [(T, h) for T in range(NST) for h in range(2)]
        adv(prep_gen(items[0][0], items[0][1], slots[0]), 1000)
        for i, (T, h) in enumerate(items):
            if i + 1 < len(items):
                adv(prep_gen(items[i + 1][0], items[i + 1][1], slots[(i + 1) % 2]), 1000)
            recur(T, h, slots[i % 2], None)
            if h == 1:
                final(T)


    def proj_feat(self, bank, bbank, wt, bw, col0, m, T):
        c, nc = self.c, self.nc
        tok = slice(T * 512, (T + 1) * 512)
        for ch in range(8):
            c.op("pe", lambda: nc.tensor.matmul(bank[0:m, :], lhsT=wt[:, ch, col0:col0 + m], rhs=self.xT[:, ch, tok],
                                                start=(ch == 0), stop=(ch == 7)),
                 reads=[bw] + self.bxT[T * 4:(T + 1) * 4], writes=[bbank])

    def proj_tok(self, dst_ap, bbank, wt, bw, col0, n, t):
        c, nc = self.c, self.nc
        for ch in range(8):
            c.op("pe", lambda: nc.tensor.matmul(dst_ap, lhsT=self.xT[:, ch, t * 128:(t + 1) * 128],
                                                rhs=wt[:, ch, col0:col0 + n], start=(ch == 0), stop=(ch == 7)),
                 reads=[bw, self.bxT[t]], writes=[bbank])

    def attn_branch(self, T, ktiles, KAfn, nk_fn, QA_ap, bQA, extra_fn, Vfn, vcols, bK, sub_range_fn, n_fill=None):
        c, nc = self.c, self.nc
        first = {}
        last = {}
        for a in ktiles:
            for s_ in sub_range_fn(a):
                first.setdefault(s_, a)
                last[s_] = a
        pend = None

        def emit_pv(a, PT, bPT, nk):
            for s_ in sub_range_fn(a):
                c.op("pe", lambda: nc.tensor.matmul(self.pf[2 + s_][:, 0:vcols], lhsT=PT[0:nk, s_ * 128:(s_ + 1) * 128],
                                                    rhs=Vfn(a), start=(first[s_] == a), stop=(last[s_] == a)),
                     reads=[bPT, bK], writes=[self.bpf[2 + s_]])

        for a in ktiles:
            i = self.sc_rr
            self.sc_rr ^= 1
            bank, bb = self.pf[i], self.bpf[i]
            nk = nk_fn(a)
            ex = extra_fn(a)
            subs = sub_range_fn(a)
            c0, c1 = min(subs) * 128, (max(subs) + 1) * 128
            c.op("pe", lambda: nc.tensor.matmul(bank[0:nk, c0:c1], lhsT=KAfn(a), rhs=QA_ap[:, c0:c1], start=True,
                                                stop=(len(ex) == 0)), reads=[bK, bQA], writes=[bb])
            for ei, (l_ap, r_ap, bufs) in enumerate(ex):
                c.op("pe", lambda: nc.tensor.matmul(bank[0:nk, c0:c1], lhsT=l_ap, rhs=r_ap[:, c0:c1], start=False,
                                                    stop=(ei == len(ex) - 1)), reads=bufs, writes=[bb])
            PT, bPT = self.PT[i], self.bPT[i]
            c.op("act", lambda: nc.scalar.activation(out=PT[0:nk, c0:c1], in_=bank[0:nk, c0:c1], func=AF.Exp, scale=0.125),
                 reads=[bb], writes=[bPT])
            for _ in range(N_DUMMY if n_fill is None else n_fill):
                c.op("pe", lambda: nc.tensor.matmul(self.pf[6][:, 0:FILL_N], lhsT=self.ident_b[:, :], rhs=self.warm_rhs[:, 0:FILL_N],
                                                    start=True, stop=True), reads=[], writes=[])
            if pend is not None:
                emit_pv(*pend)
            pend = (a, PT, bPT, nk)
        emit_pv(*pend)

    def dbg_dump(self, blk, ap, buf, np_, ncols):
        c, nc = self.c, self.nc
        t = c.sb([128, 1024], F32, "dbgd"); b = Buf("dbgd")
        c.op("pool", lambda: nc.gpsimd.tensor_copy(out=t[0:np_, 0:ncols], in_=ap), reads=[buf], writes=[b])
        bo = Buf("dbgo"); self.out_bufs.append(bo)
        c.dma("sp", self.dbg[blk * 128:blk * 128 + np_, 0:ncols], t[0:np_, 0:ncols], reads=[b], writes=[bo])

    def layer0(self):
        c, nc, W, Cd = self.c, self.nc, self.W, self.Cd
        pf, bpf, pb, bpb = self.pf, self.bpf, self.pb, self.bpb
        xT, bxT = self.xT, self.bxT
        self.sc_rr = 0
        with c.phase():
            cm = c.sb([128, 4, 512], BF16, "cm"); wm = c.sb([128, 4, 512], BF16, "wm")
            bc0 = Buf("c0")
            c.dma("sp", cm[:], Cd["cm"][:, :, :], writes=[bc0])
            c.dma("sp", wm[:], Cd["wm"][:, :, :], writes=[bc0])
            self.warm_rhs = cm[:, 0, :]
            self.PT = [c.sb([128, 512], BF16, "PT") for _ in range(2)]
            self.bPT = [Buf("PT0"), Buf("PT1")]
            QA = c.sb([96, 4, 512], BF16, "QA"); bQA = Buf("QA")
            c.op("dve", lambda: nc.vector.memset(QA[:], 0.0), writes=[bQA])
            zs = c.sb([128, 4, 256], BF16, "zs0"); bzs = Buf("zs0")
            oh = c.sb([128, 4, 256], F32, "oh"); boh = [Buf(f"oh{s_}") for s_ in range(4)]
            og = c.sb([128, 4, 256], BF16, "og0"); bog = Buf("og0")
            st = c.sb([128, 16], F32, "st0"); bsts = [Buf(f"st0_{i}") for i in range(4)]

            def diag_extra(T):
                def f(a):
                    if a >= 4 * T:
                        return [(self.ident_b[:], cm[:, a - 4 * T, :], [self.bconst, bc0])]
                    return []
                return f

            def causal_subs(T):
                return lambda a: [s_ for s_ in range(4) if 4 * T + s_ >= a]

            wpass, bwpass = self.wpass, self.bwpass

            def load_pass_weights(p):
                wt, bw = wpass[p % 2], bwpass[p % 2]
                if p == 0:
                    self.load_weight_bf16(wt, bw, W["ev_wm"], 0, 1024)
                elif p == 1:
                    self.load_weight_bf16(wt, bw, W["ev_wn"], 0, 908)

            load_pass_weights(0)
            for hq in range(1):
                with c.phase():
                    wM, bwM = wpass[hq % 2], bwpass[hq % 2]
                    self.load_xT_alloc()
                    self.load_xT_tiles(self.x, 0, 4)
                    load_pass_weights(hq + 1)
                    KA = c.sb([96, 4, S], BF16, "KAm"); bKA = Buf("KAm")
                    c.op("pool", lambda: nc.gpsimd.memset(KA[64:96, :, :], 0.0), writes=[bKA])
                    Vm = c.sb([128, NT, 4, 65], BF16, "Vm")
                    mobc = c.sb([128, NST, 256], F32, "mobc"); ownc = c.sb([128, NST, 256], F32, "ownc")
                    c.dma("sp", mobc[:], Cd["mobc4"][:, :, :], writes=[bc0])
                    c.dma("sp", ownc[:], Cd["ownc4"][:, :, :], writes=[bc0])
                    for j in range(4):
                        c.dma("sp", KA[64:83, j, :], Cd["kaug_m"][:, :], writes=[bKA])
                    c.op("pool", lambda: nc.gpsimd.memset(Vm[:, :, :, 64:65], 1.0), writes=[bKA])
                    kms = c.sb([64, 4, 16], F32, "kms"); kmT = c.sb([64, 4, 16], BF16, "kmT"); bkm = Buf("km")
                    gm = c.sb([128, 16, 16], F32, "gm"); m8 = c.sb([128, 16, 8], F32, "m8"); b1 = c.sb([128, 16, 16], F32, "b1")
                    bgm = Buf("gm"); bm = [Buf(f"bm{i}") for i in range(16)]
                    btok = c.sb([128, 16, 80], BF16, "btok"); bbtok = Buf("btok")
                    c.op("dve", lambda: nc.vector.memset(btok[:], 0.0), writes=[bbtok])
                    for T in range(NST):
                        tok = slice(T * 512, (T + 1) * 512)
                        if T + 1 < NST:
                            self.load_xT_tiles(self.x, (T + 1) * 4, (T + 2) * 4)
                        for jp in range(2):
                            i = jp % 2
                            self.proj_feat(pf[i], bpf[i], wM, bwM, 256 + jp * 128, 128, T)
                            c.op("act", lambda: nc.scalar.copy(out=KA[0:64, 2 * jp, tok], in_=pf[i][0:64, :]),
                                 reads=[bpf[i]], writes=[bKA])
                            c.op("dve", lambda: nc.vector.tensor_copy(out=KA[0:64, 2 * jp + 1, tok], in_=pf[i][64:128, :]),
                                 reads=[bpf[i]], writes=[bKA])
                        for s_ in range(4):
                            t = T * 4 + s_
                            bank = 2 + (s_ % 2)
                            self.proj_tok(pf[bank][:, 0:256], bpf[bank], wM, bwM, 512, 256, t)
                            c.op("act" if s_ % 2 == 0 else "dve",
                                 (lambda: nc.scalar.copy(out=Vm[:, t, :, 0:64],
                                                         in_=pf[bank][:, 0:256].rearrange("p (h d) -> p h d", h=4)))
                                 if s_ % 2 == 0 else
                                 (lambda: nc.vector.tensor_copy(out=Vm[:, t, :, 0:64],
                                                                in_=pf[bank][:, 0:256].rearrange("p (h d) -> p h d", h=4))),
                                 reads=[bpf[bank]], writes=[bKA])
                    for j in range(4):
                        c.op("dve", lambda: nc.vector.tensor_reduce(
                            out=kms[:, j, :], in_=KA[0:64, j, :].rearrange("p (n m) -> p n m", m=256), axis=AX.X,
                            op=ALU.add), reads=[bKA], writes=[bkm])
                    c.op("dve", lambda: nc.vector.tensor_scalar(out=kmT[:], in0=kms[:], scalar1=1.0 / 256, scalar2=None,
                                                                op0=ALU.mult), reads=[bkm], writes=[bkm])
                    for T in range(NST):
                        tok = slice(T * 512, (T + 1) * 512)
                        for jp in range(2):
                            i = jp % 2
                            self.proj_feat(pf[i], bpf[i], wM, bwM, jp * 128, 128, T)
                            c.op("act", lambda: nc.scalar.copy(out=QA[0:64, 2 * jp, :], in_=pf[i][0:64, :]),
                                 reads=[bpf[i]], writes=[bQA])
                            c.op("dve", lambda: nc.vector.tensor_copy(out=QA[0:64, 2 * jp + 1, :], in_=pf[i][64:128, :]),
                                 reads=[bpf[i]], writes=[bQA])
                        c.dma("sp", QA[80:83, :, :], Cd["qal"][:, 0:4, tok], writes=[bQA])
                        for s_ in range(4):
                            for j in range(4):
                                idx = s_ * 4 + j
                                c.op("pe", lambda: nc.tensor.matmul(pf[2][:, idx * 16:(idx + 1) * 16],
                                                                    lhsT=QA[0:64, j, s_ * 128:(s_ + 1) * 128],
                                                                    rhs=kmT[:, j, :], start=True, stop=True),
                                     reads=[bQA, bkm], writes=[bpf[2]])
                        c.op("dve", lambda: nc.vector.tensor_tensor(out=gm[:].rearrange("p i n -> p (i n)"), in0=pf[2][:, 0:256],
                                                                    in1=mobc[:, T, :], op=ALU.add),
                             reads=[bpf[2], bc0], writes=[bgm])
                        for idx in range(16):
                            c.op("dve", lambda: nc.vector.max(out=m8[:, idx, :], in_=gm[:, idx, :]), reads=[bgm], writes=[bm[idx]])
                            c.op("dve", lambda: nc.vector.tensor_scalar(out=b1[:, idx, :], in0=gm[:, idx, :], scalar1=m8[:, idx, 2:3],
                                                                        scalar2=1.0, op0=ALU.is_ge, op1=ALU.subtract),
                                 reads=[bgm, bm[idx]], writes=[bm[idx]])
                        c.op("dve", lambda: nc.vector.tensor_tensor(out=btok[:, :, 64:80], in0=b1[:],
                                                                    in1=ownc[:, T, :].rearrange("p (i n) -> p i n", n=16),
                                                                    op=ALU.max), reads=bm + [bc0], writes=[bbtok])
                        for s_ in range(4):
                            for j in range(4):
                                idx = s_ * 4 + j
                                slot = (idx % 8) * 128
                                c.op("pe", lambda: nc.tensor.transpose(out=pb[0][0:80, slot:slot + 128], in_=btok[:, idx, :],
                                                                       identity=self.ident_b[:]),
                                     reads=[bbtok, self.bconst], writes=[bpb[0]])
                                c.op("act", lambda: nc.scalar.copy(out=QA[64:80, j, s_ * 128:(s_ + 1) * 128],
                                                                   in_=pb[0][64:80, slot:slot + 128]), reads=[bpb[0]], writes=[bQA])
                        for s_ in range(4):
                            t = T * 4 + s_
                            bank = s_ % 2
                            self.proj_tok(pf[bank][:, 0:256], bpf[bank], wM, bwM, 768, 256, t)
                            c.op("act", lambda: nc.scalar.activation(out=zs[:, s_, :], in_=pf[bank][:, 0:256], func=AF.Silu),
                                 reads=[bpf[bank]], writes=[bzs])
                        for j in range(4):
                            self.attn_branch(T, list(range(4 * T + 4)),
                                             lambda a: KA[0:96, j, a * 128:(a + 1) * 128], lambda a: 128,
                                             QA[0:96, j, :], bQA, diag_extra(T),
                                             lambda a: Vm[:, a, j, :], 65, bKA, causal_subs(T))
                            for s_ in range(4):
                                bst = bsts[s_]; k0 = s_ * 4
                                c.op("dve", lambda: nc.vector.reciprocal(out=st[:, k0:k0 + 1], in_=pf[2 + s_][:, 64:65]),
                                     reads=[bpf[2 + s_]], writes=[bst])
                                c.op("dve", lambda: nc.vector.tensor_scalar(out=oh[:, s_, j * 64:(j + 1) * 64],
                                                                            in0=pf[2 + s_][:, 0:64], scalar1=st[:, k0:k0 + 1],
                                                                            scalar2=None, op0=ALU.mult),
                                     reads=[bpf[2 + s_], bst], writes=[boh[s_]])
                        c.op("pool", lambda: nc.gpsimd.tensor_tensor(out=og[:], in0=oh[:], in1=zs[:], op=ALU.mult),
                             reads=boh + [bzs], writes=[bog])
                        if self.mode == "dbg":
                            self.dbg_dump(0, QA[0:83, 0, :], bQA, 83, 512)
                            self.dbg_dump(1, KA[0:83, 0, 0:512], bKA, 83, 512)
                            self.dbg_dump(2, zs[:, 0, :], bzs, 128, 256)
                            self.dbg_dump(3, oh[:, 0, :], boh[0], 128, 256)
                            self.dbg_dump(4, Vm[:, 0, 0, :], bKA, 128, 65)
                            self.dbg_dump(5, kmT[:, 0, :], bkm, 64, 16)
                            self.dbg_dump(6, self.PT[0][:], self.bPT[0], 128, 512)
                            self.dbg_dump(7, og[:, 0, :], bog, 128, 256)
                            self.dbg_dump(8, pf[2][:, 0:65], bpf[2], 128, 65)
                            raise _Stop()
                        c.dma("pool", self.ogl[T // 4][(T % 4) * 512:(T % 4 + 1) * 512, 0:256].rearrange("(s p) n -> p s n", p=128),
                              og[:], reads=[bog], writes=[self.bogl[T // 4]])

            for g in range(1):
                with c.phase():
                    wN, bwN = wpass[1], bwpass[1]
                    self.load_weight_bf16(wpass[0], bwpass[0], W["ev_w_out"], 0, D)
                    KS = c.sb([96, S], BF16, "KS"); KWn = c.sb([96, S], BF16, "KW"); bKN = Buf("KN")
                    c.op("pool", lambda: nc.gpsimd.memset(KS[64:96, :], 0.0), writes=[bKN])
                    c.op("pool", lambda: nc.gpsimd.memset(KWn[64:96, :], 0.0), writes=[bKN])
                    c.op("dve", lambda: nc.vector.memset(QA[64:96, :, :], 0.0), writes=[bQA])
                    Vs = c.sb([128, NT, 65], BF16, "Vs"); Vw = c.sb([128, NT, 65], BF16, "Vw")
                    c.dma("sp", KS[64:67, :], Cd["kaug_n"][:, :], writes=[bKN])
                    c.dma("sp", KWn[64:67, :], Cd["kaug_n"][:, :], writes=[bKN])
                    c.op("pool", lambda: nc.gpsimd.memset(Vs[:, :, 64:65], 1.0), writes=[bKN])
                    c.op("pool", lambda: nc.gpsimd.memset(Vw[:, :, 64:65], 1.0), writes=[bKN])
                    esel = c.sb([96, S], BF16, "esel"); selc = c.sb([128, NT, 64], F32, "selc")
                    besel = Buf("esel")
                    c.op("pool", lambda: nc.gpsimd.memset(esel[64:96, :], 0.0), writes=[besel])
                    c.dma("sp", esel[0:64, :], Cd["esel"][:, :], writes=[besel])
                    c.dma("sp", selc[:], Cd["selc"][:, :, :], writes=[bc0])
                    KCA = c.sb([96, 256], BF16, "KCA"); VCA = c.sb([128, 2, 129], BF16, "VCA"); bKC = Buf("KC")
                    c.op("dve", lambda: nc.vector.memset(VCA[:], 0.0), writes=[bKC])
                    c.op("dve", lambda: nc.vector.memset(KCA[:], 0.0), writes=[bKC])
                    c.dma("sp", KCA[64:67, 0:255], Cd["kaug_c"][:, :], writes=[bKC])
                    c.dma("sp", VCA[:, :, 65:129], Cd["ovl"][:, :, :], writes=[bKC])
                    c.op("pool", lambda: nc.gpsimd.memset(VCA[:, :, 64:65], 1.0), writes=[bKC])
                    w2b = c.sb([128, 2, 64], BF16, "w2b"); w2f = c.sb([128, 2, 64], F32, "w2f"); bw2 = Buf("w2")
                    c.dma("sp", w2f[:], W["ev_cmp_w2"].rearrange("k h d -> h k d"), writes=[bw2])
                    c.op("dve", lambda: nc.vector.tensor_copy(out=w2b[:], in_=w2f[:]), reads=[bw2], writes=[bw2])
                    b1t = c.sb([128, 2], F32, "b1t")
                    c.dma("sp", b1t[:], W["ev_cmp_b1"][:, :], writes=[bw2])
                    with c.phase():
                        cmpT = c.sb([128, S], BF16, "cmpT"); bcmpT = Buf("cmpT")
                        w1f = c.sb([128, 32, 128], F32, "w1f"); w1b = c.sb([128, 32, 128], BF16, "w1b"); bw1 = Buf("w1")
                        posf = c.sb([128, 32], F32, "posf"); posb = c.sb([128, 32], BF16, "posb")
                        for kv in range(2):
                            c.dma("sp", w1f[kv * 64:(kv + 1) * 64, :, :],
                                  W["ev_cmp_w1"][kv].rearrange("(l d) h -> d l h", d=64), writes=[bw1])
                            c.dma("sp", posf[kv * 64:(kv + 1) * 64, :], W["ev_cmp_pos"][kv], writes=[bw1])
                        c.op("pool", lambda: nc.gpsimd.tensor_copy(out=w1b[:], in_=w1f[:]), reads=[bw1], writes=[bw1])
                        c.op("dve", lambda: nc.vector.tensor_copy(out=posb[:], in_=posf[:]), reads=[bw1], writes=[bw1])
                        for T in range(NST):
                            tok = slice(T * 512, (T + 1) * 512)
                            self.proj_feat(pf[0], bpf[0], wN, bwN, 256, 128, T)
                            c.op("act", lambda: nc.scalar.copy(out=cmpT[:, tok], in_=pf[0][:, :]), reads=[bpf[0]],
                                 writes=[bcmpT])
                            self.proj_feat(pf[1], bpf[1], wN, bwN, 384, 128, T)
                            c.op("dve", lambda: nc.vector.tensor_copy(out=KS[0:64, tok], in_=pf[1][0:64, :]),
                                 reads=[bpf[1]], writes=[bKN])
                            c.op("act", lambda: nc.scalar.copy(out=KWn[0:64, tok], in_=pf[1][64:128, :]), reads=[bpf[1]],
                                 writes=[bKN])
                            for s_ in range(4):
                                t = T * 4 + s_
                                bank = 2 + (s_ % 2)
                                self.proj_tok(pf[bank][:, 0:128], bpf[bank], wN, bwN, 512, 128, t)
                                c.op("dve", lambda: nc.vector.tensor_copy(out=Vs[:, t, 0:64], in_=pf[bank][:, 0:64]),
                                     reads=[bpf[bank]], writes=[bKN])
                                c.op("act", lambda: nc.scalar.copy(out=Vw[:, t, 0:64], in_=pf[bank][:, 64:128]),
                                     reads=[bpf[bank]], writes=[bKN])
                        hidT = c.sb([128, 256], BF16, "hidT"); bhid = Buf("hid")
                        bh = c.sb([128, 2], F32, "bh")
                        for kv in range(2):
                            rows = slice(kv * 64, (kv + 1) * 64)
                            for l in range(32):
                                c.op("pe", lambda: nc.tensor.matmul(pf[0][:, 0:255], lhsT=w1b[rows, l, :],
                                                                    rhs=cmpT[rows, l:l + 16 * 254 + 1:16],
                                                                    start=(l == 0), stop=(l == 31)),
                                     reads=[bw1, bcmpT], writes=[bpf[0]])
                            for l in range(32):
                                c.op("pe", lambda: nc.tensor.matmul(pf[1][:, 0:1], lhsT=w1b[rows, l, :],
                                                                    rhs=posb[rows, l:l + 1], start=(l == 0), stop=(l == 31)),
                                     reads=[bw1], writes=[bpf[1]])
                            c.op("dve", lambda: nc.vector.tensor_tensor(out=bh[:, kv:kv + 1], in0=pf[1][:, 0:1],
                                                                        in1=b1t[:, kv:kv + 1], op=ALU.add),
                                 reads=[bpf[1], bw2], writes=[bhid])
                            c.op("act", lambda: nc.scalar.activation(out=hidT[:, 0:255], in_=pf[0][:, 0:255], func=AF.Silu,
                                                                     bias=bh[:, kv:kv + 1], scale=1.0),
                                 reads=[bpf[0], bhid], writes=[bhid])
                            if kv == 0:
                                c.op("pe", lambda: nc.tensor.matmul(pf[2][0:64, 0:255], lhsT=w2b[:, 0, :], rhs=hidT[:, 0:255],
                                                                    start=True, stop=True), reads=[bw2, bhid], writes=[bpf[2]])
                                c.op("dve", lambda: nc.vector.tensor_copy(out=KCA[0:64, 0:255], in_=pf[2][0:64, 0:255]),
                                     reads=[bpf[2]], writes=[bKC])
                            else:
                                for nt_, nn in enumerate([128, 127]):
                                    c.op("pe", lambda: nc.tensor.matmul(pf[2][0:nn, 0:64], lhsT=hidT[:, nt_ * 128:nt_ * 128 + nn],
                                                                        rhs=w2b[:, 1, :], start=True, stop=True),
                                         reads=[bw2, bhid], writes=[bpf[2]])
                                    c.op("dve", lambda: nc.vector.tensor_copy(out=VCA[0:nn, nt_, 0:64], in_=pf[2][0:nn, 0:64]),
                                         reads=[bpf[2]], writes=[bKC])
                    cmpm = c.sb([128, 2, 512], BF16, "cmpm"); bcmpm = Buf("cmpm")
                    BST = c.sb([96, 512], BF16, "BST"); bBST = Buf("BST")
                    c.op("pool", lambda: nc.gpsimd.memset(BST[64:96, :], 0.0), writes=[bBST])
                    sg = c.sb([128, 4, 12], F32, "sg"); bsg = Buf("sg")
                    imp = c.sb([128, 4, 64], F32, "imp"); bimp = [Buf(f"imp{s_}") for s_ in range(4)]
                    sc = c.sb([128, 64], F32, "sc"); sc2 = c.sb([128, 64], F32, "sc2")
                    m8a = c.sb([128, 8], F32, "m8a"); m8b = c.sb([128, 8], F32, "m8b"); bsc = Buf("sc")
                    bsel = c.sb([128, 64], BF16, "bsel")
                    nkc = [128, 127]
                    for T in range(NST):
                        tok = slice(T * 512, (T + 1) * 512)
                        c.dma("sp", cmpm[:], Cd["cmpm"][:, :, tok], writes=[bcmpm])
                        for jp in range(2):
                            i = jp % 2
                            self.proj_feat(pf[i], bpf[i], wN, bwN, jp * 128, 128, T)
                            c.op("act", lambda: nc.scalar.copy(out=QA[0:64, 2 * jp, :], in_=pf[i][0:64, :]),
                                 reads=[bpf[i]], writes=[bQA])
                            c.op("dve", lambda: nc.vector.tensor_copy(out=QA[0:64, 2 * jp + 1, :], in_=pf[i][64:128, :]),
                                 reads=[bpf[i]], writes=[bQA])
                        c.dma("sp", QA[64:67, :, :], Cd["qal"][:, 0:4, tok], writes=[bQA])
                        for s_ in range(4):
                            t = T * 4 + s_
                            bank = s_ % 2
                            self.proj_tok(pf[bank][:, 0:256], bpf[bank], wN, bwN, 652, 256, t)
                            c.op("act", lambda: nc.scalar.activation(out=zs[:, s_, :], in_=pf[bank][:, 0:256], func=AF.Silu),
                                 reads=[bpf[bank]], writes=[bzs])
                        for s_ in range(4):
                            t = T * 4 + s_
                            self.proj_tok(pf[2][:, s_ * 12:(s_ + 1) * 12], bpf[2], wN, bwN, 640, 12, t)
                        c.op("act", lambda: nc.scalar.activation(out=sg[:].rearrange("p s n -> p (s n)"), in_=pf[2][:, 0:48],
                                                                 func=AF.Sigmoid), reads=[bpf[2]], writes=[bsg])
                        for j in range(4):
                            self.attn_branch(
                                T, [0, 1], lambda a: KCA[0:96, a * 128:a * 128 + nkc[a]], lambda a: nkc[a],
                                QA[0:96, j, :], bQA,
                                lambda a: [(self.ident_b[0:nkc[a], 0:nkc[a]], cmpm[0:nkc[a], a, :], [self.bconst, bcmpm])],
                                lambda a: VCA[0:nkc[a], a, :], 129, bKC, lambda a: [0, 1, 2, 3])
                            for s_ in range(4):
                                acc, bacc = pf[2 + s_], bpf[2 + s_]
                                bst = bsts[s_]; k0 = s_ * 4
                                c.op("dve", lambda: nc.vector.tensor_scalar(out=st[:, k0 + 0:k0 + 1], in0=acc[:, 64:65], scalar1=1e-30,
                                                                            scalar2=None, op0=ALU.max), reads=[bacc], writes=[bst])
                                c.op("dve", lambda: nc.vector.reciprocal(out=st[:, k0 + 1:k0 + 2], in_=st[:, k0 + 0:k0 + 1]), reads=[bst], writes=[bst])
                                c.op("dve", lambda: nc.vector.tensor_tensor(out=st[:, k0 + 2:k0 + 3], in0=st[:, k0 + 1:k0 + 2],
                                                                            in1=sg[:, s_, j * 3:j * 3 + 1], op=ALU.mult),
                                     reads=[bst, bsg], writes=[bst])
                                c.op("dve", lambda: nc.vector.tensor_scalar(out=oh[:, s_, j * 64:(j + 1) * 64], in0=acc[:, 0:64],
                                                                            scalar1=st[:, k0 + 2:k0 + 3], scalar2=None, op0=ALU.mult),
                                     reads=[bacc, bst], writes=[boh[s_]])
                                if j == 0:
                                    c.op("dve", lambda: nc.vector.tensor_scalar(out=imp[:, s_, :], in0=acc[:, 65:129],
                                                                                scalar1=st[:, k0 + 1:k0 + 2], scalar2=None, op0=ALU.mult),
                                         reads=[bacc, bst], writes=[bimp[s_]])
                                else:
                                    c.op("dve", lambda: nc.vector.scalar_tensor_tensor(
                                        out=imp[:, s_, :], in0=acc[:, 65:129], scalar=st[:, k0 + 1:k0 + 2], in1=imp[:, s_, :],
                                        op0=ALU.mult, op1=ALU.add), reads=[bacc, bst, bimp[s_]], writes=[bimp[s_]])
                        for s_ in range(4):
                            t = T * 4 + s_
                            c.op("dve", lambda: nc.vector.tensor_tensor(out=sc[:], in0=imp[:, s_, :], in1=selc[:, t, :], op=ALU.add),
                                 reads=[bimp[s_], bc0], writes=[bsc])
                            c.op("dve", lambda: nc.vector.max(out=m8a[:], in_=sc[:]), reads=[bsc], writes=[bsc])
                            c.op("dve", lambda: nc.vector.match_replace(out=sc2[:], in_to_replace=m8a[:], in_values=sc[:],
                                                                        imm_value=-3e38), reads=[bsc], writes=[bsc])
                            c.op("dve", lambda: nc.vector.max(out=m8b[:], in_=sc2[:]), reads=[bsc], writes=[bsc])
                            c.op("dve", lambda: nc.vector.tensor_scalar(out=bsel[:], in0=sc[:], scalar1=m8b[:, 7:8], scalar2=1.0,
                                                                        op0=ALU.is_ge, op1=ALU.subtract), reads=[bsc], writes=[bsc])
                            c.op("pe", lambda: nc.tensor.transpose(out=pb[0][0:64, s_ * 128:(s_ + 1) * 128], in_=bsel[:],
                                                                   identity=self.ident_b[:]), reads=[bsc, self.bconst],
                                 writes=[bpb[0]])
                        c.op("act", lambda: nc.scalar.copy(out=BST[0:64, :], in_=pb[0][0:64, 0:512]), reads=[bpb[0]], writes=[bBST])
                        for j in range(4):
                            def sel_extra(a):
                                ex = [(esel[:, a * 128:(a + 1) * 128], BST[:], [besel, bBST])]
                                if a >= 4 * T:
                                    ex.append((self.ident_b[:], cm[:, a - 4 * T, :], [self.bconst, bc0]))
                                return ex
                            self.attn_branch(T, list(range(4 * T + 4)), lambda a: KS[0:96, a * 128:(a + 1) * 128],
                                             lambda a: 128, QA[0:96, j, :], bQA, sel_extra,
                                             lambda a: Vs[:, a, :], 65, bKN, causal_subs(T), n_fill=N_FILL_SEL)
                            for br, gi in ((0, 1),):
                                for s_ in range(4):
                                    acc, bacc = pf[2 + s_], bpf[2 + s_]
                                    bst = bsts[s_]; k0 = s_ * 4
                                    c.op("dve", lambda: nc.vector.reciprocal(out=st[:, k0 + 1:k0 + 2], in_=acc[:, 64:65]),
                                         reads=[bacc], writes=[bst])
                                    c.op("dve", lambda: nc.vector.tensor_tensor(out=st[:, k0 + 2:k0 + 3], in0=st[:, k0 + 1:k0 + 2],
                                                                                in1=sg[:, s_, j * 3 + gi:j * 3 + gi + 1],
                                                                                op=ALU.mult), reads=[bst, bsg], writes=[bst])
                                    c.op("dve", lambda: nc.vector.scalar_tensor_tensor(
                                        out=oh[:, s_, j * 64:(j + 1) * 64], in0=acc[:, 0:64], scalar=st[:, k0 + 2:k0 + 3],
                                        in1=oh[:, s_, j * 64:(j + 1) * 64], op0=ALU.mult, op1=ALU.add),
                                        reads=[bacc, bst, boh[s_]], writes=[boh[s_]])

                            def win_extra(a):
                                if a >= 4 * T:
                                    return [(self.ident_b[:], cm[:, a - 4 * T, :], [self.bconst, bc0])]
                                return [(self.ident_b[:], wm[:, a - (4 * T - 4), :], [self.bconst, bc0])]

                            def win_subs(a):
                                return [s_ for s_ in range(4) if 4 * T + s_ - 4 <= a <= 4 * T + s_]
                            self.attn_branch(T, list(range(max(0, 4 * T - 4), 4 * T + 4)),
                                             lambda a: KWn[0:96, a * 128:(a + 1) * 128], lambda a: 128, QA[0:96, j, :], bQA,
                                             win_extra, lambda a: Vw[:, a, :], 65, bKN, win_subs)
                            for s_ in range(4):
                                acc, bacc = pf[2 + s_], bpf[2 + s_]
                                bst = bsts[s_]; k0 = s_ * 4
                                c.op("dve", lambda: nc.vector.reciprocal(out=st[:, k0 + 1:k0 + 2], in_=acc[:, 64:65]),
                                     reads=[bacc], writes=[bst])
                                c.op("dve", lambda: nc.vector.tensor_tensor(out=st[:, k0 + 2:k0 + 3], in0=st[:, k0 + 1:k0 + 2],
                                                                            in1=sg[:, s_, j * 3 + 2:j * 3 + 3],
                                                                            op=ALU.mult), reads=[bst, bsg], writes=[bst])
                                c.op("dve", lambda: nc.vector.scalar_tensor_tensor(
                                    out=oh[:, s_, j * 64:(j + 1) * 64], in0=acc[:, 0:64], scalar=st[:, k0 + 2:k0 + 3],
                                    in1=oh[:, s_, j * 64:(j + 1) * 64], op0=ALU.mult, op1=ALU.add),
                                    reads=[bacc, bst, boh[s_]], writes=[boh[s_]])
                        c.op("pool", lambda: nc.gpsimd.tensor_tensor(out=og[:], in0=oh[:], in1=zs[:], op=ALU.mult),
                             reads=boh + [bzs], writes=[bog])
                        c.dma("pool", self.ogl[T // 4][(T % 4) * 512:(T % 4 + 1) * 512, 256:512].rearrange("(s p) n -> p s n", p=128),
                              og[:], reads=[bog], writes=[self.bogl[T // 4]])
                        if T % 4 == 3:
                            self.coll("AllGather", self.ogl[T // 4], self.ogg[T // 4], self.bogl[T // 4], self.bogg[T // 4])

    def final_pass(self, ogg, bogg, wo_src, res_src, res_bufs, lng_src, lnb_src, dst, dst_bufs, make_xT):
        c, nc = self.c, self.nc
        pf, bpf, pb, bpb = self.pf, self.bpf, self.pb, self.bpb
        NTH = NT // 2
        with c.phase():
            if isinstance(wo_src, tuple):
                wo, bwo = wo_src
            else:
                wo = c.sb([128, 8, D], BF16, "wo0"); bwo = Buf("wo0")
                self.load_weight_bf16(wo, bwo, wo_src, 0, D)
            lng = c.sb([128, D], F32, "lng0"); lnb = c.sb([128, D], F32, "lnb0"); bsm = Buf("small0")
            c.dma("sp", lng[:], lng_src[0:1, :].partition_broadcast(128), writes=[bsm])
            c.dma("sp", lnb[:], lnb_src[0:1, :].partition_broadcast(128), writes=[bsm])
            cand_ = [c.sb([128, 2, 2, 512], BF16, "cand") for _ in range(2)]; bcand_ = [Buf("cand0"), Buf("cand1")]
            ogt_ = [c.sb([128, D], BF16, "ogt0") for _ in range(2)]; bogt_ = [Buf("ogt0a"), Buf("ogt0b")]
            ogT_ = [c.sb([128, 8, 128], BF16, "ogT0") for _ in range(2)]; bogT_ = [Buf("ogT0a"), Buf("ogT0b")]
            res_ = [c.sb([128, D], F32, "res0") for _ in range(2)]; bres_ = [Buf("res0a"), Buf("res0b")]
            rr_ = [c.sb([128, D], F32, "rr0") for _ in range(2)]; brr_ = [Buf("rr0a"), Buf("rr0b")]
            lnst_ = [c.sb([128, 12], F32, "lnst0") for _ in range(2)]; lnmv_ = [c.sb([128, 8], F32, "lnmv0") for _ in range(2)]
            bln_ = [Buf("ln0a"), Buf("ln0b")]
            if make_xT:
                self.xTt = [c.sb([128, 8, 128], BF16, "xTt") for _ in range(2)]; self.bxTt = [Buf("xTt0"), Buf("xTt1")]

            def stage_a_pe(t):
                p_ = t % 2
                cand, bcand = cand_[p_], bcand_[p_]
                ogt, bogt, ogT, bogT = ogt_[p_], bogt_[p_], ogT_[p_], bogT_[p_]
                res, bres = res_[p_], bres_[p_]
                for r_ in range(2):
                    for h_ in range(2):
                        row0 = r_ * SH + t * 128
                        c.dma("sp", cand[:, r_, h_, :], ogg[h_][row0:row0 + 128, :], reads=[bogg[h_]], writes=[bcand])
                c.dma("sp", res[:], res_src[t * 128:(t + 1) * 128, :],
                      reads=([res_bufs[t]] if res_bufs else []), writes=[bres])
                for r_ in range(2):
                    o_ = ogt[:, r_ * 512:(r_ + 1) * 512]
                    c.op("dve", lambda: nc.vector.tensor_scalar(out=o_, in0=cand[:, r_, 0, :], scalar1=self.sel[:, 0:1],
                                                                scalar2=None, op0=ALU.mult),
                         reads=[bcand, self.bconst], writes=[bogt])
                    c.op("dve", lambda: nc.vector.scalar_tensor_tensor(out=o_, in0=cand[:, r_, 1, :], scalar=self.sel[:, 1:2],
                                                                       in1=o_, op0=ALU.mult, op1=ALU.add),
                         reads=[bcand, self.bconst, bogt], writes=[bogt])
                for ch in range(8):
                    c.op("pe", lambda: nc.tensor.transpose(out=pb[1][:, ch * 128:(ch + 1) * 128],
                                                           in_=ogt[:, ch * 128:(ch + 1) * 128], identity=self.ident_b[:]),
                         reads=[bogt, self.bconst], writes=[bpb[1]])
                c.op("act", lambda: nc.scalar.copy(out=ogT[:].rearrange("p c n -> p (c n)"), in_=pb[1][:]),
                     reads=[bpb[1]], writes=[bogT])
                for half in range(2):
                    bk = 4 + half
                    for ch in range(8):
                        c.op("pe", lambda: nc.tensor.matmul(pf[bk][:], lhsT=ogT[:, ch, :],
                                                            rhs=wo[:, ch, half * 512:(half + 1) * 512],
                                                            start=(ch == 0), stop=(ch == 7)), reads=[bogT, bwo], writes=[bpf[bk]])

            def stage_a_dve(t):
                p_ = t % 2
                res, bres, rr, brr = res_[p_], bres_[p_], rr_[p_], brr_[p_]
                for half in range(2):
                    bk = 4 + half
                    c.op("dve", lambda: nc.vector.scalar_tensor_tensor(
                        out=rr[:, half * 512:(half + 1) * 512], in0=res[:, half * 512:(half + 1) * 512], scalar=ALPHA,
                        in1=pf[bk][:], op0=ALU.mult, op1=ALU.add), reads=[bres, bpf[bk]], writes=[brr])

            def stage_bc(t):
                p_ = t % 2
                rr, brr = rr_[p_], brr_[p_]
                self.ln_stats, self.ln_mv, self.bln = lnst_[p_], lnmv_[p_], bln_[p_]
                self.ln_store(rr, brr, lng, lnb, bsm, dst, dst_bufs[t], t, also_T=make_xT)

            stage_a_pe(0)
            stage_a_dve(0)
            for t in range(NTH):
                if t + 1 < NTH:
                    stage_a_pe(t + 1)
                stage_bc(t)
                if t + 1 < NTH:
                    stage_a_dve(t + 1)


_CACHE = {}


def _get_prog(mode):
    if mode not in _CACHE:
        _CACHE[mode] = Prog(mode)
    return _CACHE[mode]


def _in_map(p, inputs, core):
    b, r = core // 2, core % 2
    asc = np.ascontiguousarray
    ar = np.arange

    def f(k):
        return np.asarray(inputs[k], dtype=np.float32)[0]
    x = np.asarray(inputs["x"], dtype=np.float32)[b]
    m = {"x": asc(x), "xh": asc(x[r * SH:(r + 1) * SH])}
    w_in = f("ev_w_in")
    cols_m = np.concatenate([ar(base + r * 256, base + (r + 1) * 256) for base in (1816, 2328, 2840, 3352)])
    m["ev_wm"] = asc(w_in[:, cols_m])
    cols_n = np.concatenate([ar(r * 256, (r + 1) * 256)]
                            + [ar(512 + si * 128 + r * 64, 512 + si * 128 + (r + 1) * 64) for si in (0, 1, 2, 4, 3, 5)]
                            + [ar(1280 + r * 12, 1280 + (r + 1) * 12), ar(1304 + r * 256, 1304 + (r + 1) * 256)])
    m["ev_wn"] = asc(w_in[:, cols_n])
    m["ev_cmp_pos"] = asc(f("ev_cmp_pos").transpose(0, 2, 1))
    m["ev_cmp_w1"] = asc(f("ev_cmp_w1"))
    m["ev_cmp_b1"] = asc(f("ev_cmp_b1").T)
    m["ev_cmp_w2"] = asc(f("ev_cmp_w2"))
    perm = np.concatenate([ar(512, 768), ar(0, 256), ar(768, 1024), ar(256, 512)])
    m["ev_w_out"] = asc(f("ev_w_out")[perm, :])
    m["ev_ln_g"] = asc(f("ev_ln_g").reshape(1, D))
    m["ev_ln_b"] = asc(f("ev_ln_b").reshape(1, D))
    w1 = f("od_w_in")
    cols1 = np.concatenate([ar(r * 256, (r + 1) * 256), ar(512 + r * 256, 512 + (r + 1) * 256),
                            ar(1024 + r * 512, 1024 + (r + 1) * 512), ar(2048, 2064),
                            ar(2064 + r * 512, 2064 + (r + 1) * 512)])
    m["od_w1"] = asc(w1[:, cols1])
    m["od_gate_w2"] = asc(f("od_gate_w2")[:, r * 256:(r + 1) * 256])
    m["od_gate_b"] = asc(f("od_gate_b")[r * 256:(r + 1) * 256].reshape(2, 128).T)
    m["od_gn_g"] = asc(f("od_gn_g")[r * 512:(r + 1) * 512].reshape(1, 512))
    m["od_w_out"] = asc(f("od_w_out"))
    m["od_ln_g"] = asc(f("od_ln_g").reshape(1, D))
    m["od_ln_b"] = asc(f("od_ln_b").reshape(1, D))
    for nm, arr in make_consts(r).items():
        m["c_" + nm] = arr
    return m


def kernel(**inputs):
    inputs = {k: np.asarray(v) for k, v in inputs.items()}
    p = _get_prog("full")
    in_maps = [_in_map(p, inputs, core) for core in range(8)]
    res = run_bass_kernel_spmd(p.nc, in_maps, core_ids=list(range(8)))
    out = np.empty((4, S, D), np.float32)
    for core in range(8):
        b, r = core // 2, core % 2
        out[b, r * SH:(r + 1) * SH] = res.results[core]["out"]
    return out
```

```python
import numpy as np
from contextlib import ExitStack
import concourse.bass as bass
import concourse.mybir as mybir
from concourse.bass_utils import run_bass_kernel_spmd

F32 = mybir.dt.float32
BF16 = mybir.dt.bfloat16
AF = mybir.ActivationFunctionType
ALU = mybir.AluOpType
AX = mybir.AxisListType

S = 4096
D = 1024
NT = S // 128
NST = S // 512
SH = S // 2
ALPHA = float((2.0 * 2) ** 0.25)
LN_EPS = 1e-5
EV_IN = 3864
OD_IN = 3088
NEGM = -30000.0

EPOCH = 30000
STRICT_SAME_ENGINE = False
import os
N_DUMMY = int(os.environ.get('N_DUMMY', '1'))
N_FILL_SEL = int(os.environ.get('N_FILL_SEL', '0'))
L1_FILL = int(os.environ.get('L1_FILL', '0'))
FILL_N = int(os.environ.get('FILL_N', '512'))
NDMA = 24


class Buf:
    __slots__ = ("name", "w", "r")

    def __init__(self, name=""):
        self.name = name
        self.w = []
        self.r = []


class Ctx:
    def __init__(self, nc, stack):
        self.nc = nc
        self.stack = stack
        self.cur = stack
        self.eng = {"pe": nc.tensor, "act": nc.scalar, "dve": nc.vector,
                    "pool": nc.gpsimd, "sp": nc.sync}
        self.sem = {}
        self.cnt = {}
        self.nsem = 0
        for k in self.eng:
            self._new_sem(k)
        self.waited = {}
        self.dma_sems = [[self._alloc(f"dma{i}") for i in range(NDMA)], [self._alloc(f"swdma{i}") for i in range(12)]]
        self.dma_val = [[0] * NDMA, [0] * 12]
        self.dma_rr = [0, 0]
        self.sw_tickets = []
        self.n_ins = 0
        self.n_wait = 0
        self.nname = 0

    def _alloc(self, name):
        self.nsem += 1
        return self.stack.enter_context(self.nc.semaphore(f"{name}_{self.nsem}"))

    def _new_sem(self, k):
        self.sem[k] = self._alloc(f"e_{k}")
        self.cnt[k] = 0

    def sb(self, shape, dt, name=None):
        self.nname += 1
        return self.cur.enter_context(
            self.nc.sbuf_tensor(f"{name or 'sb'}_{self.nname}", list(shape), dt))

    def barrier(self):
        tickets = [(self.sem[k], self.cnt[k], k) for k in self.eng if self.cnt[k] > 0]
        for g_ in range(2):
            tickets += [(self.dma_sems[g_][i], v, "dma") for i, v in enumerate(self.dma_val[g_]) if v > 0]
        tickets += self.sw_tickets
        for e in self.eng:
            self._wait(e, [t for t in tickets if t[2] != e])

    def phase(self):
        ctx = self

        class _Ph:
            def __enter__(self_):
                self_.prev = ctx.cur
                self_.st = ExitStack()
                self_.st.__enter__()
                ctx.cur = self_.st
                return self_

            def __exit__(self_, *a):
                ctx.barrier()
                ctx.cur = self_.prev
                return self_.st.__exit__(*a)
        return _Ph()

    def ps(self, shape, dt, name=None):
        self.nname += 1
        return self.stack.enter_context(
            self.nc.psum_tensor(f"{name or 'ps'}_{self.nname}", list(shape), dt))

    def _wait(self, eng, tickets):
        best = {}
        for t in tickets:
            sem, val, src = t
            key = (eng, id(sem))
            if self.waited.get(key, 0) >= val:
                continue
            if key not in best or best[key][1] < val:
                best[key] = t
        for key, (sem, val, src) in best.items():
            self.eng[eng].wait_ge(sem, val)
            self.waited[key] = val
            self.n_wait += 1

    def _deps(self, eng, reads, writes):
        deps = []
        for b in reads:
            deps.extend(b.w)
        for b in writes:
            for t in b.w:
                if t[2] != eng or (STRICT_SAME_ENGINE and eng not in ("pe", "dma")):
                    deps.append(t)
            for t in b.r:
                if t[2] != eng or (STRICT_SAME_ENGINE and eng != "pe") or eng == "dma":
                    deps.append(t)
        return deps

    def _commit(self, ticket, reads, writes):
        for b in reads:
            b.r.append(ticket)
            if len(b.r) > 64:
                best = {}
                for t in b.r:
                    k = id(t[0])
                    if k not in best or best[k][1] < t[1]:
                        best[k] = t
                b.r = list(best.values())
        for b in writes:
            if ticket[2] == "dma" and b.w and all(t[2] == "dma" for t in b.w):
                b.w = b.w + [ticket]
                if len(b.w) > 32:
                    best = {}
                    for t in b.w:
                        k = id(t[0])
                        if k not in best or best[k][1] < t[1]:
                            best[k] = t
                    b.w = list(best.values())
            else:
                b.w = [ticket]
            b.r = []

    def op(self, eng, fn, reads=(), writes=()):
        self._wait(eng, self._deps(eng, reads, writes))
        ins = fn()
        if self.cnt[eng] >= EPOCH:
            self._new_sem(eng)
        self.cnt[eng] += 1
        ins.then_inc(self.sem[eng], 1)
        ticket = (self.sem[eng], self.cnt[eng], eng)
        self._commit(ticket, reads, writes)
        self.n_ins += 1
        return ticket

    def dma(self, q, out, in_, reads=(), writes=(), **kw):
        grp = 1 if q == "pool" else 0
        i = self.dma_rr[grp]
        self.dma_rr[grp] = (i + 1) % len(self.dma_sems[grp])
        sem = self.dma_sems[grp][i]
        deps = self._deps("dma", reads, writes)
        if self.dma_val[grp][i] > 0:
            deps.append((sem, self.dma_val[grp][i], "dma"))
        self._wait(q, deps)
        ins = self.eng[q].dma_start(out=out, in_=in_, **kw)
        self.dma_val[grp][i] += 16
        ins.then_inc(sem, 16)
        ticket = (sem, self.dma_val[grp][i], "dma")
        self._commit(ticket, reads, writes)
        self.n_ins += 1
        return ticket

    def wait_all(self, eng, bufs):
        deps = []
        for b in bufs:
            deps.extend(b.w)
            deps.extend(b.r)
        self._wait(eng, deps)


def _bf16(a):
    import ml_dtypes
    return np.asarray(a, dtype=np.float32).astype(ml_dtypes.bfloat16)


def make_consts(r=0):
    c = {}
    c["ident_f"] = np.eye(128, dtype=np.float32)
    c["ident_b"] = _bf16(np.eye(128))
    k = np.arange(128)[:, None]
    q = np.arange(128)[None, :]
    c["tri_b"] = _bf16((k <= q).astype(np.float32))
    c["ones_f"] = np.ones((128, 128), np.float32)
    slopes = 2.0 ** (-(np.arange(8) + 1.0))
    tq = np.arange(S, dtype=np.float64)
    qal = np.zeros((3, 8, S), np.float32)
    for h in range(8):
        qal[0, h] = 8.0 * slopes[h]
        qal[1, h] = 1024.0 * slopes[h]
        qal[2, h] = -8.0 * slopes[h] * tq
    c["qal"] = _bf16(qal[:, 4 * r:4 * r + 4, :])
    sel = np.zeros((128, 2), np.float32)
    sel[:, r] = 1.0
    c["sel"] = sel
    kp = np.arange(S)
    kal = np.stack([kp % 128, kp // 128, np.ones(S)]).astype(np.float32)
    c["kaug_n"] = _bf16(kal)
    cp = 16 * np.arange(255) + 31
    c["kaug_c"] = _bf16(np.stack([cp % 128, cp // 128, np.ones(255)]).astype(np.float32))
    e16 = (kp[None, :] // 256 == np.arange(16)[:, None]).astype(np.float32) * 30000.0
    c["kaug_m"] = _bf16(np.concatenate([e16, kal], axis=0))
    c["esel"] = _bf16((kp[None, :] // 64 == np.arange(64)[:, None]).astype(np.float32) * 30000.0)
    kk = np.arange(128)[:, None, None] + 128 * np.arange(4)[None, :, None]
    qq = np.arange(512)[None, None, :]
    c["cm"] = _bf16(np.where(kk <= qq, 0.0, NEGM))
    c["wm"] = _bf16(np.where(kk > qq, 0.0, NEGM))
    n = np.arange(128)[:, None, None] + 128 * np.arange(2)[None, :, None]
    qs = np.arange(S)[None, None, :]
    c["cmpm"] = _bf16(np.where((16 * n + 31 <= qs) & (n < 255), 0.0, NEGM))
    n2 = np.arange(128)[:, None, None] + 128 * np.arange(2)[None, :, None]
    sb = np.arange(64)[None, None, :]
    c["ovl"] = _bf16(((16 * n2 <= 64 * sb + 63) & (16 * n2 + 31 >= 64 * sb) & (n2 < 255)).astype(np.float32))
    tqm = (np.arange(128)[:, None, None] + 128 * np.arange(32)[None, :, None])
    cur = tqm // 64
    forced = (sb == 0) | (sb == cur) | (sb == cur - 1)
    c["selc"] = np.where(sb <= cur, np.where(forced, 1e4, 0.0), -1e30).astype(np.float32)
    nb = np.arange(16)[None, None, :]
    curm = tqm // 256
    mobc = np.where(nb < curm, 0.0, -1e30).astype(np.float32)
    ownc = np.where(nb == curm, 0.0, -1.0).astype(np.float32)
    c["mobc4"] = np.ascontiguousarray(np.broadcast_to(mobc.reshape(128, NST, 4, 1, 16), (128, NST, 4, 4, 16))).reshape(128, NST, 256)
    c["ownc4"] = np.ascontiguousarray(np.broadcast_to(ownc.reshape(128, NST, 4, 1, 16), (128, NST, 4, 4, 16))).reshape(128, NST, 256)
    return c


class _Stop(Exception):
    pass


class Prog:
    def __init__(self, mode="full"):
        self.mode = mode
        self.nc = bass.Bass("TRN2", target_bir_lowering=False)
        self.din = {}
        self.build()

    def dram_in(self, name, shape, dt=F32):
        t = self.nc.dram_tensor(name, list(shape), dt, kind="ExternalInput").ap()
        self.din[name] = t
        return t

    def build(self):
        nc = self.nc
        x = self.dram_in("x", [S, D])
        xh = self.dram_in("xh", [SH, D])
        W = {}
        for nm, shp in [("ev_wm", [D, 1024]), ("ev_wn", [D, 908]), ("ev_cmp_pos", [2, 64, 32]), ("ev_cmp_w1", [2, 2048, 128]),
                        ("ev_cmp_b1", [128, 2]), ("ev_cmp_w2", [2, 128, 64]), ("ev_w_out", [D, D]),
                        ("ev_ln_g", [1, D]), ("ev_ln_b", [1, D]), ("od_w1", [D, 1552]),
                        ("od_gate_w2", [16, 256]), ("od_gate_b", [128, 2]), ("od_gn_g", [1, 512]),
                        ("od_w_out", [D, D]), ("od_ln_g", [1, D]), ("od_ln_b", [1, D])]:
            W[nm] = self.dram_in(nm, shp)
        self.W = W
        C = {}
        for nm, arr in make_consts().items():
            C[nm] = self.dram_in("c_" + nm, list(arr.shape), F32 if arr.dtype == np.float32 else BF16)
        self.Cd = C
        out = nc.dram_tensor("out", [SH, D], F32, kind="ExternalOutput").ap()
        self.x, self.xh, self.out = x, xh, out
        self.x1d = nc.dram_tensor("x1d", [SH, D], F32).ap()
        self.ogl = [nc.dram_tensor(f"ogl{h}", [SH, 512], BF16).ap() for h in range(2)]
        self.ogg = [nc.dram_tensor(f"ogg{h}", [2 * SH, 512], BF16).ap() for h in range(2)]
        self.xTl = [nc.dram_tensor(f"xTl{q}", [D, 512], BF16).ap() for q in range(4)]
        self.xTg = [nc.dram_tensor(f"xTg{q}", [2 * D, 512], BF16).ap() for q in range(4)]
        self.og1l = [nc.dram_tensor(f"og1l{h}", [SH, 512], BF16).ap() for h in range(2)]
        self.og1g = [nc.dram_tensor(f"og1g{h}", [2 * SH, 512], BF16).ap() for h in range(2)]
        self.dbg = None
        self.bx1d = [Buf(f"x1d{t}") for t in range(NT // 2)]
        mk = lambda n: [Buf(n + "0"), Buf(n + "1")]
        self.bogl = mk("ogl"); self.bogg = mk("ogg"); self.bxTl = [Buf(f"xTl{q}") for q in range(4)]; self.bxTg = [Buf(f"xTg{q}") for q in range(4)]
        self.bog1l = mk("og1l"); self.bog1g = mk("og1g")

        with ExitStack() as st:
            c = Ctx(nc, st)
            self.c = c
            self.pf = [c.ps([128, 512], F32, "pf") for _ in range(7)]
            self.bpf = [Buf(f"pf{i}") for i in range(7)]
            pb0 = c.ps([128, 1024], BF16, "pb")
            self.pb = [pb0, pb0]
            b_pb0 = Buf("pb0")
            self.bpb = [b_pb0, b_pb0]
            self.ident_f = c.sb([128, 128], F32); self.ident_b = c.sb([128, 128], BF16)
            self.tri_b = c.sb([128, 128], BF16); self.ones_f = c.sb([128, 128], F32)
            self.sel = c.sb([128, 2], F32)
            self.bconst = Buf("const")
            for t, nm in [(self.ident_f, "ident_f"), (self.ident_b, "ident_b"), (self.tri_b, "tri_b"),
                          (self.ones_f, "ones_f"), (self.sel, "sel")]:
                c.dma("sp", t[:], C[nm][:, :], writes=[self.bconst])
            self.xT = c.sb([128, 8, S], BF16, "xT")
            self.bxT = [Buf(f"xT{t}") for t in range(NT)]
            self.wpass = [c.sb([128, 8, 1024], BF16, "wpA"), c.sb([128, 8, 1024], BF16, "wpB")]
            self.bwpass = [Buf("wpA"), Buf("wpB")]
            self.out_bufs = []
            self.layer0()
            with c.phase():
                l1w = self.l1_weights()
                self.final_pass(self.ogg, self.bogg, (self.wpass[0], self.bwpass[0]), self.xh, None, W["ev_ln_g"], W["ev_ln_b"],
                                self.x1d, self.bx1d, make_xT=True)
                self.reload_xT_quarter(3)
                with c.phase():
                    self.layer1(weights=l1w)
                bout = Buf("out"); self.out_bufs.append(bout)
                self.final_pass(self.og1g, self.bog1g, (l1w[2], l1w[3]), self.x1d, self.bx1d, W["od_ln_g"], W["od_ln_b"],
                                self.out, [bout] * (NT // 2), make_xT=False)
            c.wait_all("sp", self.out_bufs)
            print("instructions", c.n_ins, "waits", c.n_wait, "sems", c.nsem)

    def coll(self, kind, src, dst, bsrc, bdst):
        c, nc = self.c, self.nc
        c._wait("pool", c._deps("dma", [bsrc], [bdst]))
        sem = c._alloc("cc")
        ins = nc.gpsimd.collective_compute(kind, ALU.bypass, replica_groups=[[0, 1], [2, 3], [4, 5], [6, 7]],
                                           ins=[src[:, :]], outs=[dst[:, :]])
        ins.then_inc(sem, 1)
        tk = (sem, 1, "dma")
        c.sw_tickets.append(tk)
        c._commit(tk, [bsrc], [bdst])

    def load_weight_bf16(self, dst, dst_buf, src, col0, ncols):
        c = self.c
        for o in range(0, ncols, 1024):
            n = min(1024, ncols - o)
            c.dma("pool", dst[:, :, o:o + n],
                  src[:, col0 + o:col0 + o + n].rearrange("(c p) n -> p c n", p=128), writes=[dst_buf])

    def load_xT_alloc(self):
        c = self.c
        self.xin = [c.sb([128, D], F32, "xin") for _ in range(4)]
        self.bxin = [Buf() for _ in range(4)]

    def load_xT_tiles(self, src, t0, t1):
        c, nc = self.c, self.nc
        xin, bxin = self.xin, self.bxin
        for t in range(t0, t1):
            s = t % 4
            c.dma("sp", xin[s][:], src[t * 128:(t + 1) * 128, :], writes=[bxin[s]])
            for h in range(2):
                bank, bb = self.pf[2 + h], self.bpf[2 + h]
                for j in range(4):
                    ch = h * 4 + j
                    c.op("pe", lambda: nc.tensor.transpose(out=bank[:, j * 128:(j + 1) * 128],
                                                           in_=xin[s][:, ch * 128:(ch + 1) * 128],
                                                           identity=self.ident_f[:]),
                         reads=[bxin[s], self.bconst], writes=[bb])
                eng = "act" if h == 0 else "dve"
                dst = self.xT[:, h * 4:(h + 1) * 4, t * 128:(t + 1) * 128]
                src_ps = bank[:].rearrange("p (c n) -> p c n", c=4)
                if eng == "act":
                    c.op("act", lambda: nc.scalar.copy(out=dst, in_=src_ps), reads=[bb], writes=[self.bxT[t]])
                else:
                    c.op("dve", lambda: nc.vector.tensor_copy(out=dst, in_=src_ps), reads=[bb],
                         writes=[self.bxT[t]])

    def ln_store(self, r, br, g_t, b_t, bgb, dst_dram, dst_buf, t, also_T=None):
        c, nc = self.c, self.nc
        self.ln_half_bufs = [Buf("lnh0"), Buf("lnh1")]
        st = self.ln_stats; bst = self.bln
        for h in range(2):
            c.op("dve", lambda: nc.vector.bn_stats(out=st[:, h * 6:(h + 1) * 6], in_=r[:, h * 512:(h + 1) * 512]),
                 reads=[br], writes=[bst])
        mv = self.ln_mv
        c.op("dve", lambda: nc.vector.bn_aggr(out=mv[:, 0:2], in_=st[:, 0:12]), reads=[bst], writes=[bst])
        c.op("dve", lambda: nc.vector.tensor_scalar(out=mv[:, 2:3], in0=mv[:, 1:2], scalar1=LN_EPS, scalar2=None,
                                                    op0=ALU.add), reads=[bst], writes=[bst])
        c.op("act", lambda: nc.scalar.activation(out=mv[:, 3:4], in_=mv[:, 2:3], func=AF.Sqrt),
             reads=[bst], writes=[bst])
        c.op("dve", lambda: nc.vector.reciprocal(out=mv[:, 4:5], in_=mv[:, 3:4]), reads=[bst], writes=[bst])
        c.op("dve", lambda: nc.vector.tensor_scalar(out=r[:], in0=r[:], scalar1=mv[:, 0:1], scalar2=mv[:, 4:5],
                                                    op0=ALU.subtract, op1=ALU.mult), reads=[br, bst], writes=[br])
        c.op("pool", lambda: nc.gpsimd.tensor_tensor(out=r[:], in0=r[:], in1=g_t[:], op=ALU.mult),
             reads=[br, bgb], writes=[br])
        c.op("pool", lambda: nc.gpsimd.tensor_tensor(out=r[:], in0=r[:], in1=b_t[:], op=ALU.add),
             reads=[br, bgb], writes=[br])
        c.dma(self.store_q, dst_dram[t * 128:(t + 1) * 128, :], r[:], reads=[br], writes=[dst_buf])
        if also_T:
            self.x1T_transposes(r, br, t)

    def x1T_transposes(self, r, br, t):
        c, nc = self.c, self.nc
        xt = self.xTt[t % 2]; bxt = self.bxTt[t % 2]
        for h in range(2):
            bank, bb = self.pf[h], self.bpf[h]
            for j in range(4):
                ch = h * 4 + j
                c.op("pe", lambda: nc.tensor.transpose(out=bank[:, j * 128:(j + 1) * 128],
                                                       in_=r[:, ch * 128:(ch + 1) * 128],
                                                       identity=self.ident_f[:]),
                     reads=[br, self.bconst], writes=[bb])
            c.op("act", lambda: nc.scalar.copy(out=xt[:, h * 4:(h + 1) * 4, :],
                                               in_=bank[:].rearrange("p (c n) -> p c n", c=4)),
                 reads=[bb], writes=[bxt])
        q_ = t // 4
        c.dma(self.store_q, self.xTl[q_][:, (t % 4) * 128:(t % 4 + 1) * 128].rearrange("(c p) n -> p c n", p=128),
              xt[:], reads=[bxt], writes=[self.bxTl[q_]])
        if t % 4 == 3:
            self.coll("AllGather", self.xTl[q_], self.xTg[q_], self.bxTl[q_], self.bxTg[q_])
            if q_ >= 1:
                self.reload_xT_quarter(q_ - 1)

    def reload_xT_quarter(self, q_):
        c = self.c
        for r_ in range(2):
            tok0 = r_ * SH + q_ * 512
            c.dma("sp", self.xT[:, :, tok0:tok0 + 512],
                  self.xTg[q_][r_ * D:(r_ + 1) * D, :].rearrange("(c p) n -> p c n", p=128),
                  reads=[self.bxTg[q_]], writes=self.bxT[tok0 // 128:tok0 // 128 + 4])

    def l1_weights(self):
        c, W = self.c, self.W
        wb = c.sb([128, 8, 1552], BF16, "w1"); bwb = Buf("w1")
        self.load_weight_bf16(wb, bwb, W["od_w1"], 0, 1552)
        wo = c.sb([128, 8, D], BF16, "wo1"); bwo = Buf("wo1")
        self.load_weight_bf16(wo, bwo, W["od_w_out"], 0, D)
        return wb, bwb, wo, bwo

    def layer1(self, weights=None):
        c, nc, W = self.c, self.nc, self.W
        pf, bpf, pb, bpb = self.pf, self.bpf, self.pb, self.bpb
        wb, bwb, wo, bwo = weights if weights is not None else self.l1_weights()
        gw2 = c.sb([16, 256], F32, "gw2"); negb = c.sb([128, 2], F32, "negb")
        gng = c.sb([128, 512], F32, "gng")
        bsm = Buf("small1")
        c.dma("sp", gw2[:], W["od_gate_w2"][:, :], writes=[bsm])
        c.dma("sp", negb[:], W["od_gate_b"][:, :], writes=[bsm])
        c.dma("sp", gng[:], W["od_gn_g"][0:1, :].partition_broadcast(128), writes=[bsm])
        c.op("dve", lambda: nc.vector.tensor_scalar(out=negb[:], in0=negb[:], scalar1=-1.0, scalar2=None,
                                                    op0=ALU.mult), reads=[bsm], writes=[bsm])
        Sf = c.sb([128, 2, 256], F32, "Sf"); Sb = c.sb([128, 2, 256], BF16, "Sb")
        bS = [Buf(f"S{h}") for h in range(2)]; bSb = [Buf(f"Sb{h}") for h in range(2)]
        for h in range(2):
            c.op("dve", lambda: nc.vector.memset(Sf[:, h, :], 0.0), writes=[bS[h]])
            c.op("pool", lambda: nc.gpsimd.memset(Sb[:, h, :], 0.0), writes=[bSb[h]])
        glT = c.sb([16, 512], F32, "glT"); bgl = Buf("glT")

        class Slot:
            pass
        slots = []
        e1_ = c.sb([128, 512], F32, "e1"); be1_ = Buf("e1")
        sp_ = e1_; bsp_ = be1_
        bs_ = c.sb([128, 512], F32, "bs"); bbs_ = Buf("bs")
        for si in range(2):
            o = Slot()
            o.e1, o.be1, o.sp, o.bsp, o.bs, o.bbs = e1_, be1_, sp_, bsp_, bs_, bbs_
            o.eb = c.sb([128, 512], F32, "eb"); o.beb = Buf("eb")
            o.enb = c.sb([128, 512], F32, "enb"); o.benb = Buf("enb")
            o.qt = c.sb([128, 512], BF16, "qt"); o.bqt = Buf("qt")
            o.kt = c.sb([128, 512], BF16, "kt"); o.bkt = Buf("kt")
            o.kh = c.sb([128, 512], BF16, "kh"); o.bkh = Buf("kh")
            o.khtok = c.sb([128, 4, 128], BF16, "khtok"); o.bkhtok = Buf("khtok")
            o.vtok = c.sb([128, 4, 256], BF16, "vtok"); o.bvtok = Buf("vtok")
            o.gz = c.sb([128, 4, 256], BF16, "gz"); o.bgz = Buf("gz")
            slots.append(o)
        zs = [c.sb([128, 256], F32, "zs") for _ in range(2)]; bzs = [Buf("zs0"), Buf("zs1")]
        attm = [c.sb([128, 128], BF16, "attm") for _ in range(2)]; battm = [Buf("attm0"), Buf("attm1")]
        _jk = c.sb([128, 256], BF16, "junk"); junk = [_jk, _jk]; _bj = Buf("junk"); bjunk = [_bj, _bj]
        bU = Buf("U")
        stat = c.sb([128, 2, 8], F32, "stat"); bstat = [Buf("stat0"), Buf("stat1")]
        ogt = c.sb([128, 2, 4, 512], BF16, "ogt"); bogt = [[Buf(f"ogt{p_}{j}") for j in range(4)] for p_ in range(2)]
        xT, bxT = self.xT, self.bxT
        dk_scale = 128.0 ** -0.5

        def prep_gen(T, h, o):
            tok = slice(T * 512, (T + 1) * 512)
            bx = bxT[T * 4:(T + 1) * 4]
            if h == 0:
                for ch in range(8):
                    c.op("pe", lambda: nc.tensor.matmul(pf[0][0:16, :], lhsT=wb[:, ch, 1024:1040], rhs=xT[:, ch, tok],
                                                        start=(ch == 0), stop=(ch == 7)),
                         reads=[bwb] + bx, writes=[bpf[0]])
                c.op("act", lambda: nc.scalar.copy(out=glT[:], in_=pf[0][0:16, :]), reads=[bpf[0]], writes=[bgl])
            yield
            c.op("pe", lambda: nc.tensor.matmul(pf[1][:], lhsT=gw2[:, h * 128:(h + 1) * 128], rhs=glT[:],
                                                start=True, stop=True), reads=[bsm, bgl], writes=[bpf[1]])
            c.op("act", lambda: nc.scalar.activation(out=o.e1[:], in_=pf[1][:], func=AF.Exp, scale=-1.0,
                                                     bias=negb[:, h:h + 1]), reads=[bpf[1], bsm], writes=[o.be1])
            c.op("act", lambda: nc.scalar.activation(out=o.sp[:], in_=o.e1[:], func=AF.Ln, bias=1.0, scale=1.0),
                 reads=[o.be1], writes=[o.bsp])
            for j in range(4):
                cs = slice(j * 128, (j + 1) * 128)
                c.op("dve", lambda: nc.vector.tensor_tensor_scan(out=o.bs[:, cs], data0=self.ones_f[:, :],
                                                                 data1=o.sp[:, cs], initial=0.0,
                                                                 op0=ALU.mult, op1=ALU.subtract),
                     reads=[o.bsp, self.bconst], writes=[o.bbs])
            c.op("act", lambda: nc.scalar.activation(out=o.eb[:], in_=o.bs[:], func=AF.Exp, scale=1.0 / 16),
                 reads=[o.bbs], writes=[o.beb])
            c.op("act", lambda: nc.scalar.activation(out=o.enb[:], in_=o.bs[:], func=AF.Exp, scale=-1.0 / 16),
                 reads=[o.bbs], writes=[o.benb])
            yield
            for ch in range(8):
                c.op("pe", lambda: nc.tensor.matmul(pf[0][:], lhsT=wb[:, ch, h * 128:(h + 1) * 128],
                                                    rhs=xT[:, ch, tok], start=(ch == 0), stop=(ch == 7)),
                     reads=[bwb] + bx, writes=[bpf[0]])
            c.op("dve", lambda: nc.vector.scalar_tensor_tensor(out=o.qt[:], in0=pf[0][:], scalar=dk_scale, in1=o.eb[:],
                                                               op0=ALU.mult, op1=ALU.mult),
                 reads=[bpf[0], o.beb], writes=[o.bqt])
            yield
            for ch in range(8):
                c.op("pe", lambda: nc.tensor.matmul(pf[1][:], lhsT=wb[:, ch, 256 + h * 128:256 + (h + 1) * 128],
                                                    rhs=xT[:, ch, tok], start=(ch == 0), stop=(ch == 7)),
                     reads=[bwb] + bx, writes=[bpf[1]])
            c.op("dve", lambda: nc.vector.tensor_tensor(out=o.kt[:], in0=pf[1][:], in1=o.enb[:], op=ALU.mult),
                 reads=[bpf[1], o.benb], writes=[o.bkt])
            for j in range(4):
                cs = slice(j * 128, (j + 1) * 128)
                c.op("dve", lambda: nc.vector.scalar_tensor_tensor(
                    out=o.kh[:, cs], in0=pf[1][:, cs], scalar=o.eb[:, j * 128 + 127:j * 128 + 128], in1=o.enb[:, cs],
                    op0=ALU.mult, op1=ALU.mult), reads=[bpf[1], o.beb, o.benb], writes=[o.bkh])
            yield
            for j in range(4):
                cs = slice(j * 128, (j + 1) * 128)
                c.op("pe", lambda: nc.tensor.transpose(out=pb[0][:, cs], in_=o.kh[:, cs], identity=self.ident_b[:]),
                     reads=[o.bkh, self.bconst], writes=[bpb[0]])
            c.op("act", lambda: nc.scalar.copy(out=o.khtok[:].rearrange("p j d -> p (j d)"), in_=pb[0][:, 0:512]),
                 reads=[bpb[0]], writes=[o.bkhtok])
            for j in range(4):
                yield
                bank = 2 + (j % 2)
                for ch in range(8):
                    c.op("pe", lambda: nc.tensor.matmul(
                        pf[bank][:, 0:256], lhsT=xT[:, ch, T * 512 + j * 128:T * 512 + (j + 1) * 128],
                        rhs=wb[:, ch, 512 + h * 256:512 + (h + 1) * 256], start=(ch == 0), stop=(ch == 7)),
                        reads=[bwb, bx[j]], writes=[bpf[bank]])
                yield
                for ch in range(8):
                    c.op("pe", lambda: nc.tensor.matmul(
                        pf[bank][:, 256:512], lhsT=xT[:, ch, T * 512 + j * 128:T * 512 + (j + 1) * 128],
                        rhs=wb[:, ch, 1040 + h * 256:1040 + (h + 1) * 256], start=(ch == 0), stop=(ch == 7)),
                        reads=[bwb, bx[j]], writes=[bpf[bank]])
                c.op("act", lambda: nc.scalar.copy(out=o.vtok[:, j, :], in_=pf[bank][:, 0:256]), reads=[bpf[bank]],
                     writes=[o.bvtok])
                zi = j % 2
                c.op("act", lambda: nc.scalar.activation(out=zs[zi][:], in_=pf[bank][:, 256:512], func=AF.Silu),
                     reads=[bpf[bank]], writes=[bzs[zi]])
                c.op("pool", lambda: nc.gpsimd.tensor_tensor(out=o.gz[:, j, :], in0=zs[zi][:],
                                                             in1=gng[:, h * 256:(h + 1) * 256], op=ALU.mult),
                     reads=[bzs[zi], bsm], writes=[o.bgz])

        def adv(gen, n):
            if gen is None:
                return
            for _ in range(n):
                try:
                    next(gen)
                except StopIteration:
                    return

        def recur(T, h, o, gen=None):
            par = T % 2
            for j in range(4):
                cs = slice(j * 128, (j + 1) * 128)
                ai = j % 2
                c.op("pe", lambda: nc.tensor.matmul(pf[4][:, 0:128], lhsT=o.kt[:, cs], rhs=o.qt[:, cs],
                                                    start=True, stop=True), reads=[o.bkt, o.bqt], writes=[bpf[4]])
                c.op("dve", lambda: nc.vector.tensor_tensor(out=attm[ai][:], in0=pf[4][:, 0:128], in1=self.tri_b[:],
                                                            op=ALU.mult), reads=[bpf[4], self.bconst], writes=[battm[ai]])
                c.op("pe", lambda: nc.tensor.matmul(pf[4][:, 256:512], lhsT=o.khtok[:, j, :], rhs=o.vtok[:, j, :],
                                                    start=True, stop=True), reads=[o.bkhtok, o.bvtok], writes=[bU])
                adv(gen, 2)
                for _ in range(L1_FILL):
                    c.op("pe", lambda: nc.tensor.matmul(pf[6][:, :], lhsT=self.ident_b[:, :], rhs=wo[:, 0, 0:512],
                                                        start=True, stop=True), reads=[bwo], writes=[])
                c.op("pe", lambda: nc.tensor.matmul(pf[5][:, 0:256], lhsT=attm[ai][:], rhs=o.vtok[:, j, :],
                                                    start=True, stop=False), reads=[battm[ai], o.bvtok], writes=[bpf[5]])
                c.op("pe", lambda: nc.tensor.matmul(pf[5][:, 0:256], lhsT=o.qt[:, cs], rhs=Sb[:, h, :],
                                                    start=False, stop=True), reads=[o.bqt, bSb[h]], writes=[bpf[5]])
                c.op("dve", lambda: nc.vector.scalar_tensor_tensor(
                    out=Sf[:, h, :], in0=Sf[:, h, :], scalar=o.eb[:, j * 128 + 127:j * 128 + 128],
                    in1=pf[4][:, 256:512], op0=ALU.mult, op1=ALU.add), reads=[bS[h], o.beb, bU], writes=[bS[h]])
                c.op("pool", lambda: nc.gpsimd.tensor_copy(out=Sb[:, h, :], in_=Sf[:, h, :]),
                     reads=[bS[h]], writes=[bSb[h]])
                c.op("act", lambda: nc.scalar.activation(out=junk[ai][:], in_=pf[5][:, 0:256], func=AF.Square,
                                                         accum_out=stat[:, ai, 0:1]), reads=[bpf[5]], writes=[bjunk[ai], bstat[ai]])
                c.op("dve", lambda: nc.vector.tensor_scalar(out=stat[:, ai, 1:2], in0=stat[:, ai, 0:1], scalar1=1.0 / 256,
                                                            scalar2=LN_EPS, op0=ALU.mult, op1=ALU.add),
                     reads=[bstat[ai]], writes=[bstat[ai]])
                c.op("act", lambda: nc.scalar.activation(out=stat[:, ai, 2:3], in_=stat[:, ai, 1:2], func=AF.Sqrt),
                     reads=[bstat[ai]], writes=[bstat[ai]])
                c.op("dve", lambda: nc.vector.reciprocal(out=stat[:, ai, 3:4], in_=stat[:, ai, 2:3]),
                     reads=[bstat[ai]], writes=[bstat[ai]])
                c.op("dve", lambda: nc.vector.scalar_tensor_tensor(
                    out=ogt[:, par, j, h * 256:(h + 1) * 256], in0=pf[5][:, 0:256], scalar=stat[:, ai, 3:4],
                    in1=o.gz[:, j, :], op0=ALU.mult, op1=ALU.mult), reads=[bpf[5], bstat[ai], o.bgz], writes=[bogt[par][j]])
                adv(gen, 2)
            adv(gen, 1000)

        def final(T):
            par = T % 2
            c.dma("pool", self.og1l[T // 4][(T % 4) * 512:(T % 4 + 1) * 512, :].rearrange("(s p) n -> p s n", p=128),
                  ogt[:, par, :, :], reads=bogt[par], writes=[self.bog1l[T // 4]])
            if T % 4 == 3:
                self.coll("AllGather", self.og1l[T // 4], self.og1g[T // 4], self.bog1l[T // 4], self.bog1g[T // 4])

        items = [(T, h) for T in range(NST) for h in range(2)]
        adv(prep_gen(items[0][0], items[0][1], slots[0]), 1000)
        for i, (T, h) in enumerate(items):
            if i + 1 < len(items):
                adv(prep_gen(items[i + 1][0], items[i + 1][1], slots[(i + 1) % 2]), 1000)
            recur(T, h, slots[i % 2], None)
            if h == 1:
                final(T)


    def proj_feat(self, bank, bbank, wt, bw, col0, m, T):
        c, nc = self.c, self.nc
        tok = slice(T * 512, (T + 1) * 512)
        for ch in range(8):
            c.op("pe", lambda: nc.tensor.matmul(bank[0:m, :], lhsT=wt[:, ch, col0:col0 + m], rhs=self.xT[:, ch, tok],
                                                start=(ch == 0), stop=(ch == 7)),
                 reads=[bw] + self.bxT[T * 4:(T + 1) * 4], writes=[bbank])

    def proj_tok(self, dst_ap, bbank, wt, bw, col0, n, t):
        c, nc = self.c, self.nc
        for ch in range(8):
            c.op("pe", lambda: nc.tensor.matmul(dst_ap, lhsT=self.xT[:, ch, t * 128:(t + 1) * 128],
                                                rhs=wt[:, ch, col0:col0 + n], start=(ch == 0), stop=(ch == 7)),
                 reads=[bw, self.bxT[t]], writes=[bbank])

    def attn_branch(self, T, ktiles, KAfn, nk_fn, QA_ap, bQA, extra_fn, Vfn, vcols, bK, sub_range_fn, n_fill=None):
        c, nc = self.c, self.nc
        first = {}
        last = {}
        for a in ktiles:
            for s_ in sub_range_fn(a):
                first.setdefault(s_, a)
                last[s_] = a
        pend = None

        def emit_pv(a, PT, bPT, nk):
            for s_ in sub_range_fn(a):
                c.op("pe", lambda: nc.tensor.matmul(self.pf[2 + s_][:, 0:vcols], lhsT=PT[0:nk, s_ * 128:(s_ + 1) * 128],
                                                    rhs=Vfn(a), start=(first[s_] == a), stop=(last[s_] == a)),
                     reads=[bPT, bK], writes=[self.bpf[2 + s_]])

        for a in ktiles:
            i = self.sc_rr
            self.sc_rr ^= 1
            bank, bb = self.pf[i], self.bpf[i]
            nk = nk_fn(a)
            ex = extra_fn(a)
            subs = sub_range_fn(a)
            c0, c1 = min(subs) * 128, (max(subs) + 1) * 128
            c.op("pe", lambda: nc.tensor.matmul(bank[0:nk, c0:c1], lhsT=KAfn(a), rhs=QA_ap[:, c0:c1], start=True,
                                                stop=(len(ex) == 0)), reads=[bK, bQA], writes=[bb])
            for ei, (l_ap, r_ap, bufs) in enumerate(ex):
                c.op("pe", lambda: nc.tensor.matmul(bank[0:nk, c0:c1], lhsT=l_ap, rhs=r_ap[:, c0:c1], start=False,
                                                    stop=(ei == len(ex) - 1)), reads=bufs, writes=[bb])
            PT, bPT = self.PT[i], self.bPT[i]
            c.op("act", lambda: nc.scalar.activation(out=PT[0:nk, c0:c1], in_=bank[0:nk, c0:c1], func=AF.Exp, scale=0.125),
                 reads=[bb], writes=[bPT])
            for _ in range(N_DUMMY if n_fill is None else n_fill):
                c.op("pe", lambda: nc.tensor.matmul(self.pf[6][:, 0:FILL_N], lhsT=self.ident_b[:, :], rhs=self.warm_rhs[:, 0:FILL_N],
                                                    start=True, stop=True), reads=[], writes=[])
            if pend is not None:
                emit_pv(*pend)
            pend = (a, PT, bPT, nk)
        emit_pv(*pend)

    def dbg_dump(self, blk, ap, buf, np_, ncols):
        c, nc = self.c, self.nc
        t = c.sb([128, 1024], F32, "dbgd"); b = Buf("dbgd")
        c.op("pool", lambda: nc.gpsimd.tensor_copy(out=t[0:np_, 0:ncols], in_=ap), reads=[buf], writes=[b])
        bo = Buf("dbgo"); self.out_bufs.append(bo)
        c.dma("sp", self.dbg[blk * 128:blk * 128 + np_, 0:ncols], t[0:np_, 0:ncols], reads=[b], writes=[bo])

    def layer0(self):
        c, nc, W, Cd = self.c, self.nc, self.W, self.Cd
        pf, bpf, pb, bpb = self.pf, self.bpf, self.pb, self.bpb
        xT, bxT = self.xT, self.bxT
        self.sc_rr = 0
        with c.phase():
            cm = c.sb([128, 4, 512], BF16, "cm"); wm = c.sb([128, 4, 512], BF16, "wm")
            bc0 = Buf("c0")
            c.dma("sp", cm[:], Cd["cm"][:, :, :], writes=[bc0])
            c.dma("sp", wm[:], Cd["wm"][:, :, :], writes=[bc0])
            self.warm_rhs = cm[:, 0, :]
            self.PT = [c.sb([128, 512], BF16, "PT") for _ in range(2)]
            self.bPT = [Buf("PT0"), Buf("PT1")]
            QA = c.sb([96, 4, 512], BF16, "QA"); bQA = Buf("QA")
            c.op("dve", lambda: nc.vector.memset(QA[:], 0.0), writes=[bQA])
            zs = c.sb([128, 4, 256], BF16, "zs0"); bzs = Buf("zs0")
            oh = c.sb([128, 4, 256], F32, "oh"); boh = [Buf(f"oh{s_}") for s_ in range(4)]
            og = c.sb([128, 4, 256], BF16, "og0"); bog = Buf("og0")
            st = c.sb([128, 16], F32, "st0"); bsts = [Buf(f"st0_{i}") for i in range(4)]

            def diag_extra(T):
                def f(a):
                    if a >= 4 * T:
                        return [(self.ident_b[:], cm[:, a - 4 * T, :], [self.bconst, bc0])]
                    return []
                return f

            def causal_subs(T):
                return lambda a: [s_ for s_ in range(4) if 4 * T + s_ >= a]

            wpass, bwpass = self.wpass, self.bwpass
            self.bw_kv = Buf("wM_kv")

            def load_pass_weights(p):
                wt, bw = wpass[p % 2], bwpass[p % 2]
                if p == 0:
                    c.dma("pool", wt[:, :, 256:768], W["ev_wm"][:, 256:768].rearrange("(c p) n -> p c n", p=128),
                          writes=[self.bw_kv])
                    c.dma("pool", wt[:, :, 0:256], W["ev_wm"][:, 0:256].rearrange("(c p) n -> p c n", p=128), writes=[bw])
                    c.dma("pool", wt[:, :, 768:1024], W["ev_wm"][:, 768:1024].rearrange("(c p) n -> p c n", p=128),
                          writes=[bw])
                elif p == 1:
                    self.load_weight_bf16(wt, bw, W["ev_wn"], 0, 908)

            load_pass_weights(0)
            for hq in range(1):
                with c.phase():
                    wM, bwM = wpass[hq % 2], bwpass[hq % 2]
                    self.load_xT_alloc()
                    self.load_xT_tiles(self.x, 0, 4)
                    load_pass_weights(hq + 1)
                    KA = c.sb([96, 4, S], BF16, "KAm"); bKA = Buf("KAm")
                    c.op("pool", lambda: nc.gpsimd.memset(KA[64:96, :, :], 0.0), writes=[bKA])
                    Vm = c.sb([128, NT, 4, 65], BF16, "Vm")
                    mobc = c.sb([128, NST, 256], F32, "mobc"); ownc = c.sb([128, NST, 256], F32, "ownc")
                    c.dma("sp", mobc[:], Cd["mobc4"][:, :, :], writes=[bc0])
                    c.dma("sp", ownc[:], Cd["ownc4"][:, :, :], writes=[bc0])
                    for j in range(4):
                        c.dma("sp", KA[64:83, j, :], Cd["kaug_m"][:, :], writes=[bKA])
                    c.op("pool", lambda: nc.gpsimd.memset(Vm[:, :, :, 64:65], 1.0), writes=[bKA])
                    kms = c.sb([64, 4, 16], F32, "kms"); kmT = c.sb([64, 4, 16], BF16, "kmT"); bkm = Buf("km")
                    gm = c.sb([128, 16, 16], F32, "gm"); m8 = c.sb([128, 16, 8], F32, "m8"); b1 = c.sb([128, 16, 16], F32, "b1")
                    bgm = Buf("gm"); bm = [Buf(f"bm{i}") for i in range(16)]
                    btok = c.sb([128, 16, 80], BF16, "btok"); bbtok = Buf("btok")
                    c.op("dve", lambda: nc.vector.memset(btok[:], 0.0), writes=[bbtok])
                    for T in range(NST):
                        tok = slice(T * 512, (T + 1) * 512)
                        if T + 1 < NST:
                            self.load_xT_tiles(self.x, (T + 1) * 4, (T + 2) * 4)
                        for jp in range(2):
                            i = jp % 2
                            self.proj_feat(pf[i], bpf[i], wM, self.bw_kv, 256 + jp * 128, 128, T)
                            c.op("act", lambda: nc.scalar.copy(out=KA[0:64, 2 * jp, tok], in_=pf[i][0:64, :]),
                                 reads=[bpf[i]], writes=[bKA])
                            c.op("dve", lambda: nc.vector.tensor_copy(out=KA[0:64, 2 * jp + 1, tok], in_=pf[i][64:128, :]),
                                 reads=[bpf[i]], writes=[bKA])
                        for s_ in range(4):
                            t = T * 4 + s_
                            bank = 2 + (s_ % 2)
                            self.proj_tok(pf[bank][:, 0:256], bpf[bank], wM, self.bw_kv, 512, 256, t)
                            c.op("act" if s_ % 2 == 0 else "dve",
                                 (lambda: nc.scalar.copy(out=Vm[:, t, :, 0:64],
                                                         in_=pf[bank][:, 0:256].rearrange("p (h d) -> p h d", h=4)))
                                 if s_ % 2 == 0 else
                                 (lambda: nc.vector.tensor_copy(out=Vm[:, t, :, 0:64],
                                                                in_=pf[bank][:, 0:256].rearrange("p (h d) -> p h d", h=4))),
                                 reads=[bpf[bank]], writes=[bKA])
                    for j in range(4):
                        c.op("dve", lambda: nc.vector.tensor_reduce(
                            out=kms[:, j, :], in_=KA[0:64, j, :].rearrange("p (n m) -> p n m", m=256), axis=AX.X,
                            op=ALU.add), reads=[bKA], writes=[bkm])
                    c.op("dve", lambda: nc.vector.tensor_scalar(out=kmT[:], in0=kms[:], scalar1=1.0 / 256, scalar2=None,
                                                                op0=ALU.mult), reads=[bkm], writes=[bkm])
                    for T in range(NST):
                        tok = slice(T * 512, (T + 1) * 512)
                        for jp in range(2):
                            i = jp % 2
                            self.proj_feat(pf[i], bpf[i], wM, bwM, jp * 128, 128, T)
                            c.op("act", lambda: nc.scalar.copy(out=QA[0:64, 2 * jp, :], in_=pf[i][0:64, :]),
                                 reads=[bpf[i]], writes=[bQA])
                            c.op("dve", lambda: nc.vector.tensor_copy(out=QA[0:64, 2 * jp + 1, :], in_=pf[i][64:128, :]),
                                 reads=[bpf[i]], writes=[bQA])
                        c.dma("sp", QA[80:83, :, :], Cd["qal"][:, 0:4, tok], writes=[bQA])
                        for s_ in range(4):
                            for j in range(4):
                                idx = s_ * 4 + j
                                c.op("pe", lambda: nc.tensor.matmul(pf[2][:, idx * 16:(idx + 1) * 16],
                                                                    lhsT=QA[0:64, j, s_ * 128:(s_ + 1) * 128],
                                                                    rhs=kmT[:, j, :], start=True, stop=True),
                                     reads=[bQA, bkm], writes=[bpf[2]])
                        c.op("dve", lambda: nc.vector.tensor_tensor(out=gm[:].rearrange("p i n -> p (i n)"), in0=pf[2][:, 0:256],
                                                                    in1=mobc[:, T, :], op=ALU.add),
                             reads=[bpf[2], bc0], writes=[bgm])
                        for idx in range(16):
                            c.op("dve", lambda: nc.vector.max(out=m8[:, idx, :], in_=gm[:, idx, :]), reads=[bgm], writes=[bm[idx]])
                            c.op("dve", lambda: nc.vector.tensor_scalar(out=b1[:, idx, :], in0=gm[:, idx, :], scalar1=m8[:, idx, 2:3],
                                                                        scalar2=1.0, op0=ALU.is_ge, op1=ALU.subtract),
                                 reads=[bgm, bm[idx]], writes=[bm[idx]])
                        c.op("dve", lambda: nc.vector.tensor_tensor(out=btok[:, :, 64:80], in0=b1[:],
                                                                    in1=ownc[:, T, :].rearrange("p (i n) -> p i n", n=16),
                                                                    op=ALU.max), reads=bm + [bc0], writes=[bbtok])
                        for s_ in range(4):
                            for j in range(4):
                                idx = s_ * 4 + j
                                slot = (idx % 8) * 128
                                c.op("pe", lambda: nc.tensor.transpose(out=pb[0][0:80, slot:slot + 128], in_=btok[:, idx, :],
                                                                       identity=self.ident_b[:]),
                                     reads=[bbtok, self.bconst], writes=[bpb[0]])
                                c.op("act", lambda: nc.scalar.copy(out=QA[64:80, j, s_ * 128:(s_ + 1) * 128],
                                                                   in_=pb[0][64:80, slot:slot + 128]), reads=[bpb[0]], writes=[bQA])
                        for s_ in range(4):
                            t = T * 4 + s_
                            bank = s_ % 2
                            self.proj_tok(pf[bank][:, 0:256], bpf[bank], wM, bwM, 768, 256, t)
                            c.op("act", lambda: nc.scalar.activation(out=zs[:, s_, :], in_=pf[bank][:, 0:256], func=AF.Silu),
                                 reads=[bpf[bank]], writes=[bzs])
                        for j in range(4):
                            self.attn_branch(T, list(range(4 * T + 4)),
                                             lambda a: KA[0:96, j, a * 128:(a + 1) * 128], lambda a: 128,
                                             QA[0:96, j, :], bQA, diag_extra(T),
                                             lambda a: Vm[:, a, j, :], 65, bKA, causal_subs(T))
                            for s_ in range(4):
                                bst = bsts[s_]; k0 = s_ * 4
                                c.op("dve", lambda: nc.vector.reciprocal(out=st[:, k0:k0 + 1], in_=pf[2 + s_][:, 64:65]),
                                     reads=[bpf[2 + s_]], writes=[bst])
                                c.op("dve", lambda: nc.vector.tensor_scalar(out=oh[:, s_, j * 64:(j + 1) * 64],
                                                                            in0=pf[2 + s_][:, 0:64], scalar1=st[:, k0:k0 + 1],
                                                                            scalar2=None, op0=ALU.mult),
                                     reads=[bpf[2 + s_], bst], writes=[boh[s_]])
                        c.op("pool", lambda: nc.gpsimd.tensor_tensor(out=og[:], in0=oh[:], in1=zs[:], op=ALU.mult),
                             reads=boh + [bzs], writes=[bog])
                        if self.mode == "dbg":
                            self.dbg_dump(0, QA[0:83, 0, :], bQA, 83, 512)
                            self.dbg_dump(1, KA[0:83, 0, 0:512], bKA, 83, 512)
                            self.dbg_dump(2, zs[:, 0, :], bzs, 128, 256)
                            self.dbg_dump(3, oh[:, 0, :], boh[0], 128, 256)
                            self.dbg_dump(4, Vm[:, 0, 0, :], bKA, 128, 65)
                            self.dbg_dump(5, kmT[:, 0, :], bkm, 64, 16)
                            self.dbg_dump(6, self.PT[0][:], self.bPT[0], 128, 512)
                            self.dbg_dump(7, og[:, 0, :], bog, 128, 256)
                            self.dbg_dump(8, pf[2][:, 0:65], bpf[2], 128, 65)
                            raise _Stop()
                        c.dma("pool", self.ogl[T // 4][(T % 4) * 512:(T % 4 + 1) * 512, 0:256].rearrange("(s p) n -> p s n", p=128),
                              og[:], reads=[bog], writes=[self.bogl[T // 4]])

            for g in range(1):
                with c.phase():
                    wN, bwN = wpass[1], bwpass[1]
                    self.load_weight_bf16(wpass[0], bwpass[0], W["ev_w_out"], 0, D)
                    KS = c.sb([96, S], BF16, "KS"); KWn = c.sb([96, S], BF16, "KW"); bKN = Buf("KN")
                    c.op("pool", lambda: nc.gpsimd.memset(KS[64:96, :], 0.0), writes=[bKN])
                    c.op("pool", lambda: nc.gpsimd.memset(KWn[64:96, :], 0.0), writes=[bKN])
                    c.op("dve", lambda: nc.vector.memset(QA[64:96, :, :], 0.0), writes=[bQA])
                    Vs = c.sb([128, NT, 65], BF16, "Vs"); Vw = c.sb([128, NT, 65], BF16, "Vw")
                    c.dma("sp", KS[64:67, :], Cd["kaug_n"][:, :], writes=[bKN])
                    c.dma("sp", KWn[64:67, :], Cd["kaug_n"][:, :], writes=[bKN])
                    c.op("pool", lambda: nc.gpsimd.memset(Vs[:, :, 64:65], 1.0), writes=[bKN])
                    c.op("pool", lambda: nc.gpsimd.memset(Vw[:, :, 64:65], 1.0), writes=[bKN])
                    esel = c.sb([96, S], BF16, "esel"); selc = c.sb([128, NT, 64], F32, "selc")
                    besel = Buf("esel")
                    c.op("pool", lambda: nc.gpsimd.memset(esel[64:96, :], 0.0), writes=[besel])
                    c.dma("sp", esel[0:64, :], Cd["esel"][:, :], writes=[besel])
                    c.dma("sp", selc[:], Cd["selc"][:, :, :], writes=[bc0])
                    KCA = c.sb([96, 256], BF16, "KCA"); VCA = c.sb([128, 2, 129], BF16, "VCA"); bKC = Buf("KC")
                    c.op("dve", lambda: nc.vector.memset(VCA[:], 0.0), writes=[bKC])
                    c.op("dve", lambda: nc.vector.memset(KCA[:], 0.0), writes=[bKC])
                    c.dma("sp", KCA[64:67, 0:255], Cd["kaug_c"][:, :], writes=[bKC])
                    c.dma("sp", VCA[:, :, 65:129], Cd["ovl"][:, :, :], writes=[bKC])
                    c.op("pool", lambda: nc.gpsimd.memset(VCA[:, :, 64:65], 1.0), writes=[bKC])
                    w2b = c.sb([128, 2, 64], BF16, "w2b"); w2f = c.sb([128, 2, 64], F32, "w2f"); bw2 = Buf("w2")
                    c.dma("sp", w2f[:], W["ev_cmp_w2"].rearrange("k h d -> h k d"), writes=[bw2])
                    c.op("dve", lambda: nc.vector.tensor_copy(out=w2b[:], in_=w2f[:]), reads=[bw2], writes=[bw2])
                    b1t = c.sb([128, 2], F32, "b1t")
                    c.dma("sp", b1t[:], W["ev_cmp_b1"][:, :], writes=[bw2])
                    with c.phase():
                        cmpT = c.sb([128, S], BF16, "cmpT"); bcmpT = Buf("cmpT")
                        w1f = c.sb([128, 32, 128], F32, "w1f"); w1b = c.sb([128, 32, 128], BF16, "w1b"); bw1 = Buf("w1")
                        posf = c.sb([128, 32], F32, "posf"); posb = c.sb([128, 32], BF16, "posb")
                        for kv in range(2):
                            c.dma("sp", w1f[kv * 64:(kv + 1) * 64, :, :],
                                  W["ev_cmp_w1"][kv].rearrange("(l d) h -> d l h", d=64), writes=[bw1])
                            c.dma("sp", posf[kv * 64:(kv + 1) * 64, :], W["ev_cmp_pos"][kv], writes=[bw1])
                        c.op("pool", lambda: nc.gpsimd.tensor_copy(out=w1b[:], in_=w1f[:]), reads=[bw1], writes=[bw1])
                        c.op("dve", lambda: nc.vector.tensor_copy(out=posb[:], in_=posf[:]), reads=[bw1], writes=[bw1])
                        for T in range(NST):
                            tok = slice(T * 512, (T + 1) * 512)
                            self.proj_feat(pf[0], bpf[0], wN, bwN, 256, 128, T)
                            c.op("act", lambda: nc.scalar.copy(out=cmpT[:, tok], in_=pf[0][:, :]), reads=[bpf[0]],
                                 writes=[bcmpT])
                            self.proj_feat(pf[1], bpf[1], wN, bwN, 384, 128, T)
                            c.op("dve", lambda: nc.vector.tensor_copy(out=KS[0:64, tok], in_=pf[1][0:64, :]),
                                 reads=[bpf[1]], writes=[bKN])
                            c.op("act", lambda: nc.scalar.copy(out=KWn[0:64, tok], in_=pf[1][64:128, :]), reads=[bpf[1]],
                                 writes=[bKN])
                            for s_ in range(4):
                                t = T * 4 + s_
                                bank = 2 + (s_ % 2)
                                self.proj_tok(pf[bank][:, 0:128], bpf[bank], wN, bwN, 512, 128, t)
                                c.op("dve", lambda: nc.vector.tensor_copy(out=Vs[:, t, 0:64], in_=pf[bank][:, 0:64]),
                                     reads=[bpf[bank]], writes=[bKN])
                                c.op("act", lambda: nc.scalar.copy(out=Vw[:, t, 0:64], in_=pf[bank][:, 64:128]),
                                     reads=[bpf[bank]], writes=[bKN])
                        hidT = c.sb([128, 256], BF16, "hidT"); bhid = Buf("hid")
                        bh = c.sb([128, 2], F32, "bh")
                        for kv in range(2):
                            rows = slice(kv * 64, (kv + 1) * 64)
                            for l in range(32):
                                c.op("pe", lambda: nc.tensor.matmul(pf[0][:, 0:255], lhsT=w1b[rows, l, :],
                                                                    rhs=cmpT[rows, l:l + 16 * 254 + 1:16],
                                                                    start=(l == 0), stop=(l == 31)),
                                     reads=[bw1, bcmpT], writes=[bpf[0]])
                            for l in range(32):
                                c.op("pe", lambda: nc.tensor.matmul(pf[1][:, 0:1], lhsT=w1b[rows, l, :],
                                                                    rhs=posb[rows, l:l + 1], start=(l == 0), stop=(l == 31)),
                                     reads=[bw1], writes=[bpf[1]])
                            c.op("dve", lambda: nc.vector.tensor_tensor(out=bh[:, kv:kv + 1], in0=pf[1][:, 0:1],
                                                                        in1=b1t[:, kv:kv + 1], op=ALU.add),
                                 reads=[bpf[1], bw2], writes=[bhid])
                            c.op("act", lambda: nc.scalar.activation(out=hidT[:, 0:255], in_=pf[0][:, 0:255], func=AF.Silu,
                                                                     bias=bh[:, kv:kv + 1], scale=1.0),
                                 reads=[bpf[0], bhid], writes=[bhid])
                            if kv == 0:
                                c.op("pe", lambda: nc.tensor.matmul(pf[2][0:64, 0:255], lhsT=w2b[:, 0, :], rhs=hidT[:, 0:255],
                                                                    start=True, stop=True), reads=[bw2, bhid], writes=[bpf[2]])
                                c.op("dve", lambda: nc.vector.tensor_copy(out=KCA[0:64, 0:255], in_=pf[2][0:64, 0:255]),
                                     reads=[bpf[2]], writes=[bKC])
                            else:
                                for nt_, nn in enumerate([128, 127]):
                                    c.op("pe", lambda: nc.tensor.matmul(pf[2][0:nn, 0:64], lhsT=hidT[:, nt_ * 128:nt_ * 128 + nn],
                                                                        rhs=w2b[:, 1, :], start=True, stop=True),
                                         reads=[bw2, bhid], writes=[bpf[2]])
                                    c.op("dve", lambda: nc.vector.tensor_copy(out=VCA[0:nn, nt_, 0:64], in_=pf[2][0:nn, 0:64]),
                                         reads=[bpf[2]], writes=[bKC])
                    cmpm = c.sb([128, 2, 512], BF16, "cmpm"); bcmpm = Buf("cmpm")
                    BST = c.sb([96, 512], BF16, "BST"); bBST = Buf("BST")
                    c.op("pool", lambda: nc.gpsimd.memset(BST[64:96, :], 0.0), writes=[bBST])
                    sg = c.sb([128, 4, 12], F32, "sg"); bsg = Buf("sg")
                    imp = c.sb([128, 4, 64], F32, "imp"); bimp = [Buf(f"imp{s_}") for s_ in range(4)]
                    sc = c.sb([128, 64], F32, "sc"); sc2 = c.sb([128, 64], F32, "sc2")
                    m8a = c.sb([128, 8], F32, "m8a"); m8b = c.sb([128, 8], F32, "m8b"); bsc = Buf("sc")
                    bsel = c.sb([128, 64], BF16, "bsel")
                    nkc = [128, 127]
                    for T in range(NST):
                        tok = slice(T * 512, (T + 1) * 512)
                        c.dma("sp", cmpm[:], Cd["cmpm"][:, :, tok], writes=[bcmpm])
                        for jp in range(2):
                            i = jp % 2
                            self.proj_feat(pf[i], bpf[i], wN, bwN, jp * 128, 128, T)
                            c.op("act", lambda: nc.scalar.copy(out=QA[0:64, 2 * jp, :], in_=pf[i][0:64, :]),
                                 reads=[bpf[i]], writes=[bQA])
                            c.op("dve", lambda: nc.vector.tensor_copy(out=QA[0:64, 2 * jp + 1, :], in_=pf[i][64:128, :]),
                                 reads=[bpf[i]], writes=[bQA])
                        c.dma("sp", QA[64:67, :, :], Cd["qal"][:, 0:4, tok], writes=[bQA])
                        for s_ in range(4):
                            t = T * 4 + s_
                            bank = s_ % 2
                            self.proj_tok(pf[bank][:, 0:256], bpf[bank], wN, bwN, 652, 256, t)
                            c.op("act", lambda: nc.scalar.activation(out=zs[:, s_, :], in_=pf[bank][:, 0:256], func=AF.Silu),
                                 reads=[bpf[bank]], writes=[bzs])
                        for s_ in range(4):
                            t = T * 4 + s_
                            self.proj_tok(pf[2][:, s_ * 12:(s_ + 1) * 12], bpf[2], wN, bwN, 640, 12, t)
                        c.op("act", lambda: nc.scalar.activation(out=sg[:].rearrange("p s n -> p (s n)"), in_=pf[2][:, 0:48],
                                                                 func=AF.Sigmoid), reads=[bpf[2]], writes=[bsg])
                        for j in range(4):
                            self.attn_branch(
                                T, [0, 1], lambda a: KCA[0:96, a * 128:a * 128 + nkc[a]], lambda a: nkc[a],
                                QA[0:96, j, :], bQA,
                                lambda a: [(self.ident_b[0:nkc[a], 0:nkc[a]], cmpm[0:nkc[a], a, :], [self.bconst, bcmpm])],
                                lambda a: VCA[0:nkc[a], a, :], 129, bKC, lambda a: [0, 1, 2, 3])
                            for s_ in range(4):
                                acc, bacc = pf[2 + s_], bpf[2 + s_]
                                bst = bsts[s_]; k0 = s_ * 4
                                c.op("dve", lambda: nc.vector.tensor_scalar(out=st[:, k0 + 0:k0 + 1], in0=acc[:, 64:65], scalar1=1e-30,
                                                                            scalar2=None, op0=ALU.max), reads=[bacc], writes=[bst])
                                c.op("dve", lambda: nc.vector.reciprocal(out=st[:, k0 + 1:k0 + 2], in_=st[:, k0 + 0:k0 + 1]), reads=[bst], writes=[bst])
                                c.op("dve", lambda: nc.vector.tensor_tensor(out=st[:, k0 + 2:k0 + 3], in0=st[:, k0 + 1:k0 + 2],
                                                                            in1=sg[:, s_, j * 3:j * 3 + 1], op=ALU.mult),
                                     reads=[bst, bsg], writes=[bst])
                                c.op("dve", lambda: nc.vector.tensor_scalar(out=oh[:, s_, j * 64:(j + 1) * 64], in0=acc[:, 0:64],
                                                                            scalar1=st[:, k0 + 2:k0 + 3], scalar2=None, op0=ALU.mult),
                                     reads=[bacc, bst], writes=[boh[s_]])
                                if j == 0:
                                    c.op("dve", lambda: nc.vector.tensor_scalar(out=imp[:, s_, :], in0=acc[:, 65:129],
                                                                                scalar1=st[:, k0 + 1:k0 + 2], scalar2=None, op0=ALU.mult),
                                         reads=[bacc, bst], writes=[bimp[s_]])
                                else:
                                    c.op("dve", lambda: nc.vector.scalar_tensor_tensor(
                                        out=imp[:, s_, :], in0=acc[:, 65:129], scalar=st[:, k0 + 1:k0 + 2], in1=imp[:, s_, :],
                                        op0=ALU.mult, op1=ALU.add), reads=[bacc, bst, bimp[s_]], writes=[bimp[s_]])
                        for s_ in range(4):
                            t = T * 4 + s_
                            c.op("dve", lambda: nc.vector.tensor_tensor(out=sc[:], in0=imp[:, s_, :], in1=selc[:, t, :], op=ALU.add),
                                 reads=[bimp[s_], bc0], writes=[bsc])
                            c.op("dve", lambda: nc.vector.max(out=m8a[:], in_=sc[:]), reads=[bsc], writes=[bsc])
                            c.op("dve", lambda: nc.vector.match_replace(out=sc2[:], in_to_replace=m8a[:], in_values=sc[:],
                                                                        imm_value=-3e38), reads=[bsc], writes=[bsc])
                            c.op("dve", lambda: nc.vector.max(out=m8b[:], in_=sc2[:]), reads=[bsc], writes=[bsc])
                            c.op("dve", lambda: nc.vector.tensor_scalar(out=bsel[:], in0=sc[:], scalar1=m8b[:, 7:8], scalar2=1.0,
                                                                        op0=ALU.is_ge, op1=ALU.subtract), reads=[bsc], writes=[bsc])
                            c.op("pe", lambda: nc.tensor.transpose(out=pb[0][0:64, s_ * 128:(s_ + 1) * 128], in_=bsel[:],
                                                                   identity=self.ident_b[:]), reads=[bsc, self.bconst],
                                 writes=[bpb[0]])
                        c.op("act", lambda: nc.scalar.copy(out=BST[0:64, :], in_=pb[0][0:64, 0:512]), reads=[bpb[0]], writes=[bBST])
                        for j in range(4):
                            def sel_extra(a):
                                ex = [(esel[:, a * 128:(a + 1) * 128], BST[:], [besel, bBST])]
                                if a >= 4 * T:
                                    ex.append((self.ident_b[:], cm[:, a - 4 * T, :], [self.bconst, bc0]))
                                return ex
                            self.attn_branch(T, list(range(4 * T + 4)), lambda a: KS[0:96, a * 128:(a + 1) * 128],
                                             lambda a: 128, QA[0:96, j, :], bQA, sel_extra,
                                             lambda a: Vs[:, a, :], 65, bKN, causal_subs(T), n_fill=N_FILL_SEL)
                            for br, gi in ((0, 1),):
                                for s_ in range(4):
                                    acc, bacc = pf[2 + s_], bpf[2 + s_]
                                    bst = bsts[s_]; k0 = s_ * 4
                                    c.op("dve", lambda: nc.vector.reciprocal(out=st[:, k0 + 1:k0 + 2], in_=acc[:, 64:65]),
                                         reads=[bacc], writes=[bst])
                                    c.op("dve", lambda: nc.vector.tensor_tensor(out=st[:, k0 + 2:k0 + 3], in0=st[:, k0 + 1:k0 + 2],
                                                                                in1=sg[:, s_, j * 3 + gi:j * 3 + gi + 1],
                                                                                op=ALU.mult), reads=[bst, bsg], writes=[bst])
                                    c.op("dve", lambda: nc.vector.scalar_tensor_tensor(
                                        out=oh[:, s_, j * 64:(j + 1) * 64], in0=acc[:, 0:64], scalar=st[:, k0 + 2:k0 + 3],
                                        in1=oh[:, s_, j * 64:(j + 1) * 64], op0=ALU.mult, op1=ALU.add),
                                        reads=[bacc, bst, boh[s_]], writes=[boh[s_]])

                            def win_extra(a):
                                if a >= 4 * T:
                                    return [(self.ident_b[:], cm[:, a - 4 * T, :], [self.bconst, bc0])]
                                return [(self.ident_b[:], wm[:, a - (4 * T - 4), :], [self.bconst, bc0])]

                            def win_subs(a):
                                return [s_ for s_ in range(4) if 4 * T + s_ - 4 <= a <= 4 * T + s_]
                            self.attn_branch(T, list(range(max(0, 4 * T - 4), 4 * T + 4)),
                                             lambda a: KWn[0:96, a * 128:(a + 1) * 128], lambda a: 128, QA[0:96, j, :], bQA,
                                             win_extra, lambda a: Vw[:, a, :], 65, bKN, win_subs)
                            for s_ in range(4):
                                acc, bacc = pf[2 + s_], bpf[2 + s_]
                                bst = bsts[s_]; k0 = s_ * 4
                                c.op("dve", lambda: nc.vector.reciprocal(out=st[:, k0 + 1:k0 + 2], in_=acc[:, 64:65]),
                                     reads=[bacc], writes=[bst])
                                c.op("dve", lambda: nc.vector.tensor_tensor(out=st[:, k0 + 2:k0 + 3], in0=st[:, k0 + 1:k0 + 2],
                                                                            in1=sg[:, s_, j * 3 + 2:j * 3 + 3],
                                                                            op=ALU.mult), reads=[bst, bsg], writes=[bst])
                                c.op("dve", lambda: nc.vector.scalar_tensor_tensor(
                                    out=oh[:, s_, j * 64:(j + 1) * 64], in0=acc[:, 0:64], scalar=st[:, k0 + 2:k0 + 3],
                                    in1=oh[:, s_, j * 64:(j + 1) * 64], op0=ALU.mult, op1=ALU.add),
                                    reads=[bacc, bst, boh[s_]], writes=[boh[s_]])
                        c.op("pool", lambda: nc.gpsimd.tensor_tensor(out=og[:], in0=oh[:], in1=zs[:], op=ALU.mult),
                             reads=boh + [bzs], writes=[bog])
                        c.dma("pool", self.ogl[T // 4][(T % 4) * 512:(T % 4 + 1) * 512, 256:512].rearrange("(s p) n -> p s n", p=128),
                              og[:], reads=[bog], writes=[self.bogl[T // 4]])
                        if T % 4 == 3:
                            self.coll("AllGather", self.ogl[T // 4], self.ogg[T // 4], self.bogl[T // 4], self.bogg[T // 4])

    def final_pass(self, ogg, bogg, wo_src, res_src, res_bufs, lng_src, lnb_src, dst, dst_bufs, make_xT):
        c, nc = self.c, self.nc
        pf, bpf, pb, bpb = self.pf, self.bpf, self.pb, self.bpb
        NTH = NT // 2
        self.store_q = "act"
        with c.phase():
            if isinstance(wo_src, tuple):
                wo, bwo = wo_src
            else:
                wo = c.sb([128, 8, D], BF16, "wo0"); bwo = Buf("wo0")
                self.load_weight_bf16(wo, bwo, wo_src, 0, D)
            lng = c.sb([128, D], F32, "lng0"); lnb = c.sb([128, D], F32, "lnb0"); bsm = Buf("small0")
            c.dma("sp", lng[:], lng_src[0:1, :].partition_broadcast(128), writes=[bsm])
            c.dma("sp", lnb[:], lnb_src[0:1, :].partition_broadcast(128), writes=[bsm])
            cand_ = [c.sb([128, 2, 2, 512], BF16, "cand") for _ in range(2)]; bcand_ = [Buf("cand0"), Buf("cand1")]
            ogt_ = [c.sb([128, D], BF16, "ogt0") for _ in range(2)]; bogt_ = [Buf("ogt0a"), Buf("ogt0b")]
            ogT_ = [c.sb([128, 8, 128], BF16, "ogT0") for _ in range(2)]; bogT_ = [Buf("ogT0a"), Buf("ogT0b")]
            res_ = [c.sb([128, D], F32, "res0") for _ in range(2)]; bres_ = [Buf("res0a"), Buf("res0b")]
            rr_ = [c.sb([128, D], F32, "rr0") for _ in range(2)]; brr_ = [Buf("rr0a"), Buf("rr0b")]
            lnst_ = [c.sb([128, 12], F32, "lnst0") for _ in range(2)]; lnmv_ = [c.sb([128, 8], F32, "lnmv0") for _ in range(2)]
            bln_ = [Buf("ln0a"), Buf("ln0b")]
            if make_xT:
                self.xTt = [c.sb([128, 8, 128], BF16, "xTt") for _ in range(2)]; self.bxTt = [Buf("xTt0"), Buf("xTt1")]

            def stage_a_pe(t):
                p_ = t % 2
                cand, bcand = cand_[p_], bcand_[p_]
                ogt, bogt, ogT, bogT = ogt_[p_], bogt_[p_], ogT_[p_], bogT_[p_]
                res, bres = res_[p_], bres_[p_]
                for h_ in range(2):
                    src_ap = ogg[h_].rearrange("(r n) f -> n r f", r=2)[t * 128:(t + 1) * 128, :, :]
                    c.dma("sp", cand[:, :, h_, :], src_ap, reads=[bogg[h_]], writes=[bcand])
                c.dma("sp", res[:], res_src[t * 128:(t + 1) * 128, :],
                      reads=([res_bufs[t]] if res_bufs else []), writes=[bres])
                for r_ in range(2):
                    o_ = ogt[:, r_ * 512:(r_ + 1) * 512]
                    c.op("dve", lambda: nc.vector.tensor_scalar(out=o_, in0=cand[:, r_, 0, :], scalar1=self.sel[:, 0:1],
                                                                scalar2=None, op0=ALU.mult),
                         reads=[bcand, self.bconst], writes=[bogt])
                    c.op("dve", lambda: nc.vector.scalar_tensor_tensor(out=o_, in0=cand[:, r_, 1, :], scalar=self.sel[:, 1:2],
                                                                       in1=o_, op0=ALU.mult, op1=ALU.add),
                         reads=[bcand, self.bconst, bogt], writes=[bogt])
                for ch in range(8):
                    c.op("pe", lambda: nc.tensor.transpose(out=pb[1][:, ch * 128:(ch + 1) * 128],
                                                           in_=ogt[:, ch * 128:(ch + 1) * 128], identity=self.ident_b[:]),
                         reads=[bogt, self.bconst], writes=[bpb[1]])
                c.op("act", lambda: nc.scalar.copy(out=ogT[:].rearrange("p c n -> p (c n)"), in_=pb[1][:]),
                     reads=[bpb[1]], writes=[bogT])
                for half in range(2):
                    bk = 4 + half
                    for ch in range(8):
                        c.op("pe", lambda: nc.tensor.matmul(pf[bk][:], lhsT=ogT[:, ch, :],
                                                            rhs=wo[:, ch, half * 512:(half + 1) * 512],
                                                            start=(ch == 0), stop=(ch == 7)), reads=[bogT, bwo], writes=[bpf[bk]])

            def stage_a_dve(t):
                p_ = t % 2
                res, bres, rr, brr = res_[p_], bres_[p_], rr_[p_], brr_[p_]
                for half in range(2):
                    bk = 4 + half
                    c.op("dve", lambda: nc.vector.scalar_tensor_tensor(
                        out=rr[:, half * 512:(half + 1) * 512], in0=res[:, half * 512:(half + 1) * 512], scalar=ALPHA,
                        in1=pf[bk][:], op0=ALU.mult, op1=ALU.add), reads=[bres, bpf[bk]], writes=[brr])

            def stage_bc(t):
                p_ = t % 2
                rr, brr = rr_[p_], brr_[p_]
                self.ln_stats, self.ln_mv, self.bln = lnst_[p_], lnmv_[p_], bln_[p_]
                self.ln_store(rr, brr, lng, lnb, bsm, dst, dst_bufs[t], t, also_T=make_xT)

            stage_a_pe(0)
            stage_a_dve(0)
            for t in range(NTH):
                if t + 1 < NTH:
                    stage_a_pe(t + 1)
                stage_bc(t)
                if t + 1 < NTH:
                    stage_a_dve(t + 1)


_CACHE = {}


def _get_prog(mode):
    if mode not in _CACHE:
        _CACHE[mode] = Prog(mode)
    return _CACHE[mode]


def _in_map(p, inputs, core):
    b, r = core // 2, core % 2
    asc = np.ascontiguousarray
    ar = np.arange

    def f(k):
        return np.asarray(inputs[k], dtype=np.float32)[0]
    x = np.asarray(inputs["x"], dtype=np.float32)[b]
    m = {"x": asc(x), "xh": asc(x[r * SH:(r + 1) * SH])}
    w_in = f("ev_w_in")
    cols_m = np.concatenate([ar(base + r * 256, base + (r + 1) * 256) for base in (1816, 2328, 2840, 3352)])
    m["ev_wm"] = asc(w_in[:, cols_m])
    cols_n = np.concatenate([ar(r * 256, (r + 1) * 256)]
                            + [ar(512 + si * 128 + r * 64, 512 + si * 128 + (r + 1) * 64) for si in (0, 1, 2, 4, 3, 5)]
                            + [ar(1280 + r * 12, 1280 + (r + 1) * 12), ar(1304 + r * 256, 1304 + (r + 1) * 256)])
    m["ev_wn"] = asc(w_in[:, cols_n])
    m["ev_cmp_pos"] = asc(f("ev_cmp_pos").transpose(0, 2, 1))
    m["ev_cmp_w1"] = asc(f("ev_cmp_w1"))
    m["ev_cmp_b1"] = asc(f("ev_cmp_b1").T)
    m["ev_cmp_w2"] = asc(f("ev_cmp_w2"))
    perm = np.concatenate([ar(512, 768), ar(0, 256), ar(768, 1024), ar(256, 512)])
    m["ev_w_out"] = asc(f("ev_w_out")[perm, :])
    m["ev_ln_g"] = asc(f("ev_ln_g").reshape(1, D))
    m["ev_ln_b"] = asc(f("ev_ln_b").reshape(1, D))
    w1 = f("od_w_in")
    cols1 = np.concatenate([ar(r * 256, (r + 1) * 256), ar(512 + r * 256, 512 + (r + 1) * 256),
                            ar(1024 + r * 512, 1024 + (r + 1) * 512), ar(2048, 2064),
                            ar(2064 + r * 512, 2064 + (r + 1) * 512)])
    m["od_w1"] = asc(w1[:, cols1])
    m["od_gate_w2"] = asc(f("od_gate_w2")[:, r * 256:(r + 1) * 256])
    m["od_gate_b"] = asc(f("od_gate_b")[r * 256:(r + 1) * 256].reshape(2, 128).T)
    m["od_gn_g"] = asc(f("od_gn_g")[r * 512:(r + 1) * 512].reshape(1, 512))
    m["od_w_out"] = asc(f("od_w_out"))
    m["od_ln_g"] = asc(f("od_ln_g").reshape(1, D))
    m["od_ln_b"] = asc(f("od_ln_b").reshape(1, D))
    for nm, arr in make_consts(r).items():
        m["c_" + nm] = arr
    return m


def kernel(**inputs):
    inputs = {k: np.asarray(v) for k, v in inputs.items()}
    p = _get_prog("full")
    in_maps = [_in_map(p, inputs, core) for core in range(8)]
    res = run_bass_kernel_spmd(p.nc, in_maps, core_ids=list(range(8)))
    out = np.empty((4, S, D), np.float32)
    for core in range(8):
        b, r = core // 2, core % 2
        out[b, r * SH:(r + 1) * SH] = res.results[core]["out"]
    return out
```
